# Optimizing a Trainium2 kernel written in Bass

```python
import jax
import jax.numpy as jnp
from jax import lax
import numpy as np


D_MODEL = 1024
BATCH = 4
SEQ = 4096
DEPTH = 2

GRID_W = 64
CTX_LEN = 256
Q_BLOCK = 128
ROPE_THETA = 10000.0
EPS = 1e-6

MLA_HEADS = 8
MLA_NOPE = 64
MLA_ROPE = 32
MLA_V = 64
MLA_Q_LORA = 256
MLA_KV_LORA = 128
MLA_SCALE = (MLA_NOPE + MLA_ROPE) ** -0.5
GQA_HEADS = 8
GQA_KV_HEADS = 2
GQA_HEAD_DIM = 64
GQA_SCALE = GQA_HEAD_DIM ** -0.5
ATTN_IN = MLA_Q_LORA + MLA_KV_LORA + MLA_ROPE + (GQA_HEADS + 2 * GQA_KV_HEADS) * GQA_HEAD_DIM
ATTN_OUT = MLA_HEADS * MLA_V + GQA_HEADS * GQA_HEAD_DIM

SSM_HEADS = 16
SSM_HEAD_DIM = 64
SSM_GROUPS = 2
SSM_HPG = SSM_HEADS // SSM_GROUPS
SSM_STATE = 128
SSM_CONV = 5
SSD_CHUNK = 128
SSM_D = SSM_HEADS * SSM_HEAD_DIM
SSM_XBC = SSM_D + 2 * SSM_GROUPS * SSM_STATE
CONF_D = D_MODEL
CONF_K = 31
SSM_IN = SSM_D + SSM_XBC + SSM_HEADS + 2 * CONF_D
SSM_OUT = SSM_D + CONF_D

N_EXPERTS = 32
MOE_TOP_K = 4
MOE_FF = D_MODEL
SWIGLU_LIMIT = 7.0
SWIGLU_ALPHA = 1.702
MOE_BLOCK = 256

N_ATTN_LAYERS = (DEPTH + 1) // 2
N_SSM_LAYERS = DEPTH // 2

kernel_name = 'hybrid_mla_gqa_ssd_conformer_moe_dit'


def rms_norm(x, g):
    xf = x.astype(jnp.float32)
    y = xf * lax.rsqrt(jnp.mean(xf * xf, axis=-1, keepdims=True) + EPS)
    return (y * g.astype(jnp.float32)).astype(x.dtype)


def layer_norm(x, g, b):
    xf = x.astype(jnp.float32)
    mu = jnp.mean(xf, axis=-1, keepdims=True)
    var = jnp.mean(jnp.square(xf - mu), axis=-1, keepdims=True)
    y = (xf - mu) * lax.rsqrt(var + EPS)
    return (y * g.astype(jnp.float32) + b.astype(jnp.float32)).astype(x.dtype)


def modulate(h, shift, scale):
    return h * (1.0 + scale) + shift


def axial_rope_tables(n_tok, rot_dim):
    rows = n_tok // GRID_W
    row = jnp.broadcast_to(jnp.arange(rows, dtype=jnp.float32)[:, None], (rows, GRID_W)).reshape(-1)
    col = jnp.broadcast_to(jnp.arange(GRID_W, dtype=jnp.float32)[None, :], (rows, GRID_W)).reshape(-1)
    n_freq = rot_dim // 4
    inv_freq = ROPE_THETA ** (-jnp.arange(n_freq, dtype=jnp.float32) / n_freq)
    ang = jnp.concatenate([row[:, None] * inv_freq, col[:, None] * inv_freq], axis=-1)
    return jnp.cos(ang), jnp.sin(ang)


def apply_rope(x, cos, sin):
    xp = x.astype(jnp.float32).reshape(x.shape[:-1] + (x.shape[-1] // 2, 2))
    xe, xo = xp[..., 0], xp[..., 1]
    cs, sn = cos[None, :, None, :], sin[None, :, None, :]
    out = jnp.stack([xe * cs - xo * sn, xe * sn + xo * cs], axis=-1)
    return out.reshape(x.shape).astype(x.dtype)


def depthwise_conv(u, w, b):
    k = w.shape[0]
    y = lax.conv_general_dilated(u, w[:, None, :].astype(u.dtype), window_strides=(1,), padding=[(k // 2, k // 2)], dimension_numbers=('NWC', 'WIO', 'NWC'), feature_group_count=u.shape[-1])
    return y + b.astype(u.dtype)


def blocked_attention(q, k, v, scale):
    b, tq, kh, g, dk = q.shape
    nb = tq // Q_BLOCK
    qb = jnp.moveaxis(q.reshape(b, nb, Q_BLOCK, kh, g, dk), 1, 0)

    def one_block(qi):
        s = jnp.einsum('bqhgd,bkhd->bhgqk', qi, k, preferred_element_type=jnp.float32) * scale
        pr = jax.nn.softmax(s, axis=-1).astype(v.dtype)
        return jnp.einsum('bhgqk,bkhd->bqhgd', pr, v)

    o = lax.map(one_block, qb)
    return jnp.moveaxis(o, 0, 1).reshape(b, tq, kh, g, v.shape[-1])


def attn_project(h, w_in, g_cq, w_uq, g_ckv, w_ukv, g_q, g_k):
    b, t, _ = h.shape
    u = h @ w_in
    o1 = MLA_Q_LORA
    o2 = o1 + MLA_KV_LORA
    o3 = o2 + MLA_ROPE
    o4 = o3 + GQA_HEADS * GQA_HEAD_DIM
    o5 = o4 + GQA_KV_HEADS * GQA_HEAD_DIM
    c_q, c_kv, k_rope = u[..., :o1], u[..., o1:o2], u[..., o2:o3]
    q_g, k_g, v_g = u[..., o3:o4], u[..., o4:o5], u[..., o5:]
    q_m = (rms_norm(c_q, g_cq) @ w_uq).reshape(b, t, MLA_HEADS, MLA_NOPE + MLA_ROPE)
    kv_m = (rms_norm(c_kv, g_ckv) @ w_ukv).reshape(b, t, MLA_HEADS, MLA_NOPE + MLA_V)
    k_nope, v_m = kv_m[..., :MLA_NOPE], kv_m[..., MLA_NOPE:]
    k_rope = k_rope.reshape(b, t, 1, MLA_ROPE)
    q_g = rms_norm(q_g.reshape(b, t, GQA_HEADS, GQA_HEAD_DIM), g_q)
    k_g = rms_norm(k_g.reshape(b, t, GQA_KV_HEADS, GQA_HEAD_DIM), g_k)
    v_g = v_g.reshape(b, t, GQA_KV_HEADS, GQA_HEAD_DIM)
    return q_m, k_nope, k_rope, v_m, q_g, k_g, v_g


def mla_keys(k_nope, k_rope):
    return jnp.concatenate([k_nope, jnp.broadcast_to(k_rope, k_nope.shape[:-1] + (MLA_ROPE,))], axis=-1)


def attention_mixer(h_lat, h_ctx, w_in, g_cq, w_uq, g_ckv, w_ukv, g_q, g_k, w_out, ctx_out):
    s = h_lat.shape[1]
    q_m, k_nope, k_rope, v_m, q_g, k_g, v_g = attn_project(h_lat, w_in, g_cq, w_uq, g_ckv, w_ukv, g_q, g_k)
    cq_m, ck_nope, ck_rope, cv_m, cq_g, ck_g, cv_g = attn_project(h_ctx, w_in, g_cq, w_uq, g_ckv, w_ukv, g_q, g_k)
    cos_m, sin_m = axial_rope_tables(s, MLA_ROPE)
    cos_g, sin_g = axial_rope_tables(s, GQA_HEAD_DIM)
    q_m = jnp.concatenate([q_m[..., :MLA_NOPE], apply_rope(q_m[..., MLA_NOPE:], cos_m, sin_m)], axis=-1)
    k_rope = apply_rope(k_rope, cos_m, sin_m)
    q_g = apply_rope(q_g, cos_g, sin_g)
    k_g = apply_rope(k_g, cos_g, sin_g)
    ck_m = mla_keys(ck_nope, ck_rope)
    k_m_all = jnp.concatenate([ck_m, mla_keys(k_nope, k_rope)], axis=1)
    v_m_all = jnp.concatenate([cv_m, v_m], axis=1)
    k_g_all = jnp.concatenate([ck_g, k_g], axis=1)
    v_g_all = jnp.concatenate([cv_g, v_g], axis=1)

    def heads_out(qm, km, vm, qg, kg, vg):
        b, t = qm.shape[:2]
        o_m = blocked_attention(qm[:, :, :, None, :], km, vm, MLA_SCALE).reshape(b, t, MLA_HEADS * MLA_V)
        qg5 = qg.reshape(b, t, GQA_KV_HEADS, GQA_HEADS // GQA_KV_HEADS, GQA_HEAD_DIM)
        o_g = blocked_attention(qg5, kg, vg, GQA_SCALE).reshape(b, t, GQA_HEADS * GQA_HEAD_DIM)
        return jnp.concatenate([o_m, o_g], axis=-1) @ w_out

    out_lat = heads_out(q_m, k_m_all, v_m_all, q_g, k_g_all, v_g_all)
    out_ctx = heads_out(cq_m, ck_m, cv_m, cq_g, ck_g, cv_g) if ctx_out else None
    return out_lat, out_ctx


def ssd_chunked(x, dt, bm, cm, a, init_state, want_y):
    f32 = jnp.float32
    b, t, g, r, p = x.shape
    n = bm.shape[-1]
    nc = t // SSD_CHUNK
    xdt = (x.astype(f32) * dt[..., None]).reshape(b, nc, SSD_CHUNK, g, r, p)
    da = (dt * a).reshape(b, nc, SSD_CHUNK, g, r)
    bc = bm.astype(f32).reshape(b, nc, SSD_CHUNK, g, n)
    a_cs = jnp.cumsum(da, axis=2)
    a_end = a_cs[:, :, -1]
    chunk_states = jnp.einsum('bclgn,bclgr,bclgrp->bcgrpn', bc, jnp.exp(a_end[:, :, None] - a_cs), xdt)

    def carry_state(state, inp):
        decay, new = inp
        return state * decay[..., None, None] + new, state

    final, states_in = lax.scan(carry_state, init_state, (jnp.moveaxis(jnp.exp(a_end), 1, 0), jnp.moveaxis(chunk_states, 1, 0)))
    if not want_y:
        return None, final
    cc = cm.astype(f32).reshape(b, nc, SSD_CHUNK, g, n)
    y_off = jnp.einsum('bclgn,cbgrpn,bclgr->bclgrp', cc, states_in, jnp.exp(a_cs))
    a_t = jnp.moveaxis(a_cs, 2, -1)
    seg = a_t[..., :, None] - a_t[..., None, :]
    lower = jnp.tril(jnp.ones((SSD_CHUNK, SSD_CHUNK), dtype=bool))
    decay = jnp.exp(jnp.where(lower, seg, -jnp.inf))
    cb = jnp.einsum('bclgn,bcsgn->bcgls', cc, bc)
    y_diag = jnp.einsum('bcgrls,bcsgrp->bclgrp', cb[:, :, :, None] * decay, xdt)
    return (y_diag + y_off).reshape(b, t, g, r, p), final


def ssd_inputs(u_scan, conv_w, conv_b):
    b, t, _ = u_scan.shape
    gn = SSM_GROUPS * SSM_STATE
    xbc = jax.nn.silu(depthwise_conv(u_scan[..., :SSM_XBC], conv_w, conv_b))
    xs = xbc[..., :SSM_D].reshape(b, t, SSM_GROUPS, SSM_HPG, SSM_HEAD_DIM)
    bm = xbc[..., SSM_D:SSM_D + gn].reshape(b, t, SSM_GROUPS, SSM_STATE)
    cm = xbc[..., SSM_D + gn:].reshape(b, t, SSM_GROUPS, SSM_STATE)
    dt_raw = u_scan[..., SSM_XBC:].astype(jnp.float32).reshape(b, t, SSM_GROUPS, SSM_HPG)
    return xs, bm, cm, dt_raw


def bidir_ssd(inputs, a_log, dt_bias, d_skip, inits, want_y):
    xs, bm, cm, dt_raw = inputs
    ys = []
    finals = []
    for d in range(2):
        dt = jax.nn.softplus(dt_raw + dt_bias[d].astype(jnp.float32).reshape(SSM_GROUPS, SSM_HPG))
        a = -jnp.exp(a_log[d].astype(jnp.float32)).reshape(SSM_GROUPS, SSM_HPG)
        seq = (xs, dt, bm, cm)
        if d == 1:
            seq = tuple(jnp.flip(z, axis=1) for z in seq)
        y, fin = ssd_chunked(seq[0], seq[1], seq[2], seq[3], a, inits[d], want_y)
        finals.append(fin)
        if want_y:
            ys.append(jnp.flip(y, axis=1) if d == 1 else y)
    if not want_y:
        return None, finals
    skip = (d_skip[0] + d_skip[1]).astype(jnp.float32).reshape(SSM_GROUPS, SSM_HPG, 1)
    return ys[0] + ys[1] + skip * xs.astype(jnp.float32), finals


def conformer_conv(u, dw_w, dw_b, ln_g, ln_b):
    v = u[..., :CONF_D] * jax.nn.sigmoid(u[..., CONF_D:])
    v = depthwise_conv(v, dw_w, dw_b)
    return jax.nn.silu(layer_norm(v, ln_g, ln_b))


def ssm_conv_mixer(h_lat, h_ctx, w_in, conv_w, conv_b, a_log, dt_bias, d_skip, norm_g, dw_w, dw_b, ln_g, ln_b, w_out, ctx_out):
    lo, hi = SSM_D, SSM_D + SSM_XBC + SSM_HEADS
    b = h_lat.shape[0]
    u_lat = h_lat @ w_in
    if ctx_out:
        u_ctx = h_ctx @ w_in
        ctx_scan = u_ctx[..., lo:hi]
    else:
        u_ctx = None
        ctx_scan = h_ctx @ w_in[:, lo:hi]
    zero = jnp.zeros((b, SSM_GROUPS, SSM_HPG, SSM_HEAD_DIM, SSM_STATE), jnp.float32)
    y_ctx, ctx_states = bidir_ssd(ssd_inputs(ctx_scan, conv_w, conv_b), a_log, dt_bias, d_skip, (zero, zero), ctx_out)
    y_lat, _ = bidir_ssd(ssd_inputs(u_lat[..., lo:hi], conv_w, conv_b), a_log, dt_bias, d_skip, ctx_states, True)

    def merge(u, y):
        bb, t, _ = u.shape
        gated = y.reshape(bb, t, SSM_D) * jax.nn.silu(u[..., :SSM_D].astype(jnp.float32))
        y_ssm = rms_norm(gated, norm_g).astype(u.dtype)
        y_conv = conformer_conv(u[..., hi:], dw_w, dw_b, ln_g, ln_b)
        return jnp.concatenate([y_ssm, y_conv], axis=-1) @ w_out

    out_ctx = merge(u_ctx, y_ctx) if ctx_out else None
    return merge(u_lat, y_lat), out_ctx


def moe_ffn(h, w_router, b_router, w_gate_up, b_gate_up, w_down, b_down):
    n_tok, d = h.shape
    n_assign = n_tok * MOE_TOP_K
    logits = h.astype(jnp.float32) @ w_router.astype(jnp.float32) + b_router.astype(jnp.float32)
    top_logit, top_e = lax.top_k(logits, MOE_TOP_K)
    top_w = jax.nn.softmax(top_logit, axis=-1).astype(h.dtype)
    flat_e = top_e.reshape(-1)
    flat_tok = jnp.arange(n_assign, dtype=jnp.int32) // MOE_TOP_K
    order = jnp.argsort(flat_e)
    e_sorted = flat_e[order]
    counts = jnp.zeros((N_EXPERTS,), jnp.int32).at[flat_e].add(1)
    padded = (counts + MOE_BLOCK - 1) // MOE_BLOCK * MOE_BLOCK
    start = jnp.cumsum(counts) - counts
    pend = jnp.cumsum(padded)
    pstart = pend - padded
    dest = pstart[e_sorted] + jnp.arange(n_assign, dtype=jnp.int32) - start[e_sorted]
    n_blocks = -(-n_assign // MOE_BLOCK) + N_EXPERTS
    n_rows = n_blocks * MOE_BLOCK
    row_tok = jnp.zeros((n_rows,), jnp.int32).at[dest].set(flat_tok[order])
    row_w = jnp.zeros((n_rows,), h.dtype).at[dest].set(top_w.reshape(-1)[order])
    block_start = jnp.arange(n_blocks, dtype=jnp.int32) * MOE_BLOCK
    block_expert = jnp.minimum(jnp.searchsorted(pend, block_start, side='right'), N_EXPERTS - 1)
    xs = h[row_tok].reshape(n_blocks, MOE_BLOCK, d)

    def expert_block(args):
        xb, e = args
        gu = xb @ w_gate_up[e] + b_gate_up[e]
        gate = jnp.minimum(gu[:, :MOE_FF], SWIGLU_LIMIT)
        up = jnp.clip(gu[:, MOE_FF:], -SWIGLU_LIMIT, SWIGLU_LIMIT)
        act = (up + 1.0) * (gate * jax.nn.sigmoid(SWIGLU_ALPHA * gate))
        return act @ w_down[e] + b_down[e]

    ys = lax.map(expert_block, (xs, block_expert)).reshape(n_rows, d) * row_w[:, None]
    return jnp.zeros_like(h).at[row_tok].add(ys)


def setup_inputs(seed: int = 0) -> dict:
    key = jax.random.key(seed)
    ks = iter(list(jax.random.split(key, 64)))
    f32 = jnp.float32

    def nrm(shape, scale):
        return jax.random.normal(next(ks), shape, f32) * scale

    def gain(shape):
        return 1.0 + 0.05 * jax.random.normal(next(ks), shape, f32)

    D = D_MODEL
    NA, NS = N_ATTN_LAYERS, N_SSM_LAYERS
    a_init = jax.random.uniform(next(ks), (NS, 2, SSM_HEADS), f32, 1.0, 16.0)
    dt_init = jnp.exp(jax.random.uniform(next(ks), (NS, 2, SSM_HEADS), f32, np.log(1e-3), np.log(1e-1)))
    return {
        'x': nrm((BATCH, SEQ, D), 1.0),
        'c': nrm((BATCH, D), 1.0),
        'ctx': nrm((BATCH, CTX_LEN, D), 1.0),
        'c_ctx': nrm((D,), 1.0),
        'w_mod': nrm((DEPTH, D, 6 * D), 0.5 * D ** -0.5),
        'b_mod': nrm((DEPTH, 6 * D), 0.02),
        'norm_g': gain((DEPTH, 2, D)),
        'attn_w_in': nrm((NA, D, ATTN_IN), D ** -0.5),
        'mla_g_cq': gain((NA, MLA_Q_LORA)),
        'mla_w_uq': nrm((NA, MLA_Q_LORA, MLA_HEADS * (MLA_NOPE + MLA_ROPE)), MLA_Q_LORA ** -0.5),
        'mla_g_ckv': gain((NA, MLA_KV_LORA)),
        'mla_w_ukv': nrm((NA, MLA_KV_LORA, MLA_HEADS * (MLA_NOPE + MLA_V)), MLA_KV_LORA ** -0.5),
        'gqa_g_q': gain((NA, GQA_HEAD_DIM)),
        'gqa_g_k': gain((NA, GQA_HEAD_DIM)),
        'attn_w_out': nrm((NA, ATTN_OUT, D), ATTN_OUT ** -0.5),
        'ssm_w_in': nrm((NS, D, SSM_IN), D ** -0.5),
        'ssm_conv_w': nrm((NS, SSM_CONV, SSM_XBC), SSM_CONV ** -0.5),
        'ssm_conv_b': nrm((NS, SSM_XBC), 0.02),
        'ssm_a_log': jnp.log(a_init),
        'ssm_dt_bias': dt_init + jnp.log(-jnp.expm1(-dt_init)),
        'ssm_d': gain((NS, 2, SSM_HEADS)),
        'ssm_norm_g': gain((NS, SSM_D)),
        'conf_dw_w': nrm((NS, CONF_K, CONF_D), CONF_K ** -0.5),
        'conf_dw_b': nrm((NS, CONF_D), 0.02),
        'conf_ln_g': gain((NS, CONF_D)),
        'conf_ln_b': nrm((NS, CONF_D), 0.02),
        'ssm_w_out': nrm((NS, SSM_OUT, D), SSM_OUT ** -0.5),
        'moe_w_router': nrm((DEPTH, D, N_EXPERTS), D ** -0.5),
        'moe_b_router': nrm((DEPTH, N_EXPERTS), 0.01),
        'moe_w_gate_up': nrm((DEPTH, N_EXPERTS, D, 2 * MOE_FF), D ** -0.5),
        'moe_b_gate_up': nrm((DEPTH, N_EXPERTS, 2 * MOE_FF), 0.02),
        'moe_w_down': nrm((DEPTH, N_EXPERTS, MOE_FF, D), MOE_FF ** -0.5),
        'moe_b_down': nrm((DEPTH, N_EXPERTS, D), 0.02),
        'final_g': gain((D,)),
    }


def reference(x, c, ctx, c_ctx, w_mod, b_mod, norm_g, attn_w_in, mla_g_cq, mla_w_uq, mla_g_ckv, mla_w_ukv, gqa_g_q, gqa_g_k, attn_w_out, ssm_w_in, ssm_conv_w, ssm_conv_b, ssm_a_log, ssm_dt_bias, ssm_d, ssm_norm_g, conf_dw_w, conf_dw_b, conf_ln_g, conf_ln_b, ssm_w_out, moe_w_router, moe_b_router, moe_w_gate_up, moe_b_gate_up, moe_w_down, moe_b_down, final_g):
    b, s, d = x.shape
    silu_c = jax.nn.silu(c)
    silu_cc = jax.nn.silu(c_ctx)
    for i in range(DEPTH):
        ctx_out = i < DEPTH - 1
        j = i // 2
        mod = (silu_c @ w_mod[i] + b_mod[i])[:, None, :]
        sh1, sc1, g1, sh2, sc2, g2 = jnp.split(mod, 6, axis=-1)
        n_cm = 6 if ctx_out else 2
        mod_c = silu_cc @ w_mod[i][:, :n_cm * d] + b_mod[i][:n_cm * d]
        cmods = jnp.split(mod_c, n_cm, axis=-1)
        h_lat = modulate(rms_norm(x, norm_g[i, 0]), sh1, sc1)
        h_ctx = modulate(rms_norm(ctx, norm_g[i, 0]), cmods[0], cmods[1])
        if i % 2 == 0:
            o_lat, o_ctx = attention_mixer(h_lat, h_ctx, attn_w_in[j], mla_g_cq[j], mla_w_uq[j], mla_g_ckv[j], mla_w_ukv[j], gqa_g_q[j], gqa_g_k[j], attn_w_out[j], ctx_out)
        else:
            o_lat, o_ctx = ssm_conv_mixer(h_lat, h_ctx, ssm_w_in[j], ssm_conv_w[j], ssm_conv_b[j], ssm_a_log[j], ssm_dt_bias[j], ssm_d[j], ssm_norm_g[j], conf_dw_w[j], conf_dw_b[j], conf_ln_g[j], conf_ln_b[j], ssm_w_out[j], ctx_out)
        x = x + g1 * o_lat
        h_lat = modulate(rms_norm(x, norm_g[i, 1]), sh2, sc2)
        if ctx_out:
            ctx = ctx + cmods[2] * o_ctx
            h_ctx = modulate(rms_norm(ctx, norm_g[i, 1]), cmods[3], cmods[4])
            tokens = jnp.concatenate([h_lat.reshape(-1, d), h_ctx.reshape(-1, d)], axis=0)
            f = moe_ffn(tokens, moe_w_router[i], moe_b_router[i], moe_w_gate_up[i], moe_b_gate_up[i], moe_w_down[i], moe_b_down[i])
            x = x + g2 * f[:b * s].reshape(b, s, d)
            ctx = ctx + cmods[5] * f[b * s:].reshape(ctx.shape)
        else:
            f = moe_ffn(h_lat.reshape(-1, d), moe_w_router[i], moe_b_router[i], moe_w_gate_up[i], moe_b_gate_up[i], moe_w_down[i], moe_b_down[i])
            x = x + g2 * f.reshape(b, s, d)
    return rms_norm(x, final_g)
```

```python
import numpy as np
from contextlib import ExitStack
import concourse.bass as bass
import concourse.mybir as mybir
from concourse.bass_utils import run_bass_kernel_spmd

F32 = mybir.dt.float32
BF16 = mybir.dt.bfloat16
U32 = mybir.dt.uint32
AF = mybir.ActivationFunctionType
ALU = mybir.AluOpType

D = 1024
SEQ = 4096
HALF = 2048
CTX = 256
NQ = SEQ + CTX
NK = CTX + SEQ
EPS = 1e-6
ATTN_IN = 1184
N_EXP = 32
SSM_IN = 4624


class Buf:
    __slots__ = ("w", "r", "owner")

    def __init__(self):
        self.w = None
        self.r = []
        self.owner = None


class Sched:
    ENGS = ("pe", "dve", "act", "pool", "sp")
    NDSEM = 28
    NHW = 16

    def __init__(self, nc, same_engine_sync=True):
        self.nc = nc
        self.ops = []
        self.same_engine_sync = same_engine_sync

    def op(self, eng, fn, reads=(), writes=(), dma=False):
        j = len(self.ops)
        deps = set()
        for b in list(reads) + list(writes):
            if b.owner is not self:
                b.owner = self
                b.w = None
                b.r = []
        for b in reads:
            if b.w is not None:
                deps.add(b.w)
        for b in writes:
            if b.w is not None:
                deps.add(b.w)
            deps.update(b.r)
        for b in reads:
            b.r.append(j)
        for b in writes:
            b.w = j
            b.r = []
        deps.discard(j)
        self.ops.append([eng, fn, deps, dma])
        return j

    def mm(self, out, lhsT, rhs, start, stop, r, w):
        self.op("pe", lambda e: e.matmul(out, lhsT, rhs, start=start, stop=stop), r, w)

    def tr(self, out, in_, ident, r, w):
        self.op("pe", lambda e: e.transpose(out, in_, ident), r, w)

    def act(self, out, in_, func, r, w, bias=None, scale=None):
        kw = {}
        if bias is not None:
            kw["bias"] = bias
        if scale is not None:
            kw["scale"] = scale
        self.op("act", lambda e: e.activation(out=out, in_=in_, func=func, **kw), r, w)

    def ts(self, eng, out, in0, s1, s2, op0, op1, r, w):
        if op1 is None:
            self.op(eng, lambda e: e.tensor_scalar(out, in0, s1, None, op0=op0), r, w)
        else:
            self.op(eng, lambda e: e.tensor_scalar(out, in0, s1, s2, op0=op0, op1=op1), r, w)

    def tt(self, eng, out, in0, in1, op, r, w):
        self.op(eng, lambda e: e.tensor_tensor(out, in0, in1, op), r, w)

    def stt(self, out, in0, scalar, in1, op0, op1, r, w):
        self.op("dve", lambda e: e.scalar_tensor_tensor(out, in0, scalar, in1, op0=op0, op1=op1), r, w)

    def cp(self, eng, out, in_, r, w):
        if eng == "act":
            self.op("act", lambda e: e.copy(out, in_), r, w)
        else:
            self.op(eng, lambda e: e.tensor_copy(out, in_), r, w)

    def memset(self, eng, ap, val, w):
        self.op(eng, lambda e: e.memset(ap, val), (), w)

    def dma(self, eng, out, in_, r, w, **kw):
        self.op(eng, lambda e: e.dma_start(out=out, in_=in_, **kw), r, w, dma=True)

    def emit(self):
        nc = self.nc
        G = GSYNC[id(nc)]
        ops = self.ops
        n = len(ops)
        if n == 0:
            return
        needs = [False] * n
        for j, (eng, fn, deps, dma) in enumerate(ops):
            for d in deps:
                de, _, _, ddma = ops[d]
                if ddma:
                    continue
                if de != eng or dma or (self.same_engine_sync and eng != "pe"):
                    needs[d] = True
        last_of = {}
        for j, o in enumerate(ops):
            if not o[3]:
                last_of[o[0]] = j
        for e_, j in last_of.items():
            needs[j] = True
        cnt0 = dict(G["cnt"])
        dlast0 = list(G["dlast"])
        cnt = G["cnt"]
        dlast = G["dlast"]
        val = [0] * n
        prev_same = {}
        for j, (eng, fn, deps, dma) in enumerate(ops):
            if dma:
                if eng == "pool":
                    s = self.NHW + G["dks"] % (self.NDSEM - self.NHW)
                    G["dks"] += 1
                else:
                    s = G["dk"] % self.NHW
                    G["dk"] += 1
                prev_same[j] = dlast[s]
                dlast[s] += 16
                val[j] = (s, dlast[s])
            elif needs[j]:
                cnt[eng] += 1
                val[j] = cnt[eng]
        streams = {e: [] for e in self.ENGS}
        for j, o in enumerate(ops):
            streams[o[0]].append(j)
        sss = self.same_engine_sync
        csem, dsem = G["csem"], G["dsem"]
        with ExitStack() as es:
            block = es.enter_context(nc.Block())

            def run_stream(ename, e):
                known_c = dict(cnt0)
                known_d = list(dlast0)
                for j in streams[ename]:
                    eng, fn, deps, dma = ops[j]
                    needc = {}
                    needd = {}
                    for d in deps:
                        de, _, _, ddma = ops[d]
                        if ddma:
                            s, v = val[d]
                            if v > needd.get(s, 0):
                                needd[s] = v
                        else:
                            if de == eng and not dma and (eng == "pe" or not sss):
                                continue
                            v = val[d]
                            if v > needc.get(de, 0):
                                needc[de] = v
                    if dma:
                        s = val[j][0]
                        v = prev_same[j]
                        if v > needd.get(s, 0):
                            needd[s] = v
                    for de, v in needc.items():
                        if known_c[de] < v:
                            e.wait_ge(csem[de], v)
                            known_c[de] = v
                    for s, v in needd.items():
                        if known_d[s] < v:
                            e.wait_ge(dsem[s], v)
                            known_d[s] = v
                    ins = fn(e)
                    if dma:
                        ins.then_inc(dsem[val[j][0]], 16)
                    elif needs[j]:
                        ins.then_inc(csem[eng], 1)
                for x in self.ENGS:
                    if cnt[x] > known_c[x]:
                        e.wait_ge(csem[x], cnt[x])
                for s in range(self.NDSEM):
                    if dlast[s] > known_d[s]:
                        e.wait_ge(dsem[s], dlast[s])

            @block.tensor
            def _(e):
                run_stream("pe", e)

            @block.vector
            def _(e):
                run_stream("dve", e)

            @block.scalar
            def _(e):
                run_stream("act", e)

            @block.gpsimd
            def _(e):
                run_stream("pool", e)

            @block.sync
            def _(e):
                run_stream("sp", e)


GSYNC = {}


def init_sync(nc, es):
    GSYNC.clear()
    GSYNC[id(nc)] = dict(
        csem={e: es.enter_context(nc.semaphore("c_" + e)) for e in Sched.ENGS},
        dsem=[es.enter_context(nc.semaphore("d_%d" % i)) for i in range(Sched.NDSEM)],
        cnt={e: 0 for e in Sched.ENGS}, dlast=[0] * Sched.NDSEM, dk=0, dks=0)


_uid = [0]


class Tl:
    __slots__ = ("t", "b")

    def __init__(self, t):
        self.t = t
        self.b = Buf()


class Ctx:
    def __init__(self, nc, es):
        self.nc = nc
        self.es = es
        self.n = 0

    def sb(self, shape, dt, name=None):
        _uid[0] += 1
        return Tl(self.es.enter_context(self.nc.sbuf_tensor("%s_%d" % (name or "s", _uid[0]), list(shape), dt)))

    def ps(self, name=None, shape=(128, 512), dt=F32):
        _uid[0] += 1
        return Tl(self.es.enter_context(self.nc.psum_tensor("%s_%d" % (name or "p", _uid[0]), list(shape), dt)))


def dram(nc, name, shape, dt, kind):
    return nc.dram_tensor(name, list(shape), dt, kind=kind).ap()


def emit_rstd(S, ssps, rstd, n_feat, T):
    S.ts("dve", rstd.t[:, :T], ssps.t[:, :T], 1.0 / n_feat, EPS, ALU.mult, ALU.add, [ssps.b], [rstd.b])
    S.act(rstd.t[:, :T], rstd.t[:, :T], AF.Ln, [rstd.b], [rstd.b])
    S.act(rstd.t[:, :T], rstd.t[:, :T], AF.Exp, [rstd.b], [rstd.b], scale=-0.5)


def emit_norm_mod(S, K, xg, T, A, SH, hb, hf=None):
    sq, ssps, rstd, tmp, ones = K["sq"], K["ssps"], K["rstd"], K["tmp"], K["ones"]
    for kc in range(8):
        s = sq[kc % 2]
        S.act(s.t[:, :T], xg.t[:, kc, :T], AF.Square, [xg.b], [s.b])
        S.mm(ssps.t[:, :T], ones[:, :], s.t[:, :T], kc == 0, kc == 7, [s.b], [ssps.b])
    emit_rstd(S, ssps, rstd, 1024.0, T)
    for kc in range(8):
        t = tmp[kc % 2]
        S.stt(t.t[:, :T], xg.t[:, kc, :T], A[:, kc:kc + 1], rstd.t[:, :T], ALU.mult, ALU.mult,
              [xg.b, rstd.b], [t.b])
        if hf is not None:
            S.act(hf.t[:, kc, :T], t.t[:, :T], AF.Identity, [t.b], [hf.b], bias=SH[:, kc:kc + 1])
            S.cp("pool", hb.t[:, kc, :T], hf.t[:, kc, :T], [hf.b], [hb.b])
        else:
            S.act(hb.t[:, kc, :T], t.t[:, :T], AF.Identity, [t.b], [hb.b], bias=SH[:, kc:kc + 1])


def norm_scratch(C, ones_f32):
    return dict(sq=[C.sb([128, 512], F32, "sq") for _ in range(2)], ssps=C.ps("ssps"),
                rstd=C.sb([128, 512], F32, "rstd"), tmp=[C.sb([128, 512], F32, "tmp") for _ in range(2)],
                ones=ones_f32)


def phase_mod(nc, I, li, mvec):
    with ExitStack() as es:
        C = Ctx(nc, es)
        S = Sched(nc)
        cc = C.sb([128, 8, 2], F32, "cc")
        sc = C.sb([128, 8, 2], F32, "sc")
        bm = C.sb([128, 48], F32, "bm")
        ng = C.sb([128, 2, 8], F32, "ng")
        mv = C.sb([128, 48, 2], F32, "mv")
        wm = [C.sb([128, 8, 1024], F32, "wm") for _ in range(2)]
        ps = C.ps("modps")
        S.dma("sp", cc.t[:], I["cc"][:, :, :], [], [cc.b])
        S.dma("sp", bm.t[:], I["bmod_r"][li], [], [bm.b])
        S.dma("sp", ng.t[:], I["ng_r"][li], [], [ng.b])
        S.act(sc.t[:], cc.t[:], AF.Silu, [cc.b], [sc.b])
        for m6 in range(6):
            w = wm[m6 % 2]
            S.dma("sp" if m6 % 2 == 0 else "act", w.t[:], I["wmod_r"][li, m6], [], [w.b])
            for mm_ in range(8):
                m = m6 * 8 + mm_
                for kc in range(8):
                    S.mm(ps.t[:, 2 * m:2 * m + 2], w.t[:, kc, mm_ * 128:(mm_ + 1) * 128], sc.t[:, kc, :],
                         kc == 0, kc == 7, [w.b, sc.b], [ps.b])
        ps3 = ps.t[:, 0:96].rearrange("p (m j) -> p m j", j=2)
        for j in range(2):
            S.tt("dve", mv.t[:, :, j], ps3[:, :, j], bm.t[:, :], ALU.add, [ps.b, bm.b], [mv.b])
        for j in range(2):
            def seg(k):
                return mv.t[:, k * 8:(k + 1) * 8, j]
            S.stt(mvec.t[:, j, 0, :], seg(1), 1.0, ng.t[:, 0, :], ALU.add, ALU.mult, [mv.b, ng.b], [mvec.b])
            S.cp("dve", mvec.t[:, j, 1, :], seg(0), [mv.b], [mvec.b])
            S.cp("dve", mvec.t[:, j, 2, :], seg(2), [mv.b], [mvec.b])
            S.stt(mvec.t[:, j, 3, :], seg(4), 1.0, ng.t[:, 1, :], ALU.add, ALU.mult, [mv.b, ng.b], [mvec.b])
            S.cp("dve", mvec.t[:, j, 4, :], seg(3), [mv.b], [mvec.b])
            S.cp("dve", mvec.t[:, j, 5, :], seg(5), [mv.b], [mvec.b])
        S.emit()


def load_consts(nc, es, I):
    C = Ctx(nc, es)
    cf = C.sb([128, 5, 128], F32, "constf")
    cb = C.sb([128, 2, 128], BF16, "constb")
    with ExitStack() as es2:
        S = Sched(nc)
        S.dma("sp", cf.t[:], I["consts"][:, :, :], [], [cf.b])
        S.dma("pool", cb.t[:, 0, :], I["consts"][:, 0, :], [], [cb.b])
        S.dma("pool", cb.t[:, 1, :], I["consts"][:, 1, :], [], [cb.b])
        S.emit()
    return dict(ident_f=cf.t[:, 0, :], ones_f=cf.t[:, 1, :], blk_f=cf.t[:, 2, :], pm128=cf.t[:, 3, :],
                pm96=cf.t[:, 4, :], ident_b=cb.t[:, 0, :], ones_b=cb.t[:, 1, :])


def attn_groups():
    g = [("ctx", CTX, "ctxT", 0, 0, SEQ)]
    for i in range(8):
        g.append(("own", 512, "xT", i * 512, CTX + i * 512, i * 512))
    return g


def phase_attn_proj(nc, I, K0, mvec, P):
    with ExitStack() as es:
        C = Ctx(nc, es)
        S = Sched(nc)
        win = C.sb([128, 8, ATTN_IN], BF16, "win")
        wkd = C.sb([128, 8, 2, 128], BF16, "wkd")
        gv = C.sb([128, 6], F32, "gv")
        xg = [C.sb([128, 8, 512], F32, "xg") for _ in range(1)]
        hb = [C.sb([128, 8, 512], BF16, "hb") for _ in range(2)]
        rp = [C.sb([128, 4, 512], F32, "rope") for _ in range(1)]
        cqf = C.sb([128, 2, 512], F32, "cqf")
        sq2 = [C.sb([128, 512], F32, "sq2") for _ in range(2)]
        rs2 = [C.sb([128, 512], F32, "rs2") for _ in range(2)]
        qn = [C.sb([128, 512], F32, "qn") for _ in range(2)]
        t1 = [C.sb([128, 512], F32, "t1") for _ in range(2)]
        t2 = [C.sb([128, 512], F32, "t2") for _ in range(2)]
        NS = norm_scratch(C, K0["ones_f"])
        pp = [C.ps("proj") for _ in range(2)]
        aux = C.ps("aux")
        pq = C.ps("pq")
        pv = C.ps("pv")
        for kc in range(8):
            S.dma("pool", win.t[:, kc, :], I["win_r"][:, kc, :], [], [win.b])
        S.dma("sp", gv.t[:], I["attn_g"][:, :], [], [gv.b])
        for kv in range(2):
            for hh in range(2):
                S.cp("pool", wkd.t[:, :, kv, hh * 64:(hh + 1) * 64], win.t[:, :, 928 + kv * 64:928 + (kv + 1) * 64],
                     [win.b], [wkd.b])
        ckvn, krope, cqn, qg, kgd, vg = P["ckvn"], P["krope"], P["cqn"], P["qg"], P["kgd"], P["vg"]
        S.memset("pool", vg.t[:, :, :, 64:128], 1.0, [vg.b])
        ppi = [0]

        def proj(lhs_fn, M, h, T):
            p = pp[ppi[0] % 2]
            ppi[0] += 1
            for kc in range(8):
                S.mm(p.t[:M, :T], lhs_fn(kc), h.t[:, kc, :T], kc == 0, kc == 7, [win.b, wkd.b, h.b], [p.b])
            return p

        cnt = [0]

        def headnorm_rope(p, T, gcol, lat, rpt, out_ap, out_b):
            i = cnt[0] % 2
            cnt[0] += 1
            S.act(sq2[i].t[:, :T], p.t[:, :T], AF.Square, [p.b], [sq2[i].b])
            S.mm(aux.t[:, :T], K0["blk_f"], sq2[i].t[:, :T], True, True, [sq2[i].b], [aux.b])
            emit_rstd(S, aux, rs2[i], 64.0, T)
            S.stt(qn[i].t[:, :T], p.t[:, :T], gv.t[:, gcol:gcol + 1], rs2[i].t[:, :T], ALU.mult, ALU.mult,
                  [p.b, rs2[i].b, gv.b], [qn[i].b])
            if lat:
                S.mm(pq.t[:, :T], K0["pm128"], qn[i].t[:, :T], True, True, [qn[i].b], [pq.b])
                S.tt("pool", t1[i].t[:, :T], qn[i].t[:, :T], rpt.t[:, 0, :T], ALU.mult, [qn[i].b, rpt.b], [t1[i].b])
                S.tt("dve", t2[i].t[:, :T], pq.t[:, :T], rpt.t[:, 1, :T], ALU.mult, [pq.b, rpt.b], [t2[i].b])
                S.tt("dve", out_ap, t1[i].t[:, :T], t2[i].t[:, :T], ALU.add, [t1[i].b, t2[i].b], [out_b])
            else:
                S.cp("dve", out_ap, qn[i].t[:, :T], [qn[i].b], [out_b])

        for gi, (kind, T, src, c0, kpos, qpos) in enumerate(attn_groups()):
            x = xg[0]
            h = hb[gi % 2]
            rpt = rp[0]
            lat = kind != "ctx"
            j = 0 if lat else 1
            S.dma("sp", x.t[:, :, :T], I[src].rearrange("(kc p) t -> p kc t", p=128)[:, :, c0:c0 + T], [], [x.b])
            if lat:
                S.dma("act", rpt.t[:, 0:2, :T], I["ropeg"][:, :, c0:c0 + T], [], [rpt.b])
                S.dma("act", rpt.t[:, 2:4, :T], I["ropem"][:, :, c0:c0 + T], [], [rpt.b])
            emit_norm_mod(S, NS, x, T, mvec.t[:, j, 0, :], mvec.t[:, j, 1, :], h)
            isq = qpos is not None
            if isq:
                for c in range(2):
                    p = proj(lambda kc, c=c: win.t[:, kc, c * 128:(c + 1) * 128], 128, h, T)
                    S.cp("act", cqf.t[:, c, :T], p.t[:, :T], [p.b], [cqf.b])
                    S.act(sq2[c].t[:, :T], p.t[:, :T], AF.Square, [p.b], [sq2[c].b])
                    S.mm(aux.t[:, :T], K0["ones_f"], sq2[c].t[:, :T], c == 0, c == 1, [sq2[c].b], [aux.b])
                emit_rstd(S, aux, rs2[0], 256.0, T)
                for c in range(2):
                    S.stt(cqn.t[:, c, qpos:qpos + T], cqf.t[:, c, :T], gv.t[:, c:c + 1], rs2[0].t[:, :T],
                          ALU.mult, ALU.mult, [cqf.b, rs2[0].b, gv.b], [cqn.b])
            p = proj(lambda kc: win.t[:, kc, 256:384], 128, h, T)
            S.act(sq2[0].t[:, :T], p.t[:, :T], AF.Square, [p.b], [sq2[0].b])
            S.mm(aux.t[:, :T], K0["ones_f"], sq2[0].t[:, :T], True, True, [sq2[0].b], [aux.b])
            emit_rstd(S, aux, rs2[1], 128.0, T)
            S.stt(ckvn.t[:, kpos:kpos + T], p.t[:, :T], gv.t[:, 2:3], rs2[1].t[:, :T], ALU.mult, ALU.mult,
                  [p.b, rs2[1].b, gv.b], [ckvn.b])
            p = proj(lambda kc: win.t[:, kc, 320:416], 96, h, T)
            if lat:
                S.cp("act", qn[0].t[:96, :T], p.t[:96, :T], [p.b], [qn[0].b])
                S.mm(pq.t[:96, :T], K0["pm96"][:96, :96], qn[0].t[:96, :T], True, True, [qn[0].b], [pq.b])
                S.tt("pool", t1[0].t[64:96, :T], qn[0].t[64:96, :T], rpt.t[64:96, 2, :T], ALU.mult,
                     [qn[0].b, rpt.b], [t1[0].b])
                S.tt("dve", t2[0].t[64:96, :T], pq.t[64:96, :T], rpt.t[64:96, 3, :T], ALU.mult,
                     [pq.b, rpt.b], [t2[0].b])
                S.tt("dve", krope.t[64:96, kpos:kpos + T], t1[0].t[64:96, :T], t2[0].t[64:96, :T], ALU.add,
                     [t1[0].b, t2[0].b], [krope.b])
            else:
                S.cp("act", krope.t[64:96, kpos:kpos + T], p.t[64:96, :T], [p.b], [krope.b])
            if isq:
                for c in range(4):
                    p = proj(lambda kc, c=c: win.t[:, kc, 416 + c * 128:416 + (c + 1) * 128], 128, h, T)
                    headnorm_rope(p, T, 3, lat, rpt, qg.t[:, c, qpos:qpos + T], qg.b)
            for kv in range(2):
                p = proj(lambda kc, kv=kv: wkd.t[:, kc, kv, :], 128, h, T)
                headnorm_rope(p, T, 4, lat, rpt, kgd.t[:, kv, kpos:kpos + T], kgd.b)
            for tt_ in range(T // 128):
                for kc in range(8):
                    S.mm(pv.t[:, 0:128], h.t[:, kc, tt_ * 128:(tt_ + 1) * 128], win.t[:, kc, 1056:1184],
                         kc == 0, kc == 7, [h.b, win.b], [pv.b])
                kt = kpos // 128 + tt_
                S.cp("act", vg.t[:, kt, :, 0:64], pv.t[:, 0:128].rearrange("p (a b) -> p a b", a=2), [pv.b], [vg.b])
        S.emit()


def phase_attn_core(nc, I, K0, P, attn_o):
    with ExitStack() as es:
        C = Ctx(nc, es)
        S = Sched(nc)
        wuq = C.sb([128, 2, 768], BF16, "wuq")
        wukv = C.sb([128, 1024], BF16, "wukv")
        KT = [C.sb([96, NK], BF16, "KT") for _ in range(2)]
        VH = [C.sb([128, 34, 128], BF16, "VH") for _ in range(2)]
        QT = [C.sb([96, NQ], BF16, "QT") for _ in range(2)]
        rp = [C.sb([96, 2, 512], F32, "ropq") for _ in range(2)]
        qf = [C.sb([96, 512], F32, "qf") for _ in range(2)]
        t1 = [C.sb([96, 512], F32, "t1") for _ in range(2)]
        t2 = [C.sb([96, 512], F32, "t2") for _ in range(2)]
        pT = [C.sb([128, 1024], BF16, "pT") for _ in range(3)]
        rs = [C.sb([64, 512], F32, "rs") for _ in range(2)]
        ot = [C.sb([64, 512], BF16, "ot") for _ in range(2)]
        sps = [C.ps("sps", (128, 1024)) for _ in range(3)]
        accO = [C.ps("accO") for _ in range(2)]
        gen = accO[0]
        pq = accO[1]
        ckvn, krope, cqn, qg, kgd, vg = P["ckvn"], P["krope"], P["cqn"], P["qg"], P["kgd"], P["vg"]
        for c in range(2):
            S.dma("pool", wuq.t[:, c, :], I["wuq_r"][:, c, :], [], [wuq.b])
        S.dma("pool", wukv.t[:, :], I["wukv"][:, :], [], [wukv.b])
        for v_ in VH:
            S.memset("pool", v_.t[:, :, 64:128], 1.0, [v_.b])
        kgroups = [(0, CTX)] + [(CTX + i * 512, 512) for i in range(8)]
        qgroups = [(i * 512, 512, 0, 34, True) for i in range(8)] + [(SEQ, CTX, 0, 2, False)]
        unit = [0]
        for hd in range(16):
            if hd < 8:
                h = hd
                kt_, vh_, qt_ = KT[h % 2], VH[h % 2], QT[h % 2]
                for (k0, T) in kgroups:
                    S.mm(gen.t[:64, :T], wukv.t[:, h * 128:h * 128 + 64], ckvn.t[:, k0:k0 + T], True, True,
                         [wukv.b, ckvn.b], [gen.b])
                    S.cp("act", kt_.t[0:64, k0:k0 + T], gen.t[:64, :T], [gen.b], [kt_.b])
                S.cp("pool", kt_.t[64:96, :], krope.t[64:96, :], [krope.b], [kt_.b])
                for t0 in range(0, 34, 4):
                    nt = min(4, 34 - t0)
                    for i in range(nt):
                        S.mm(gen.t[:, i * 64:(i + 1) * 64], ckvn.t[:, (t0 + i) * 128:(t0 + i + 1) * 128],
                             wukv.t[:, h * 128 + 64:h * 128 + 128], True, True, [wukv.b, ckvn.b], [gen.b])
                    S.cp("dve", vh_.t[:, t0:t0 + nt, 0:64], gen.t[:, 0:nt * 64].rearrange("p (a b) -> p a b", b=64),
                         [gen.b], [vh_.b])
                for gi, (q0, T, _, _, lat) in enumerate(qgroups):
                    for c in range(2):
                        S.mm(gen.t[:96, :T], wuq.t[:, c, h * 96:(h + 1) * 96], cqn.t[:, c, q0:q0 + T], c == 0, c == 1,
                             [wuq.b, cqn.b], [gen.b])
                    S.cp("act", qt_.t[0:64, q0:q0 + T], gen.t[0:64, :T], [gen.b], [qt_.b])
                    if lat:
                        i = gi % 2
                        S.dma("sp", rp[i].t[:, :, :T], I["ropem"][0:96, :, q0:q0 + T], [], [rp[i].b])
                        S.cp("act", qf[i].t[:96, :T], gen.t[:96, :T], [gen.b], [qf[i].b])
                        S.mm(pq.t[:96, :T], K0["pm96"][:96, :96], qf[i].t[:96, :T], True, True, [qf[i].b], [pq.b])
                        S.tt("pool", t1[i].t[64:96, :T], qf[i].t[64:96, :T], rp[i].t[64:96, 0, :T], ALU.mult,
                             [qf[i].b, rp[i].b], [t1[i].b])
                        S.tt("dve", t2[i].t[64:96, :T], pq.t[64:96, :T], rp[i].t[64:96, 1, :T], ALU.mult,
                             [pq.b, rp[i].b], [t2[i].b])
                        S.tt("dve", qt_.t[64:96, q0:q0 + T], t1[i].t[64:96, :T], t2[i].t[64:96, :T], ALU.add,
                             [t1[i].b, t2[i].b], [qt_.b])
                    else:
                        S.cp("dve", qt_.t[64:96, q0:q0 + T], gen.t[64:96, :T], [gen.b], [qt_.b])
                scale = 96.0 ** -0.5
                k_ap = lambda kt, kt_=kt_: kt_.t[0:96, kt * 128:(kt + 1) * 128]
                q_ap = lambda q0, T, qt_=qt_: qt_.t[0:96, q0:q0 + T]
                v_ap = lambda kt, vh_=vh_: vh_.t[:, kt, :]
                kb, qb, vb = kt_.b, qt_.b, vh_.b
            else:
                h = hd - 8
                hh, c, kv = h % 2, h // 2, h // 4
                scale = 0.125
                k_ap = lambda kt, hh=hh, kv=kv: kgd.t[hh * 64:(hh + 1) * 64, kv, kt * 128:(kt + 1) * 128]
                q_ap = lambda q0, T, hh=hh, c=c: qg.t[hh * 64:(hh + 1) * 64, c, q0:q0 + T]
                v_ap = lambda kt, kv=kv: vg.t[:, kt, kv, :]
                kb, qb, vb = kgd.b, qg.b, vg.b
            for (q0, T, ka, kbnd, lat) in qgroups:
                u = unit[0]
                unit[0] += 1
                ao = accO[u % 2]
                npair = (kbnd - ka) // 2

                def issue_s(pi):
                    sp_ = sps[pi % 3]
                    for a_ in range(2):
                        S.mm(sp_.t[:, a_ * 512:a_ * 512 + T], k_ap(ka + 2 * pi + a_), q_ap(q0, T), True, True,
                             [kb, qb], [sp_.b])

                issue_s(0)
                if npair > 1:
                    issue_s(1)
                for pi in range(npair):
                    sp_ = sps[pi % 3]
                    p_ = pT[pi % 3]
                    if pi + 2 < npair:
                        issue_s(pi + 2)
                    S.act(p_.t[:, :].rearrange("p (a t) -> p a t", a=2)[:, :, :T],
                          sp_.t[:, :].rearrange("p (a t) -> p a t", a=2)[:, :, :T], AF.Exp, [sp_.b], [p_.b], scale=scale)
                    for a_ in range(2):
                        kt = ka + 2 * pi + a_
                        S.mm(ao.t[:, :T], v_ap(kt), p_.t[:, a_ * 512:a_ * 512 + T], kt == ka, kt == kbnd - 1,
                             [vb, p_.b], [ao.b])
                r_ = rs[u % 2]
                o_ = ot[u % 2]
                S.op("dve", lambda e, r_=r_, ao=ao, T=T: e.reciprocal(r_.t[:64, :T], ao.t[64:128, :T]),
                     [ao.b], [r_.b])
                S.tt("dve", o_.t[:64, :T], ao.t[:64, :T], r_.t[:64, :T], ALU.mult, [ao.b, r_.b], [o_.b])
                S.dma("sp", attn_o[hd * 64:(hd + 1) * 64, q0:q0 + T], o_.t[:64, :T], [o_.b], [])
        S.emit()


def phase_attn_out(nc, I, mvec, attn_o, xres):
    with ExitStack() as es:
        C = Ctx(nc, es)
        S = Sched(nc)
        wo = C.sb([128, 8, 1024], BF16, "wo")
        og = [C.sb([128, 8, 512], BF16, "og") for _ in range(2)]
        xg = [C.sb([128, 8, 512], F32, "xg") for _ in range(2)]
        pp = [C.ps("op") for _ in range(2)]
        for kc in range(8):
            S.dma("pool", wo.t[:, kc, :], I["wout_r"][:, kc, :], [], [wo.b])
        groups = [("xT", i * 512, 512, i * 512, 0) for i in range(8)] + [("ctxT", 0, CTX, SEQ, 1)]
        n = 0
        for gi, (src, c0, T, q0, j) in enumerate(groups):
            x, o = xg[gi % 2], og[gi % 2]
            S.dma("sp", x.t[:, :, :T], I[src].rearrange("(kc p) t -> p kc t", p=128)[:, :, c0:c0 + T], [], [x.b])
            S.dma("act", o.t[:, :, :T], attn_o.rearrange("(kc p) t -> p kc t", p=128)[:, :, q0:q0 + T], [], [o.b])
            for c in range(8):
                p = pp[n % 2]
                n += 1
                for kc in range(8):
                    S.mm(p.t[:, :T], wo.t[:, kc, c * 128:(c + 1) * 128], o.t[:, kc, :T], kc == 0, kc == 7,
                         [wo.b, o.b], [p.b])
                S.stt(x.t[:, c, :T], p.t[:, :T], mvec.t[:, j, 2, c:c + 1], x.t[:, c, :T], ALU.mult, ALU.add,
                      [p.b, x.b], [x.b])
            S.dma("sp", xres.rearrange("(kc p) t -> p kc t", p=128)[:, :, q0:q0 + T], x.t[:, :, :T], [x.b], [])
        S.emit()


def attention_layer(nc, I, K0, mvec, attn_o, xres):
    with ExitStack() as es:
        C = Ctx(nc, es)
        P = dict(ckvn=C.sb([128, NK], BF16, "ckvn"), krope=C.sb([96, NK], BF16, "krope"),
                 cqn=C.sb([128, 2, NQ], BF16, "cqn"), qg=C.sb([128, 4, NQ], BF16, "qg"),
                 kgd=C.sb([128, 2, NK], BF16, "kgd"), vg=C.sb([128, 34, 2, 128], BF16, "vg"))
        phase_attn_proj(nc, I, K0, mvec, P)
        phase_attn_core(nc, I, K0, P, attn_o)
    phase_attn_out(nc, I, mvec, attn_o, xres)


def make_consts():
    c = np.zeros((128, 5, 128), np.float32)
    c[:, 0, :] = np.eye(128, dtype=np.float32)
    c[:, 1, :] = 1.0
    c[0:64, 2, 0:64] = 1.0
    c[64:128, 2, 64:128] = 1.0
    for i in range(64):
        c[2 * i + 1, 3, 2 * i] = -1.0
        c[2 * i, 3, 2 * i + 1] = 1.0
    for i in range(32, 48):
        c[2 * i + 1, 4, 2 * i] = -1.0
        c[2 * i, 4, 2 * i + 1] = 1.0
    return c


def rope_tables(hf):
    pos = np.arange(SEQ) if hf == 0 else np.arange(SEQ)[::-1]
    row = (pos // 64).astype(np.float32)
    col = (pos % 64).astype(np.float32)

    def ang(rot_dim):
        nf = rot_dim // 4
        inv = (np.float32(10000.0) ** (-np.arange(nf, dtype=np.float32) / np.float32(nf))).astype(np.float32)
        return np.concatenate([row[:, None] * inv, col[:, None] * inv], axis=-1).astype(np.float32)

    ag = ang(64)
    am = ang(32)
    ropeg = np.zeros((128, 2, SEQ), np.float32)
    ropem = np.zeros((128, 2, SEQ), np.float32)
    for p in range(128):
        a = ag[:, (p % 64) // 2]
        ropeg[p, 0] = np.cos(a)
        ropeg[p, 1] = np.sin(a)
    for p in range(64, 96):
        a = am[:, (p - 64) // 2]
        ropem[p, 0] = np.cos(a)
        ropem[p, 1] = np.sin(a)
    return ropeg, ropem


def fm(v, k):
    return np.ascontiguousarray(np.asarray(v, np.float32).reshape(k, 128).T)


def wr(w):
    w = np.asarray(w, np.float32)
    K, N = w.shape
    return np.ascontiguousarray(w.reshape(K // 128, 128, N).transpose(1, 0, 2))


def shared_inputs(inp):
    sh = {}
    wm = np.asarray(inp["w_mod"], np.float32)
    sh["wmod_r"] = np.ascontiguousarray(wm.reshape(2, 8, 128, 6, 1024).transpose(0, 3, 2, 1, 4))
    bm = np.asarray(inp["b_mod"], np.float32)
    sh["bmod_r"] = np.ascontiguousarray(bm.reshape(2, 48, 128).transpose(0, 2, 1))
    ng = np.asarray(inp["norm_g"], np.float32)
    sh["ng_r"] = np.ascontiguousarray(ng.reshape(2, 2, 8, 128).transpose(0, 3, 1, 2))
    sh["consts"] = make_consts()
    sh["win_r"] = wr(inp["attn_w_in"][0])
    ag = np.zeros((128, 6), np.float32)
    ag[:, 0:2] = fm(inp["mla_g_cq"][0], 2)
    ag[:, 2] = np.asarray(inp["mla_g_ckv"][0], np.float32)
    gq = np.asarray(inp["gqa_g_q"][0], np.float32)
    gk = np.asarray(inp["gqa_g_k"][0], np.float32)
    ag[:, 3] = np.concatenate([gq, gq])
    ag[:, 4] = np.concatenate([gk, gk])
    sh["attn_g"] = ag
    sh["wuq_r"] = wr(inp["mla_w_uq"][0])
    sh["wukv"] = np.ascontiguousarray(np.asarray(inp["mla_w_ukv"][0], np.float32))
    sh["wout_r"] = wr(inp["attn_w_out"][0])
    return sh


def core_inputs_l0(inp, sh, core, ropes):
    b, hf = core // 2, core % 2
    x = np.asarray(inp["x"][b], np.float32)
    if hf == 1:
        x = x[::-1]
    d = dict(sh)
    d["xT"] = np.ascontiguousarray(x.T)
    cx = np.asarray(inp["ctx"][b], np.float32)
    if hf == 1:
        cx = cx[::-1]
    d["ctxT"] = np.ascontiguousarray(cx.T)
    cc = np.zeros((128, 8, 2), np.float32)
    cc[:, :, 0] = fm(inp["c"][b], 8)
    cc[:, :, 1] = fm(inp["c_ctx"], 8)
    d["cc"] = cc
    d["ropeg"], d["ropem"] = ropes[hf]
    return d


L0_INPUT_SHAPES = dict(
    xT=[1024, SEQ], ctxT=[1024, CTX], cc=[128, 8, 2], wmod_r=[2, 6, 128, 8, 1024], bmod_r=[2, 128, 48],
    ng_r=[2, 128, 2, 8], consts=[128, 5, 128], ropeg=[128, 2, SEQ], ropem=[128, 2, SEQ],
    win_r=[128, 8, ATTN_IN], attn_g=[128, 6], wuq_r=[128, 2, 768], wukv=[128, 1024], wout_r=[128, 8, 1024])


def build_l0(with_moe=True, n_exp=N_EXP):
    nc = bass.Bass("TRN2", target_bir_lowering=False)
    shapes = dict(L0_INPUT_SHAPES)
    if with_moe:
        shapes.update(moe_input_shapes(0, n_exp))
    I = {k: dram(nc, k, v, F32, "ExternalInput") for k, v in shapes.items()}
    attn_o = dram(nc, "attn_o", [1024, NQ], BF16, "Internal")
    xres = dram(nc, "xres", [1024, NQ], F32, "ExternalOutput")
    gt_scr = dram(nc, "gt_scr0", [32, 1152], F32, "Internal")
    with ExitStack() as es:
        init_sync(nc, es)
        K0 = load_consts(nc, es, I)
        C = Ctx(nc, es)
        mvec = C.sb([128, 2, 6, 8], F32, "mvec")
        phase_mod(nc, I, 0, mvec)
        attention_layer(nc, I, K0, mvec, attn_o, xres)
        if with_moe:
            moe_layer(nc, I, K0, mvec, 0, xres, gt_scr, moe_passes_l0(), n_exp)
    return nc


AX = mybir.AxisListType


def moe_layer(nc, I, K0, mvec, li, xres, gt_scr, passes, n_exp=N_EXP, dbg=None):
    sfx = str(li)
    for groups in passes:
        TP = sum(g[1] for g in groups)
        pos = []
        a = 0
        for g in groups:
            pos.append(a)
            a += g[1]
        with ExitStack() as es:
            C = Ctx(nc, es)
            xp = C.sb([128, 8, TP], F32, "xp")
            hb = C.sb([128, 8, TP], BF16, "hbp")
            GT = C.sb([32, TP], F32, "GT")
            xres3 = xres.rearrange("(kc p) t -> p kc t", p=128)
            with ExitStack() as es1:
                C1 = Ctx(nc, es1)
                S = Sched(nc)
                NS = norm_scratch(C1, K0["ones_f"])
                hf = [C1.sb([128, 8, 512], F32, "hf") for _ in range(2)]
                wrt = C1.sb([128, 8, 32], F32, "wrt")
                brt = C1.sb([128, 32], F32, "brt")
                lg = [C1.sb([128, 32], F32, "lg") for _ in range(2)]
                mx = [C1.sb([128, 8], F32, "mx") for _ in range(2)]
                ngm = [C1.sb([128, 1], F32, "ngm") for _ in range(2)]
                ex = [C1.sb([128, 32], F32, "ex") for _ in range(2)]
                mk = [C1.sb([128, 32], F32, "mk") for _ in range(2)]
                sm = [C1.sb([128, 1], F32, "sm") for _ in range(2)]
                Gt = [C1.sb([128, 32], F32, "Gt") for _ in range(2)]
                lps = [C1.ps("lps") for _ in range(2)]
                tps = [C1.ps("tps") for _ in range(2)]
                S.dma("sp", wrt.t[:], I["wr_r" + sfx][:, :, :], [], [wrt.b])
                S.dma("sp", brt.t[:], I["br" + sfx][0:1, :].to_broadcast([128, 32]), [], [brt.b])
                xb = [Buf() for _ in groups]
                tile_i = 0
                for gi, (c0, T, j) in enumerate(groups):
                    p0 = pos[gi]
                    xv = Tl(xp.t[:, :, p0:p0 + T])
                    xv.b = xb[gi]
                    S.dma("sp", xv.t, xres3[:, :, c0:c0 + T], [], [xv.b])
                    hfv = hf[gi % 2]
                    hbv = Tl(hb.t[:, :, p0:p0 + T])
                    hbv.b = hb.b
                    emit_norm_mod(S, NS, xv, T, mvec.t[:, j, 3, :], mvec.t[:, j, 4, :], hbv, hfv)
                    for tt_ in range(T // 128):
                        i = tile_i % 2
                        tile_i += 1
                        for kc in range(8):
                            S.mm(lps[i].t[:, 0:32], hfv.t[:, kc, tt_ * 128:(tt_ + 1) * 128], wrt.t[:, kc, :],
                                 kc == 0, kc == 7, [hfv.b, wrt.b], [lps[i].b])
                        S.tt("dve", lg[i].t[:], lps[i].t[:, 0:32], brt.t[:], ALU.add, [lps[i].b, brt.b], [lg[i].b])
                        S.op("dve", lambda e, i=i: e.max(mx[i].t[:], lg[i].t[:]), [lg[i].b], [mx[i].b])
                        S.ts("dve", ngm[i].t[:], mx[i].t[:, 0:1], -1.0, None, ALU.mult, None, [mx[i].b], [ngm[i].b])
                        S.act(ex[i].t[:], lg[i].t[:], AF.Exp, [lg[i].b, ngm[i].b], [ex[i].b], bias=ngm[i].t[:, 0:1])
                        S.ts("dve", mk[i].t[:], lg[i].t[:], mx[i].t[:, 3:4], None, ALU.is_ge, None,
                             [lg[i].b, mx[i].b], [mk[i].b])
                        S.tt("dve", ex[i].t[:], ex[i].t[:], mk[i].t[:], ALU.mult, [ex[i].b, mk[i].b], [ex[i].b])
                        S.op("dve", lambda e, i=i: e.reduce_sum(sm[i].t[:], ex[i].t[:], axis=AX.X), [ex[i].b], [sm[i].b])
                        S.op("dve", lambda e, i=i: e.reciprocal(sm[i].t[:], sm[i].t[:]), [sm[i].b], [sm[i].b])
                        S.ts("dve", Gt[i].t[:], ex[i].t[:], sm[i].t[:, 0:1], None, ALU.mult, None,
                             [ex[i].b, sm[i].b], [Gt[i].b])
                        S.tr(tps[i].t[0:32, 0:128], Gt[i].t[:], K0["ident_f"], [Gt[i].b], [tps[i].b])
                        tcol = p0 + tt_ * 128
                        S.cp("act", GT.t[0:32, tcol:tcol + 128], tps[i].t[0:32, 0:128], [tps[i].b], [GT.b])
                S.dma("sp", gt_scr[:, 0:TP], GT.t[0:32, :], [GT.b], [])
                if dbg is not None:
                    S.dma("sp", dbg["hb"][:, :, 0:TP], hb.t[:, :, :], [hb.b], [])
                S.emit()
            with ExitStack() as es2:
                C2 = Ctx(nc, es2)
                S = Sched(nc)
                actb = C2.sb([128, 8, TP], BF16, "actb")
                wgu = [C2.sb([128, 8, 2, 128], BF16, "wgu") for _ in range(4)]
                wd = [C2.sb([128, 8, 128], BF16, "wd") for _ in range(3)]
                gbc = [C2.sb([128, TP], F32, "gbc") for _ in range(2)]
                bgu = C2.sb([128, n_exp, 16], F32, "bgu")
                bd = C2.sb([32, 1024], F32, "bd")
                g1 = [C2.sb([128, 512], F32, "g1") for _ in range(3)]
                s1 = [C2.sb([128, 512], F32, "s1") for _ in range(3)]
                u1 = [C2.sb([128, 512], F32, "u1") for _ in range(3)]
                v1 = [C2.sb([128, 512], F32, "v1") for _ in range(3)]
                gps = [C2.ps("gps") for _ in range(3)]
                ups = [C2.ps("ups") for _ in range(3)]
                dps = [C2.ps("dps") for _ in range(2)]
                S.dma("sp", bgu.t[:], I["bgu_r" + sfx][:, 0:n_exp, :], [], [bgu.b])
                S.dma("sp", bd.t[0:n_exp, :], I["bd" + sfx][0:n_exp, :], [], [bd.b])
                xb = [Buf() for _ in groups]
                ab = [[Buf() for _ in groups] for _ in range(8)]
                nu = 0
                nw = 0
                nd = 0
                npd = 0
                PFG, PFD = 3, 2

                def issue_gu(u):
                    if u < n_exp * 8:
                        w_ = wgu[u % 4]
                        S.dma("pool", w_.t[:].rearrange("p a b c -> p (a b c)"), I["wgu" + sfx][u // 8, u % 8], [], [w_.b])

                def issue_d(u):
                    if u < n_exp * 8:
                        w_ = wd[u % 3]
                        S.dma("pool", w_.t[:].rearrange("p a b -> p (a b)"), I["wd" + sfx][u // 8, u % 8], [], [w_.b])

                for u in range(PFG):
                    issue_gu(u)
                for u in range(PFD):
                    issue_d(u)
                for e in range(n_exp):
                    gb = gbc[e % 2]
                    S.dma("sp", gb.t[:], gt_scr[e:e + 1, 0:TP].to_broadcast([128, TP]), [], [gb.b])
                    S.act(gb.t[:], gb.t[:], AF.Identity, [gb.b], [gb.b], scale=1.0 / 1.702)
                    for j in range(8):
                        w = wgu[nw % 4]
                        issue_gu(nw + PFG)
                        nw += 1
                        for gi, (c0, T, jj) in enumerate(groups):
                            p0 = pos[gi]
                            i = nu % 3
                            nu += 1
                            for kc in range(8):
                                S.mm(gps[i].t[:, :T], w.t[:, kc, 0, :], hb.t[:, kc, p0:p0 + T], kc == 0, kc == 7,
                                     [w.b, hb.b], [gps[i].b])
                            for kc in range(8):
                                S.mm(ups[i].t[:, :T], w.t[:, kc, 1, :], hb.t[:, kc, p0:p0 + T], kc == 0, kc == 7,
                                     [w.b, hb.b], [ups[i].b])
                            S.ts("dve", g1[i].t[:, :T], gps[i].t[:, :T], bgu.t[:, e, j:j + 1], 7.0, ALU.add, ALU.min,
                                 [gps[i].b, bgu.b], [g1[i].b])
                            S.act(s1[i].t[:, :T], g1[i].t[:, :T], AF.Silu, [g1[i].b], [s1[i].b], scale=1.702)
                            S.act(u1[i].t[:, :T], ups[i].t[:, :T], AF.Identity, [ups[i].b, bgu.b], [u1[i].b],
                                  bias=bgu.t[:, e, 8 + j:9 + j])
                            S.ts("pool", u1[i].t[:, :T], u1[i].t[:, :T], 7.0, -7.0, ALU.min, ALU.max,
                                 [u1[i].b], [u1[i].b])
                            S.stt(v1[i].t[:, :T], u1[i].t[:, :T], 1.0, s1[i].t[:, :T], ALU.add, ALU.mult,
                                  [u1[i].b, s1[i].b], [v1[i].b])
                            S.tt("pool", actb.t[:, j, p0:p0 + T], v1[i].t[:, :T], gb.t[:, p0:p0 + T], ALU.mult,
                                 [v1[i].b, gb.b], [ab[j][gi]])
                    for c in range(8):
                        w = wd[nd % 3]
                        issue_d(nd + PFD)
                        nd += 1
                        for gi, (c0, T, jj) in enumerate(groups):
                            p0 = pos[gi]
                            p = dps[npd % 2]
                            npd += 1
                            for jc in range(8):
                                S.mm(p.t[:, :T], w.t[:, jc, :], actb.t[:, jc, p0:p0 + T], jc == 0, jc == 7,
                                     [w.b, ab[jc][gi]], [p.b])
                            S.stt(xp.t[:, c, p0:p0 + T], p.t[:, :T], mvec.t[:, jj, 5, c:c + 1], xp.t[:, c, p0:p0 + T],
                                  ALU.mult, ALU.add, [p.b, xb[gi]], [xb[gi]])
                for c in range(8):
                    for gi, (c0, T, jj) in enumerate(groups):
                        p0 = pos[gi]
                        p = dps[npd % 2]
                        npd += 1
                        S.mm(p.t[:, :T], bd.t[0:n_exp, c * 128:(c + 1) * 128], GT.t[0:n_exp, p0:p0 + T], True, True,
                             [bd.b, GT.b], [p.b])
                        S.stt(xp.t[:, c, p0:p0 + T], p.t[:, :T], mvec.t[:, jj, 5, c:c + 1], xp.t[:, c, p0:p0 + T],
                              ALU.mult, ALU.add, [p.b, xb[gi]], [xb[gi]])
                for gi, (c0, T, jj) in enumerate(groups):
                    p0 = pos[gi]
                    S.dma("sp", xres3[:, :, c0:c0 + T], xp.t[:, :, p0:p0 + T], [xb[gi]], [])
                if dbg is not None:
                    S.dma("sp", dbg["actb"][:, :, 0:TP], actb.t[:, :, :], [ab[j][gi] for j in range(8) for gi in range(len(groups))], [])
                    S.dma("sp", dbg["gbc"][:, 0:TP], gbc[(n_exp - 1) % 2].t[:, :], [gbc[(n_exp - 1) % 2].b], [])
                S.emit()


def moe_passes_l0():
    ps = []
    for p in range(2):
        ps.append([(p * 1536 + k * 512, 512, 0) for k in range(3)])
    ps.append([(3072, 512, 0), (3584, 512, 0), (SEQ, CTX, 1)])
    return ps


def moe_passes_l1():
    return [[(p * 1024, 512, 0), (p * 1024 + 512, 512, 0)] for p in range(2)]


def moe_shared_inputs(inp, li, n_exp=N_EXP):
    sh = {}
    s = str(li)
    sh["wr_r" + s] = wr(inp["moe_w_router"][li])
    sh["br" + s] = np.ascontiguousarray(np.asarray(inp["moe_b_router"][li], np.float32).reshape(1, 32))
    bg = np.asarray(inp["moe_b_gate_up"][li], np.float32)
    sh["bgu_r" + s] = np.ascontiguousarray(bg.reshape(32, 16, 128).transpose(2, 0, 1))
    sh["bd" + s] = np.ascontiguousarray(np.asarray(inp["moe_b_down"][li], np.float32))
    wg = np.asarray(inp["moe_w_gate_up"][li], np.float32)[:n_exp]
    sh["wgu" + s] = np.ascontiguousarray(
        wg.reshape(n_exp, 8, 128, 2, 8, 128).transpose(0, 4, 2, 1, 3, 5)).reshape(n_exp, 8, 128, 2048)
    wdn = np.asarray(inp["moe_w_down"][li], np.float32)[:n_exp]
    sh["wd" + s] = np.ascontiguousarray(
        wdn.reshape(n_exp, 8, 128, 8, 128).transpose(0, 3, 2, 1, 4)).reshape(n_exp, 8, 128, 1024)
    return sh


def moe_input_shapes(li, n_exp=N_EXP):
    s = str(li)
    return {"wr_r" + s: [128, 8, 32], "br" + s: [1, 32], "bgu_r" + s: [128, 32, 16], "bd" + s: [32, 1024],
            "wgu" + s: [n_exp, 8, 128, 2048], "wd" + s: [n_exp, 8, 128, 1024]}


def build_moe_test(n_exp, li=0):
    nc = bass.Bass("TRN2", target_bir_lowering=False)
    shapes = dict(cc=[128, 8, 2], wmod_r=[2, 6, 128, 8, 1024], bmod_r=[2, 128, 48], ng_r=[2, 128, 2, 8],
                  consts=[128, 5, 128], x1T=[1024, NQ])
    shapes.update(moe_input_shapes(li, n_exp))
    I = {k: dram(nc, k, v, F32, "ExternalInput") for k, v in shapes.items()}
    xres = dram(nc, "xres", [1024, NQ], F32, "ExternalOutput")
    gt_scr = dram(nc, "gt_scr", [32, 1152], F32, "ExternalOutput")
    dbg = dict(hb=dram(nc, "dbg_hb", [128, 8, 1152], BF16, "ExternalOutput"),
               actb=dram(nc, "dbg_actb", [128, 8, 1152], BF16, "ExternalOutput"),
               gbc=dram(nc, "dbg_gbc", [128, 1152], F32, "ExternalOutput"))
    with ExitStack() as es:
        init_sync(nc, es)
        K0 = load_consts(nc, es, I)
        C = Ctx(nc, es)
        mvec = C.sb([128, 2, 6, 8], F32, "mvec")
        with ExitStack() as es1:
            S = Sched(nc)
            S.dma("sp", xres[:, :], I["x1T"][:, :], [], [])
            S.emit()
        phase_mod(nc, I, li, mvec)
        moe_layer(nc, I, K0, mvec, li, xres, gt_scr, moe_passes_l0(), n_exp, dbg)
    return nc


def ssm_scratch(nc):
    d = {}
    def mk(name, shape, dt):
        d[name] = dram(nc, "s1_" + name, shape, dt, "Internal")
    mk("z", [1024, HALF], F32)
    mk("xc_own", [1024, HALF], BF16); mk("B_own", [256, HALF], BF16); mk("C_own", [256, HALF], BF16)
    mk("xc_par", [1024, HALF], BF16); mk("B_par", [256, HALF], BF16)
    mk("xc_ctx", [1024, CTX], BF16); mk("B_ctx", [256, CTX], BF16)
    mk("dt_own", [HALF, 16], F32); mk("dt_par", [HALF, 16], F32); mk("dt_ctx", [CTX, 16], F32)
    mk("vc", [1024, HALF], F32)
    mk("Y", [1024, HALF], F32)
    mk("mix", [2048, HALF], BF16)
    return d


def phase_ssm_proj(nc, I, K0, mvec, SC):
    with ExitStack() as es:
        C = Ctx(nc, es)
        S = Sched(nc)
        win = C.sb([128, 8, 2576], BF16, "swin")
        hb = {"own": C.sb([128, 8, HALF], BF16, "hbo"), "par": C.sb([128, 8, HALF], BF16, "hbp"),
              "ctx": C.sb([128, 8, CTX], BF16, "hbc")}
        xg = [C.sb([128, 8, 512], F32, "xg") for _ in range(1)]
        NS = norm_scratch(C, K0["ones_f"])
        cw = C.sb([128, 12, 2, 5], F32, "cw")
        cb = C.sb([128, 12], F32, "cb")
        dw = C.sb([128, 8, 31], F32, "dw")
        db = C.sb([128, 8], F32, "db")
        uext = [C.sb([128, HALF + 4], F32, "uext") for _ in range(1)]
        vext = [C.sb([128, HALF + 30], F32, "vext") for _ in range(1)]
        acc = [C.sb([128, 512], F32, "acc") for _ in range(2)]
        ob = [C.sb([128, 512], BF16, "ob") for _ in range(2)]
        of = [C.sb([128, 512], F32, "of") for _ in range(2)]
        sg = [C.sb([128, 512], F32, "sg") for _ in range(2)]
        hl = [C.sb([128, 16], F32, "hl") for _ in range(2)]
        dtt = [C.sb([128, 16], F32, "dtt") for _ in range(2)]
        pp = [C.ps("sproj") for _ in range(3)]
        ph = C.ps("shalo")
        pd = C.ps("sdt")
        for kc in range(8):
            S.dma("pool", win.t[:, kc, :], I["swin_r"][:, kc, 0:2576], [], [win.b], max_dma_last_dim=4096)
        S.dma("sp", cw.t[:], I["cw_r"][:, :, :, :], [], [cw.b])
        S.dma("sp", cb.t[:], I["cb_r"][:, :], [], [cb.b])
        S.dma("sp", dw.t[:], I["dw_r"][:, :, :], [], [dw.b])
        S.dma("sp", db.t[:], I["db_r"][:, :], [], [db.b])
        gi = 0
        for (name, src, ng, T, j) in [("own", "x1o", 4, 512, 0), ("par", "x1p", 4, 512, 0), ("ctx", "c1", 1, CTX, 1)]:
            for g in range(ng):
                x = xg[0]
                gi += 1
                S.dma("sp", x.t[:, :, :T], I[src].rearrange("(kc p) t -> p kc t", p=128)[:, :, g * 512:g * 512 + T],
                      [], [x.b])
                hv = Tl(hb[name].t[:, :, g * 512:g * 512 + T])
                hv.b = hb[name].b
                emit_norm_mod(S, NS, x, T, mvec.t[:, j, 0, :], mvec.t[:, j, 1, :], hv)
        npj = [0]

        def proj(col0, M, name, t0, T):
            p = pp[npj[0] % 3]
            npj[0] += 1
            for kc in range(8):
                S.mm(p.t[:M, :T], win.t[:, kc, col0:col0 + M], hb[name].t[:, kc, t0:t0 + T], kc == 0, kc == 7,
                     [win.b, hb[name].b], [p.b])
            return p

        n = [0]
        for m in range(8):
            for g in range(4):
                p = proj(m * 128, 128, "own", g * 512, 512)
                o = of[n[0] % 2]
                n[0] += 1
                S.cp("act", o.t[:, :], p.t[:, :512], [p.b], [o.b])
                S.dma("sp", SC["z"][m * 128:(m + 1) * 128, g * 512:(g + 1) * 512], o.t[:, :], [o.b], [])
        sets = [("own", 12, HALF, 0, "par"), ("par", 10, HALF, 0, "own"), ("ctx", 10, CTX, 0, None)]
        dst = {"own": ("xc_own", "B_own", "C_own"), "par": ("xc_par", "B_par", None), "ctx": ("xc_ctx", "B_ctx", None)}
        nu = 0
        for (name, nch, TT, frame, other) in sets:
            for m in range(nch):
                u = uext[0]
                nu += 1
                col0 = 1024 + m * 128
                if name == "par":
                    for kc in range(8):
                        S.mm(ph.t[:, 0:2], win.t[:, kc, col0:col0 + 128], hb["own"].t[:, kc, HALF - 2:HALF], kc == 0, kc == 7,
                             [win.b, hb["own"].b], [ph.b])
                    S.cp("act", u.t[:, 0:2], ph.t[:, 0:2], [ph.b], [u.b])
                else:
                    S.memset("pool", u.t[:, 0:2], 0.0, [u.b])
                for g in range(max(1, TT // 512)):
                    T = min(512, TT)
                    p = proj(col0, 128, name, g * 512, T)
                    S.cp("act", u.t[:, 2 + g * 512:2 + g * 512 + T], p.t[:, :T], [p.b], [u.b])
                if name != "own":
                    S.memset("pool", u.t[:, 2 + TT:4 + TT], 0.0, [u.b])
                else:
                    for kc in range(8):
                        S.mm(ph.t[:, 0:2], win.t[:, kc, col0:col0 + 128], hb["par"].t[:, kc, 0:2], kc == 0, kc == 7,
                             [win.b, hb["par"].b], [ph.b])
                    S.cp("act", u.t[:, 2 + TT:4 + TT], ph.t[:, 0:2], [ph.b], [u.b])
                for g in range(max(1, TT // 512)):
                    T = min(512, TT)
                    a = acc[n[0] % 2]
                    o = ob[n[0] % 2]
                    n[0] += 1
                    b0 = g * 512
                    S.ts("dve", a.t[:, :T], u.t[:, b0:b0 + T], cw.t[:, m, frame, 0:1], cb.t[:, m:m + 1], ALU.mult, ALU.add,
                         [u.b, cw.b, cb.b], [a.b])
                    for k in range(1, 5):
                        S.stt(a.t[:, :T], u.t[:, b0 + k:b0 + k + T], cw.t[:, m, frame, k:k + 1], a.t[:, :T], ALU.mult, ALU.add,
                              [u.b, a.b], [a.b])
                    S.act(o.t[:, :T], a.t[:, :T], AF.Silu, [a.b], [o.b])
                    if m < 8:
                        d_ = SC[dst[name][0]][m * 128:(m + 1) * 128, b0:b0 + T]
                    elif m < 10:
                        d_ = SC[dst[name][1]][(m - 8) * 128:(m - 7) * 128, b0:b0 + T]
                    else:
                        d_ = SC[dst[name][2]][(m - 10) * 128:(m - 9) * 128, b0:b0 + T]
                    S.dma("sp", d_, o.t[:, :T], [o.b], [])
        nt = 0
        for (name, TT) in [("own", HALF), ("par", HALF), ("ctx", CTX)]:
            for t_ in range(TT // 128):
                d_ = dtt[nt % 2]
                nt += 1
                for kc in range(8):
                    S.mm(pd.t[:, 0:16], hb[name].t[:, kc, t_ * 128:(t_ + 1) * 128], win.t[:, kc, 2560:2576], kc == 0, kc == 7,
                         [hb[name].b, win.b], [pd.b])
                S.cp("act", d_.t[:, :], pd.t[:, 0:16], [pd.b], [d_.b])
                S.dma("sp", SC["dt_" + name][t_ * 128:(t_ + 1) * 128, :], d_.t[:, :], [d_.b], [])
        for kc in range(8):
            S.dma("pool", win.t[:, kc, 0:2048], I["swin_r"][:, kc, 2576:4624], [], [win.b], max_dma_last_dim=4096)
        for m in range(8):
            v = vext[0]
            ca, cbb = m * 128, 1024 + m * 128
            S.memset("pool", v.t[:, 0:15], 0.0, [v.b])
            for g in range(4):
                pa = proj(ca, 128, "own", g * 512, 512)
                pb_ = proj(cbb, 128, "own", g * 512, 512)
                s_ = sg[n[0] % 2]
                n[0] += 1
                S.act(s_.t[:, :], pb_.t[:, :512], AF.Sigmoid, [pb_.b], [s_.b])
                S.tt("dve", v.t[:, 15 + g * 512:15 + (g + 1) * 512], pa.t[:, :512], s_.t[:, :], ALU.mult, [pa.b, s_.b], [v.b])
            h_ = hl[m % 2]
            for kc in range(8):
                S.mm(ph.t[:, 0:16], win.t[:, kc, ca:ca + 128], hb["par"].t[:, kc, 0:16], kc == 0, kc == 7,
                     [win.b, hb["par"].b], [ph.b])
            for kc in range(8):
                S.mm(ph.t[:, 16:32], win.t[:, kc, cbb:cbb + 128], hb["par"].t[:, kc, 0:16], kc == 0, kc == 7,
                     [win.b, hb["par"].b], [ph.b])
            S.act(h_.t[:, :], ph.t[:, 16:32], AF.Sigmoid, [ph.b], [h_.b])
            S.tt("dve", v.t[:, 15 + HALF:30 + HALF], ph.t[:, 0:15], h_.t[:, 0:15], ALU.mult, [ph.b, h_.b], [v.b])
            for g in range(4):
                a = acc[n[0] % 2]
                n[0] += 1
                b0 = g * 512
                S.ts("dve", a.t[:, :], v.t[:, b0:b0 + 512], dw.t[:, m, 0:1], db.t[:, m:m + 1], ALU.mult, ALU.add,
                     [v.b, dw.b, db.b], [a.b])
                for k in range(1, 31):
                    S.stt(a.t[:, :], v.t[:, b0 + k:b0 + k + 512], dw.t[:, m, k:k + 1], a.t[:, :], ALU.mult, ALU.add,
                          [v.b, a.b], [a.b])
                S.dma("sp", SC["vc"][m * 128:(m + 1) * 128, b0:b0 + 512], a.t[:, :], [a.b], [])
        S.emit()


def phase_ssd(nc, I, K0, SC):
    with ExitStack() as es:
        C = Ctx(nc, es)
        S = Sched(nc)
        U = C.sb([128, 2, 128], F32, "Umask")
        par = C.sb([128, 2, 2, 16], F32, "ssmp")
        aneg = C.sb([128, 2, 16], F32, "aneg")
        dsk = C.sb([64, 2, 16], F32, "dsk")
        dsum = C.sb([64, 16], F32, "dsum")
        St = [C.sb([128, 16, 64], F32, "St") for _ in range(2)]
        Sb = [C.sb([128, 16, 64], BF16, "Sb") for _ in range(2)]
        xcf = [C.sb([128, 8, 128], BF16, "xcf") for _ in range(2)]
        bcf = [C.sb([128, 4, 128], BF16, "bcf") for _ in range(2)]
        xtok = [C.sb([128, 16, 64], BF16, "xtok") for _ in range(2)]
        xdt = [C.sb([128, 16, 64], BF16, "xdt") for _ in range(2)]
        xdw = [C.sb([128, 16, 64], BF16, "xdw") for _ in range(2)]
        btok = [C.sb([128, 2, 128], BF16, "btok") for _ in range(2)]
        dtr = [C.sb([128, 16], F32, "dtr") for _ in range(2)]
        dtv = [C.sb([128, 16], F32, "dtv") for _ in range(2)]
        dav = [C.sb([128, 16], F32, "dav") for _ in range(2)]
        csb = [C.sb([128, 16], F32, "csb") for _ in range(2)]
        wend = [C.sb([128, 16], F32, "wend") for _ in range(2)]
        etot = [C.sb([128, 16], F32, "etot") for _ in range(2)]
        cbm = [C.sb([128, 2, 128], F32, "cbm") for _ in range(2)]
        dall = [C.sb([128, 16, 128], F32, "dall") for _ in range(2)]
        dc = [C.sb([128, 512], F32, "dc") for _ in range(2)]
        ee = [C.sb([128, 512], F32, "ee") for _ in range(2)]
        el = [C.sb([128, 512], F32, "el") for _ in range(2)]
        mp = [C.sb([128, 512], BF16, "mp") for _ in range(2)]
        cs_ = [C.sb([128, 512], BF16, "cs") for _ in range(2)]
        ysb = [C.sb([64, 16, 128], F32, "ysb") for _ in range(2)]
        yA = [C.sb([64, 16, 128], F32, "yA") for _ in range(2)]
        xh = [C.sb([64, 16, 128], BF16, "xh") for _ in range(2)]
        ptx = C.ps("ptx", (128, 1024), BF16)
        ptb = C.ps("ptb", (128, 1024), BF16)
        pcb = C.ps("pcb")
        psmb = Buf()
        pab = [C.ps("pab") for _ in range(2)]
        py = [C.ps("py") for _ in range(2)]
        pst = C.ps("pst")
        S.dma("sp", U.t[:], I["consts2"][:, :, :], [], [U.b])
        S.dma("sp", par.t[:], I["ssm_p"][:, :, :, :], [], [par.b])
        S.dma("sp", dsk.t[:], I["dsk_r"][:, :, :], [], [dsk.b])
        S.act(aneg.t[:], par.t[:, :, 1, :], AF.Exp, [par.b], [aneg.b])
        S.ts("dve", aneg.t[:], aneg.t[:], -1.0, None, ALU.mult, None, [aneg.b], [aneg.b])
        S.tt("dve", dsum.t[:], dsk.t[:, 0, :], dsk.t[:, 1, :], ALU.add, [dsk.b], [dsum.b])
        cnt = [0]

        def chunk(d, name, ci, style, full, last_dir):
            i = cnt[0] % 2
            cnt[0] += 1
            t0 = ci * 128
            S.dma("sp", dtr[i].t[:], SC["dt_" + name][t0:t0 + 128, :], [], [dtr[i].b])
            S.dma("act", xcf[i].t[:], SC["xc_" + name].rearrange("(m p) t -> p m t", p=128)[:, :, t0:t0 + 128], [], [xcf[i].b])
            S.dma("sp", bcf[i].t[:, 0:2, :], SC["B_" + name].rearrange("(g p) t -> p g t", p=128)[:, :, t0:t0 + 128], [], [bcf[i].b])
            if full:
                S.dma("sp", bcf[i].t[:, 2:4, :], SC["C_own"].rearrange("(g p) t -> p g t", p=128)[:, :, t0:t0 + 128], [], [bcf[i].b])
            S.tt("dve", dtv[i].t[:], dtr[i].t[:], par.t[:, d, 0, :], ALU.add, [dtr[i].b, par.b], [dtv[i].b])
            S.act(dtv[i].t[:], dtv[i].t[:], AF.Exp, [dtv[i].b], [dtv[i].b])
            S.ts("dve", dtv[i].t[:], dtv[i].t[:], 1.0, None, ALU.add, None, [dtv[i].b], [dtv[i].b])
            S.act(dtv[i].t[:], dtv[i].t[:], AF.Ln, [dtv[i].b], [dtv[i].b])
            S.tt("dve", dav[i].t[:], dtv[i].t[:], aneg.t[:, d, :], ALU.mult, [dtv[i].b, aneg.b], [dav[i].b])
            S.mm(pcb.t[:, 256:272], U.t[:, style, :], dav[i].t[:], True, True, [U.b, dav[i].b], [psmb])
            S.mm(pcb.t[:, 272:288], K0["ones_f"], dav[i].t[:], True, True, [dav[i].b], [psmb])
            S.cp("act", csb[i].t[:], pcb.t[:, 256:272], [psmb], [csb[i].b])
            S.tt("dve", wend[i].t[:], pcb.t[:, 272:288], csb[i].t[:], ALU.subtract, [psmb, csb[i].b], [wend[i].b])
            S.act(wend[i].t[:], wend[i].t[:], AF.Exp, [wend[i].b], [wend[i].b])
            S.act(etot[i].t[:], pcb.t[:, 272:288], AF.Exp, [psmb], [etot[i].b])
            for m in range(8):
                S.tr(ptx.t[:, m * 128:(m + 1) * 128], xcf[i].t[:, m, :], K0["ident_b"], [xcf[i].b], [ptx.b])
            S.cp("act", xtok[i].t[:].rearrange("p h c -> p (h c)"), ptx.t[:, :], [ptx.b], [xtok[i].b])
            S.tt("dve", xdt[i].t[:], xtok[i].t[:], dtv[i].t[:, :].unsqueeze(2).to_broadcast([128, 16, 64]), ALU.mult,
                 [xtok[i].b, dtv[i].b], [xdt[i].b])
            S.tt("pool", xdw[i].t[:], xdt[i].t[:], wend[i].t[:, :].unsqueeze(2).to_broadcast([128, 16, 64]), ALU.mult,
                 [xdt[i].b, wend[i].b], [xdw[i].b])
            for g in range(2):
                S.tr(ptb.t[:, g * 128:(g + 1) * 128], bcf[i].t[:, g, :], K0["ident_b"], [bcf[i].b], [ptb.b])
            S.cp("act", btok[i].t[:].rearrange("p g c -> p (g c)"), ptb.t[:, 0:256], [ptb.b], [btok[i].b])
            if full:
                for g in range(2):
                    S.mm(pcb.t[:, g * 128:(g + 1) * 128], bcf[i].t[:, g, :], bcf[i].t[:, 2 + g, :], True, True, [bcf[i].b], [pcb.b])
                    S.tt("dve", cbm[i].t[:, g, :], pcb.t[:, g * 128:(g + 1) * 128], U.t[:, style, :], ALU.mult, [pcb.b, U.b], [cbm[i].b])
                if last_dir:
                    S.dma("sp", yA[i].t[:], SC["Y"].rearrange("(h p) t -> p h t", p=64)[:, :, t0:t0 + 128], [], [yA[i].b])
                    S.dma("act", xh[i].t[:], SC["xc_own"].rearrange("(h p) t -> p h t", p=64)[:, :, t0:t0 + 128], [], [xh[i].b])
                S.tt("pool", dall[i].t[:], U.t[:, style, :].unsqueeze(1).to_broadcast([128, 16, 128]),
                     dav[i].t[:, :].unsqueeze(2).to_broadcast([128, 16, 128]), ALU.mult, [U.b, dav[i].b], [dall[i].b])
                for blk in range(4):
                    h0 = blk * 4
                    g = h0 // 8
                    k = blk % 2
                    S.mm(pab[k].t[:, :], K0["ones_f"], dall[i].t[:, h0:h0 + 4, :].rearrange("p h l -> p (h l)"), True, True,
                         [dall[i].b], [pab[k].b])
                    pab3 = pab[k].t[:, :].rearrange("p (h l) -> p h l", h=4)
                    S.tt("dve", dc[k].t[:].rearrange("p (h l) -> p h l", h=4), pab3,
                         csb[i].t[:, h0:h0 + 4].unsqueeze(2).to_broadcast([128, 4, 128]), ALU.subtract,
                         [pab[k].b, csb[i].b], [dc[k].b])
                    S.ts("pool", dc[k].t[:], dc[k].t[:], 0.0, None, ALU.min, None, [dc[k].b], [dc[k].b])
                    S.act(ee[k].t[:], dc[k].t[:], AF.Exp, [dc[k].b], [ee[k].b])
                    S.tt("pool", mp[k].t[:].rearrange("p (h l) -> p h l", h=4), ee[k].t[:].rearrange("p (h l) -> p h l", h=4),
                         cbm[i].t[:, g, :].unsqueeze(1).to_broadcast([128, 4, 128]), ALU.mult, [ee[k].b, cbm[i].b], [mp[k].b])
                    S.act(el[k].t[:], pab[k].t[:, :], AF.Exp, [pab[k].b], [el[k].b])
                    S.tt("dve", cs_[k].t[:].rearrange("p (h l) -> p h l", h=4), el[k].t[:].rearrange("p (h l) -> p h l", h=4),
                         bcf[i].t[:, 2 + g, :].unsqueeze(1).to_broadcast([128, 4, 128]), ALU.mult, [bcf[i].b, el[k].b], [cs_[k].b])
                    for hh in range(4):
                        hd = h0 + hh
                        yo = py[k].t[0:64, hh * 128:(hh + 1) * 128]
                        S.mm(yo, xdt[i].t[:, hd, :], mp[k].t[:, hh * 128:(hh + 1) * 128], True, False, [xdt[i].b, mp[k].b], [py[k].b])
                        S.mm(yo, Sb[d].t[:, hd, :], cs_[k].t[:, hh * 128:(hh + 1) * 128], False, True, [Sb[d].b, cs_[k].b], [py[k].b])
                    yv = ysb[i].t[:, h0:h0 + 4, :]
                    pv = py[k].t[0:64, :].rearrange("p (h t) -> p h t", h=4)
                    if last_dir:
                        S.tt("dve", yv, pv, yA[i].t[:, h0:h0 + 4, :], ALU.add, [py[k].b, yA[i].b], [ysb[i].b])
                    else:
                        S.cp("act", yv, pv, [py[k].b], [ysb[i].b])
                if last_dir:
                    S.tt("pool", yA[i].t[:], xh[i].t[:], dsum.t[:, :].unsqueeze(2).to_broadcast([64, 16, 128]), ALU.mult,
                         [xh[i].b, dsum.b, yA[i].b], [yA[i].b])
                    S.tt("dve", ysb[i].t[:], ysb[i].t[:], yA[i].t[:], ALU.add, [ysb[i].b, yA[i].b], [ysb[i].b])
                S.dma("sp", SC["Y"].rearrange("(h p) t -> p h t", p=64)[:, :, t0:t0 + 128], ysb[i].t[:], [ysb[i].b], [])
            for g in range(2):
                S.mm(pst.t[:, :], btok[i].t[:, g, :], xdw[i].t[:, g * 8:(g + 1) * 8, :].rearrange("p h c -> p (h c)"),
                     True, True, [btok[i].b, xdw[i].b], [pst.b])
                sv = St[d].t[:, g * 8:(g + 1) * 8, :]
                S.tt("dve", sv, sv, etot[i].t[:, g * 8:(g + 1) * 8].unsqueeze(2).to_broadcast([128, 8, 64]), ALU.mult,
                     [St[d].b, etot[i].b], [St[d].b])
                S.tt("dve", sv, sv, pst.t[:, :].rearrange("p (h c) -> p h c", h=8), ALU.add, [St[d].b, pst.b], [St[d].b])
                S.cp("pool", Sb[d].t[:, g * 8:(g + 1) * 8, :], sv, [St[d].b], [Sb[d].b])

        for d in range(2):
            S.memset("dve", St[d].t[:], 0.0, [St[d].b])
            S.memset("pool", Sb[d].t[:], 0.0, [Sb[d].b])
        for ci in range(2):
            chunk(0, "ctx", ci, 0, False, False)
        for ci in range(16):
            chunk(0, "own", ci, 0, True, False)
        for ci in (1, 0):
            chunk(1, "ctx", ci, 1, False, False)
        for ci in range(15, -1, -1):
            chunk(1, "par", ci, 1, False, False)
        for ci in range(15, -1, -1):
            chunk(1, "own", ci, 1, True, True)
        S.emit()


def phase_ssm_out(nc, I, K0, mvec, SC, xres):
    with ExitStack() as es:
        C = Ctx(nc, es)
        S = Sched(nc)
        wo = C.sb([128, 16, 1024], BF16, "swo")
        pv = C.sb([128, 3, 8], F32, "spv")
        yg = C.sb([128, 8, 512], F32, "yg")
        zg = C.sb([128, 8, 512], F32, "zg")
        vg_ = C.sb([128, 8, 512], F32, "vg")
        xg = C.sb([128, 8, 512], F32, "xg3")
        ys = C.sb([128, 8, 512], BF16, "ys")
        yc = C.sb([128, 8, 512], BF16, "yc")
        sq = [C.sb([128, 512], F32, "sq3") for _ in range(2)]
        rstd = C.sb([128, 512], F32, "rstd3")
        mean = C.sb([128, 512], F32, "mean3")
        var = C.sb([128, 512], F32, "var3")
        tmp = [C.sb([128, 512], F32, "tmp3") for _ in range(2)]
        ss = C.ps("ss3")
        sm = C.ps("sm3")
        pp = [C.ps("po3") for _ in range(2)]
        for kc in range(16):
            S.dma("pool", wo.t[:, kc, :], I["swo_r"][:, kc, :], [], [wo.b])
        S.dma("sp", pv.t[:], I["ssm_v"][:, :, :], [], [pv.b])
        n = 0
        for g in range(4):
            c0 = g * 512
            S.dma("sp", yg.t[:], SC["Y"].rearrange("(kc p) t -> p kc t", p=128)[:, :, c0:c0 + 512], [], [yg.b])
            S.dma("act", zg.t[:], SC["z"].rearrange("(kc p) t -> p kc t", p=128)[:, :, c0:c0 + 512], [], [zg.b])
            S.dma("sp", vg_.t[:], SC["vc"].rearrange("(kc p) t -> p kc t", p=128)[:, :, c0:c0 + 512], [], [vg_.b])
            S.dma("act", xg.t[:], I["x1o"].rearrange("(kc p) t -> p kc t", p=128)[:, :, c0:c0 + 512], [], [xg.b])
            S.act(zg.t[:], zg.t[:], AF.Silu, [zg.b], [zg.b])
            S.tt("dve", yg.t[:], yg.t[:], zg.t[:], ALU.mult, [yg.b, zg.b], [yg.b])
            for kc in range(8):
                s = sq[kc % 2]
                S.act(s.t[:], yg.t[:, kc, :], AF.Square, [yg.b], [s.b])
                S.mm(ss.t[:, :], K0["ones_f"], s.t[:], kc == 0, kc == 7, [s.b], [ss.b])
            emit_rstd(S, ss, rstd, 1024.0, 512)
            for kc in range(8):
                S.stt(ys.t[:, kc, :], yg.t[:, kc, :], pv.t[:, 0, kc:kc + 1], rstd.t[:], ALU.mult, ALU.mult,
                      [yg.b, rstd.b, pv.b], [ys.b])
            for kc in range(8):
                s = sq[kc % 2]
                S.mm(sm.t[:, :], K0["ones_f"], vg_.t[:, kc, :], kc == 0, kc == 7, [vg_.b], [sm.b])
                S.act(s.t[:], vg_.t[:, kc, :], AF.Square, [vg_.b], [s.b])
                S.mm(ss.t[:, :], K0["ones_f"], s.t[:], kc == 0, kc == 7, [s.b], [ss.b])
            S.act(mean.t[:], sm.t[:, :], AF.Identity, [sm.b], [mean.b], scale=1.0 / 1024.0)
            S.tt("dve", var.t[:], mean.t[:], mean.t[:], ALU.mult, [mean.b], [var.b])
            S.stt(var.t[:], ss.t[:, :], 1.0 / 1024.0, var.t[:], ALU.mult, ALU.subtract, [ss.b, var.b], [var.b])
            S.ts("dve", var.t[:], var.t[:], EPS, None, ALU.add, None, [var.b], [var.b])
            S.act(var.t[:], var.t[:], AF.Ln, [var.b], [var.b])
            S.act(var.t[:], var.t[:], AF.Exp, [var.b], [var.b], scale=-0.5)
            for kc in range(8):
                t = tmp[kc % 2]
                S.tt("pool", t.t[:], vg_.t[:, kc, :], mean.t[:], ALU.subtract, [vg_.b, mean.b], [t.b])
                S.stt(t.t[:], t.t[:], pv.t[:, 1, kc:kc + 1], var.t[:], ALU.mult, ALU.mult, [t.b, var.b, pv.b], [t.b])
                S.act(yc.t[:, kc, :], t.t[:], AF.Silu, [t.b, pv.b], [yc.b], bias=pv.t[:, 2, kc:kc + 1])
            for c in range(8):
                p = pp[n % 2]
                n += 1
                for kc in range(16):
                    src = ys if kc < 8 else yc
                    S.mm(p.t[:, :], wo.t[:, kc, c * 128:(c + 1) * 128], src.t[:, kc % 8, :], kc == 0, kc == 15,
                         [wo.b, src.b], [p.b])
                S.stt(xg.t[:, c, :], p.t[:, :], mvec.t[:, 0, 2, c:c + 1], xg.t[:, c, :], ALU.mult, ALU.add,
                      [p.b, xg.b], [xg.b])
            S.dma("sp", xres.rearrange("(kc p) t -> p kc t", p=128)[:, :, c0:c0 + 512], xg.t[:], [xg.b], [])
        S.emit()


def phase_final_norm(nc, I, K0, xres, out):
    with ExitStack() as es:
        C = Ctx(nc, es)
        S = Sched(nc)
        fg = C.sb([128, 8], F32, "fg")
        xg = [C.sb([128, 8, 512], F32, "xgf") for _ in range(2)]
        sq = [C.sb([128, 512], F32, "sqf") for _ in range(2)]
        rstd = C.sb([128, 512], F32, "rstdf")
        ss = C.ps("ssf")
        S.dma("sp", fg.t[:], I["fg_r"][:, :], [], [fg.b])
        for g in range(4):
            x = xg[g % 2]
            c0 = g * 512
            S.dma("sp", x.t[:], xres.rearrange("(kc p) t -> p kc t", p=128)[:, :, c0:c0 + 512], [], [x.b])
            for kc in range(8):
                s = sq[kc % 2]
                S.act(s.t[:], x.t[:, kc, :], AF.Square, [x.b], [s.b])
                S.mm(ss.t[:, :], K0["ones_f"], s.t[:], kc == 0, kc == 7, [s.b], [ss.b])
            emit_rstd(S, ss, rstd, 1024.0, 512)
            for kc in range(8):
                S.stt(x.t[:, kc, :], x.t[:, kc, :], fg.t[:, kc:kc + 1], rstd.t[:], ALU.mult, ALU.mult,
                      [x.b, rstd.b, fg.b], [x.b])
            S.dma("sp", out.rearrange("(kc p) t -> p kc t", p=128)[:, :, c0:c0 + 512], x.t[:], [x.b], [])
        S.emit()


def make_consts2():
    c = np.zeros((128, 2, 128), np.float32)
    t = np.arange(128)
    c[:, 0, :] = (t[:, None] <= t[None, :]).astype(np.float32)
    c[:, 1, :] = (t[:, None] >= t[None, :]).astype(np.float32)
    return c


def shared_inputs_l1(inp):
    sh = {}
    wm = np.asarray(inp["w_mod"], np.float32)
    sh["wmod_r"] = np.ascontiguousarray(wm.reshape(2, 8, 128, 6, 1024).transpose(0, 3, 2, 1, 4))
    bm = np.asarray(inp["b_mod"], np.float32)
    sh["bmod_r"] = np.ascontiguousarray(bm.reshape(2, 48, 128).transpose(0, 2, 1))
    ng = np.asarray(inp["norm_g"], np.float32)
    sh["ng_r"] = np.ascontiguousarray(ng.reshape(2, 2, 8, 128).transpose(0, 3, 1, 2))
    sh["consts"] = make_consts()
    sh["consts2"] = make_consts2()
    sh["swin_r"] = wr(inp["ssm_w_in"][0])
    sh["swo_r"] = wr(inp["ssm_w_out"][0])
    v = np.zeros((128, 3, 8), np.float32)
    v[:, 0, :] = fm(inp["ssm_norm_g"][0], 8)
    v[:, 1, :] = fm(inp["conf_ln_g"][0], 8)
    v[:, 2, :] = fm(inp["conf_ln_b"][0], 8)
    sh["ssm_v"] = v
    sh["cb_r"] = fm(inp["ssm_conv_b"][0], 12)
    sh["db_r"] = fm(inp["conf_dw_b"][0], 8)
    sh["fg_r"] = fm(inp["final_g"], 8)
    ds = np.asarray(inp["ssm_d"][0], np.float32)
    sh["dsk_r"] = np.ascontiguousarray(np.broadcast_to(ds[None], (64, 2, 16)))
    return sh


def core_inputs_l1(inp, sh, core):
    b, hf = core // 2, core % 2
    d = dict(sh)
    cc = np.zeros((128, 8, 2), np.float32)
    cc[:, :, 0] = fm(inp["c"][b], 8)
    cc[:, :, 1] = fm(inp["c_ctx"], 8)
    d["cc"] = cc
    cwt = np.asarray(inp["ssm_conv_w"][0], np.float32)
    own = cwt if hf == 0 else cwt[::-1]
    parf = own[::-1]
    cw = np.zeros((128, 12, 2, 5), np.float32)
    cw[:, :, 0, :] = own.T.reshape(12, 128, 5).transpose(1, 0, 2)
    cw[:, :, 1, :] = parf.T.reshape(12, 128, 5).transpose(1, 0, 2)
    d["cw_r"] = cw
    dwt = np.asarray(inp["conf_dw_w"][0], np.float32)
    dwl = dwt if hf == 0 else dwt[::-1]
    d["dw_r"] = np.ascontiguousarray(dwl.T.reshape(8, 128, 31).transpose(1, 0, 2))
    p = np.zeros((128, 2, 2, 16), np.float32)
    for ld in range(2):
        gd = hf if ld == 0 else 1 - hf
        p[:, ld, 0, :] = np.asarray(inp["ssm_dt_bias"][0][gd], np.float32)[None]
        p[:, ld, 1, :] = np.asarray(inp["ssm_a_log"][0][gd], np.float32)[None]
    d["ssm_p"] = p
    return d


L1_INPUT_SHAPES = dict(
    x1o=[1024, HALF], x1p=[1024, HALF], c1=[1024, CTX], cc=[128, 8, 2], wmod_r=[2, 6, 128, 8, 1024],
    bmod_r=[2, 128, 48], ng_r=[2, 128, 2, 8], consts=[128, 5, 128], consts2=[128, 2, 128],
    swin_r=[128, 8, SSM_IN], swo_r=[128, 16, 1024], ssm_v=[128, 3, 8], cw_r=[128, 12, 2, 5], cb_r=[128, 12],
    dw_r=[128, 8, 31], db_r=[128, 8], ssm_p=[128, 2, 2, 16], dsk_r=[64, 2, 16], fg_r=[128, 8])


def build_l1(with_moe=True, n_exp=N_EXP):
    nc = bass.Bass("TRN2", target_bir_lowering=False)
    shapes = dict(L1_INPUT_SHAPES)
    if with_moe:
        shapes.update(moe_input_shapes(1, n_exp))
    I = {k: dram(nc, k, v, F32, "ExternalInput") for k, v in shapes.items()}
    xres = dram(nc, "xres1", [1024, HALF], F32, "Internal" if with_moe else "ExternalOutput")
    out = dram(nc, "outT", [1024, HALF], F32, "ExternalOutput") if with_moe else None
    gt_scr = dram(nc, "gt_scr1", [32, 1152], F32, "Internal")
    SC = ssm_scratch(nc)
    with ExitStack() as es:
        init_sync(nc, es)
        K0 = load_consts(nc, es, I)
        C = Ctx(nc, es)
        mvec = C.sb([128, 2, 6, 8], F32, "mvec1")
        phase_mod(nc, I, 1, mvec)
        phase_ssm_proj(nc, I, K0, mvec, SC)
        phase_ssd(nc, I, K0, SC)
        phase_ssm_out(nc, I, K0, mvec, SC, xres)
        if with_moe:
            moe_layer(nc, I, K0, mvec, 1, xres, gt_scr, moe_passes_l1(), n_exp)
            phase_final_norm(nc, I, K0, xres, out)
    return nc


def fused_input_shapes(n_exp=N_EXP):
    shapes = dict(L0_INPUT_SHAPES)
    shapes.update(moe_input_shapes(0, n_exp))
    for k, v in L1_INPUT_SHAPES.items():
        if k not in ("x1o", "x1p", "c1"):
            shapes[k] = v
    shapes.update(moe_input_shapes(1, n_exp))
    return shapes


def build_fused(n_exp=N_EXP, stop_after=None):
    nc = bass.Bass("TRN2", target_bir_lowering=False)
    I = {k: dram(nc, k, v, F32, "ExternalInput") for k, v in fused_input_shapes(n_exp).items()}
    attn_o = dram(nc, "attn_o", [1024, NQ], BF16, "Internal")
    xres0 = dram(nc, "xres0", [1024, NQ], F32, "Internal")
    xres1 = dram(nc, "xres1", [1024, HALF], F32, "Internal")
    gt_scr = dram(nc, "gt_scr", [32, 1536], F32, "Internal")
    out = dram(nc, "outT", [1024, HALF], F32, "ExternalOutput")
    SC = ssm_scratch(nc)
    I["x1o"] = xres0[:, 0:HALF]
    I["x1p"] = xres0[:, HALF:SEQ]
    I["c1"] = xres0[:, SEQ:NQ]
    with ExitStack() as es:
        init_sync(nc, es)
        K0 = load_consts(nc, es, I)
        C = Ctx(nc, es)
        mvec = C.sb([128, 2, 6, 8], F32, "mvec")
        phase_mod(nc, I, 0, mvec)
        attention_layer(nc, I, K0, mvec, attn_o, xres0)
        moe_layer(nc, I, K0, mvec, 0, xres0, gt_scr, moe_passes_l0(), n_exp)
        phase_mod(nc, I, 1, mvec)
        phase_ssm_proj(nc, I, K0, mvec, SC)
        phase_ssd(nc, I, K0, SC)
        phase_ssm_out(nc, I, K0, mvec, SC, xres1)
        moe_layer(nc, I, K0, mvec, 1, xres1, gt_scr, moe_passes_l1(), n_exp)
        phase_final_norm(nc, I, K0, xres1, out)
    return nc


def fused_core_inputs(inp, sh, core, ropes):
    d = core_inputs_l0(inp, sh, core, ropes)
    d.update(core_inputs_l1(inp, sh, core))
    return d


def fused_shared_inputs(inp, n_exp=N_EXP):
    sh = shared_inputs(inp)
    sh.update(shared_inputs_l1(inp))
    sh.update(moe_shared_inputs(inp, 0, n_exp))
    sh.update(moe_shared_inputs(inp, 1, n_exp))
    return sh


def assemble_output(results):
    out = np.zeros((4, SEQ, D), np.float32)
    for c in range(8):
        b, hf = c // 2, c % 2
        y = np.asarray(results[c]["outT"]).T
        if hf == 0:
            out[b, :HALF] = y
        else:
            out[b, HALF:] = y[::-1]
    return out


def kernel(**inp):
    n = 8
    inp = {k: np.asarray(v) for k, v in inp.items()}
    sh = fused_shared_inputs(inp)
    ropes = [rope_tables(0), rope_tables(1)]
    nc = build_fused()
    maps = [fused_core_inputs(inp, sh, c, ropes) for c in range(n)]
    res = run_bass_kernel_spmd(nc, maps, core_ids=list(range(n)))
    return assemble_output(res.results)
```

```python
import numpy as np
from contextlib import ExitStack
import concourse.bass as bass
import concourse.mybir as mybir
from concourse.bass_utils import run_bass_kernel_spmd

F32 = mybir.dt.float32
BF16 = mybir.dt.bfloat16
U32 = mybir.dt.uint32
AF = mybir.ActivationFunctionType
ALU = mybir.AluOpType

D = 1024
SEQ = 4096
HALF = 2048
CTX = 256
NQ = SEQ + CTX
NK = CTX + SEQ
EPS = 1e-6
ATTN_IN = 1184
N_EXP = 32
SSM_IN = 4624


class Buf:
    __slots__ = ("w", "r", "owner")

    def __init__(self):
        self.w = None
        self.r = []
        self.owner = None


class Sched:
    ENGS = ("pe", "dve", "act", "pool", "sp")
    NDSEM = 28
    NHW = 16

    def __init__(self, nc, same_engine_sync=True):
        self.nc = nc
        self.ops = []
        self.same_engine_sync = same_engine_sync

    def op(self, eng, fn, reads=(), writes=(), dma=False):
        j = len(self.ops)
        deps = set()
        for b in list(reads) + list(writes):
            if b.owner is not self:
                b.owner = self
                b.w = None
                b.r = []
        for b in reads:
            if b.w is not None:
                deps.add(b.w)
        for b in writes:
            if b.w is not None:
                deps.add(b.w)
            deps.update(b.r)
        for b in reads:
            b.r.append(j)
        for b in writes:
            b.w = j
            b.r = []
        deps.discard(j)
        self.ops.append([eng, fn, deps, dma])
        return j

    def mm(self, out, lhsT, rhs, start, stop, r, w):
        self.op("pe", lambda e: e.matmul(out, lhsT, rhs, start=start, stop=stop), r, w)

    def tr(self, out, in_, ident, r, w):
        self.op("pe", lambda e: e.transpose(out, in_, ident), r, w)

    def act(self, out, in_, func, r, w, bias=None, scale=None):
        kw = {}
        if bias is not None:
            kw["bias"] = bias
        if scale is not None:
            kw["scale"] = scale
        self.op("act", lambda e: e.activation(out=out, in_=in_, func=func, **kw), r, w)

    def ts(self, eng, out, in0, s1, s2, op0, op1, r, w):
        if op1 is None:
            self.op(eng, lambda e: e.tensor_scalar(out, in0, s1, None, op0=op0), r, w)
        else:
            self.op(eng, lambda e: e.tensor_scalar(out, in0, s1, s2, op0=op0, op1=op1), r, w)

    def tt(self, eng, out, in0, in1, op, r, w):
        self.op(eng, lambda e: e.tensor_tensor(out, in0, in1, op), r, w)

    def stt(self, out, in0, scalar, in1, op0, op1, r, w):
        self.op("dve", lambda e: e.scalar_tensor_tensor(out, in0, scalar, in1, op0=op0, op1=op1), r, w)

    def cp(self, eng, out, in_, r, w):
        if eng == "act":
            self.op("act", lambda e: e.copy(out, in_), r, w)
        else:
            self.op(eng, lambda e: e.tensor_copy(out, in_), r, w)

    def memset(self, eng, ap, val, w):
        self.op(eng, lambda e: e.memset(ap, val), (), w)

    def dma(self, eng, out, in_, r, w, **kw):
        self.op(eng, lambda e: e.dma_start(out=out, in_=in_, **kw), r, w, dma=True)

    def emit(self):
        nc = self.nc
        G = GSYNC[id(nc)]
        ops = self.ops
        n = len(ops)
        if n == 0:
            return
        needs = [False] * n
        for j, (eng, fn, deps, dma) in enumerate(ops):
            for d in deps:
                de, _, _, ddma = ops[d]
                if ddma:
                    continue
                if de != eng or dma or (self.same_engine_sync and eng != "pe"):
                    needs[d] = True
        last_of = {}
        for j, o in enumerate(ops):
            if not o[3]:
                last_of[o[0]] = j
        for e_, j in last_of.items():
            needs[j] = True
        cnt0 = dict(G["cnt"])
        dlast0 = list(G["dlast"])
        cnt = G["cnt"]
        dlast = G["dlast"]
        val = [0] * n
        prev_same = {}
        for j, (eng, fn, deps, dma) in enumerate(ops):
            if dma:
                if eng == "pool":
                    s = self.NHW + G["dks"] % (self.NDSEM - self.NHW)
                    G["dks"] += 1
                else:
                    s = G["dk"] % self.NHW
                    G["dk"] += 1
                prev_same[j] = dlast[s]
                dlast[s] += 16
                val[j] = (s, dlast[s])
            elif needs[j]:
                cnt[eng] += 1
                val[j] = cnt[eng]
        streams = {e: [] for e in self.ENGS}
        for j, o in enumerate(ops):
            streams[o[0]].append(j)
        sss = self.same_engine_sync
        csem, dsem = G["csem"], G["dsem"]
        with ExitStack() as es:
            block = es.enter_context(nc.Block())

            def run_stream(ename, e):
                known_c = dict(cnt0)
                known_d = list(dlast0)
                for j in streams[ename]:
                    eng, fn, deps, dma = ops[j]
                    needc = {}
                    needd = {}
                    for d in deps:
                        de, _, _, ddma = ops[d]
                        if ddma:
                            s, v = val[d]
                            if v > needd.get(s, 0):
                                needd[s] = v
                        else:
                            if de == eng and not dma and (eng == "pe" or not sss):
                                continue
                            v = val[d]
                            if v > needc.get(de, 0):
                                needc[de] = v
                    if dma:
                        s = val[j][0]
                        v = prev_same[j]
                        if v > needd.get(s, 0):
                            needd[s] = v
                    for de, v in needc.items():
                        if known_c[de] < v:
                            e.wait_ge(csem[de], v)
                            known_c[de] = v
                    for s, v in needd.items():
                        if known_d[s] < v:
                            e.wait_ge(dsem[s], v)
                            known_d[s] = v
                    ins = fn(e)
                    if dma:
                        ins.then_inc(dsem[val[j][0]], 16)
                    elif needs[j]:
                        ins.then_inc(csem[eng], 1)
                for x in self.ENGS:
                    if cnt[x] > known_c[x]:
                        e.wait_ge(csem[x], cnt[x])
                for s in range(self.NDSEM):
                    if dlast[s] > known_d[s]:
                        e.wait_ge(dsem[s], dlast[s])

            @block.tensor
            def _(e):
                run_stream("pe", e)

            @block.vector
            def _(e):
                run_stream("dve", e)

            @block.scalar
            def _(e):
                run_stream("act", e)

            @block.gpsimd
            def _(e):
                run_stream("pool", e)

            @block.sync
            def _(e):
                run_stream("sp", e)


GSYNC = {}


def init_sync(nc, es):
    GSYNC.clear()
    GSYNC[id(nc)] = dict(
        csem={e: es.enter_context(nc.semaphore("c_" + e)) for e in Sched.ENGS},
        dsem=[es.enter_context(nc.semaphore("d_%d" % i)) for i in range(Sched.NDSEM)],
        cnt={e: 0 for e in Sched.ENGS}, dlast=[0] * Sched.NDSEM, dk=0, dks=0)


_uid = [0]


class Tl:
    __slots__ = ("t", "b")

    def __init__(self, t):
        self.t = t
        self.b = Buf()


class Ctx:
    def __init__(self, nc, es):
        self.nc = nc
        self.es = es
        self.n = 0

    def sb(self, shape, dt, name=None):
        _uid[0] += 1
        return Tl(self.es.enter_context(self.nc.sbuf_tensor("%s_%d" % (name or "s", _uid[0]), list(shape), dt)))

    def ps(self, name=None, shape=(128, 512), dt=F32):
        _uid[0] += 1
        return Tl(self.es.enter_context(self.nc.psum_tensor("%s_%d" % (name or "p", _uid[0]), list(shape), dt)))


def dram(nc, name, shape, dt, kind):
    return nc.dram_tensor(name, list(shape), dt, kind=kind).ap()


def emit_rstd(S, ssps, rstd, n_feat, T):
    S.ts("dve", rstd.t[:, :T], ssps.t[:, :T], 1.0 / n_feat, EPS, ALU.mult, ALU.add, [ssps.b], [rstd.b])
    S.act(rstd.t[:, :T], rstd.t[:, :T], AF.Ln, [rstd.b], [rstd.b])
    S.act(rstd.t[:, :T], rstd.t[:, :T], AF.Exp, [rstd.b], [rstd.b], scale=-0.5)


def emit_norm_mod(S, K, xg, T, A, SH, hb, hf=None):
    sq, ssps, rstd, tmp, ones = K["sq"], K["ssps"], K["rstd"], K["tmp"], K["ones"]
    for kc in range(8):
        s = sq[kc % 2]
        S.act(s.t[:, :T], xg.t[:, kc, :T], AF.Square, [xg.b], [s.b])
        S.mm(ssps.t[:, :T], ones[:, :], s.t[:, :T], kc == 0, kc == 7, [s.b], [ssps.b])
    emit_rstd(S, ssps, rstd, 1024.0, T)
    for kc in range(8):
        t = tmp[kc % 2]
        S.stt(t.t[:, :T], xg.t[:, kc, :T], A[:, kc:kc + 1], rstd.t[:, :T], ALU.mult, ALU.mult,
              [xg.b, rstd.b], [t.b])
        if hf is not None:
            S.act(hf.t[:, kc, :T], t.t[:, :T], AF.Identity, [t.b], [hf.b], bias=SH[:, kc:kc + 1])
            S.cp("pool", hb.t[:, kc, :T], hf.t[:, kc, :T], [hf.b], [hb.b])
        else:
            S.act(hb.t[:, kc, :T], t.t[:, :T], AF.Identity, [t.b], [hb.b], bias=SH[:, kc:kc + 1])


def norm_scratch(C, ones_f32):
    return dict(sq=[C.sb([128, 512], F32, "sq") for _ in range(2)], ssps=C.ps("ssps"),
                rstd=C.sb([128, 512], F32, "rstd"), tmp=[C.sb([128, 512], F32, "tmp") for _ in range(2)],
                ones=ones_f32)


def phase_mod(nc, I, li, mvec):
    with ExitStack() as es:
        C = Ctx(nc, es)
        S = Sched(nc)
        cc = C.sb([128, 8, 2], F32, "cc")
        sc = C.sb([128, 8, 2], F32, "sc")
        bm = C.sb([128, 48], F32, "bm")
        ng = C.sb([128, 2, 8], F32, "ng")
        mv = C.sb([128, 48, 2], F32, "mv")
        wm = [C.sb([128, 8, 1024], F32, "wm") for _ in range(2)]
        ps = C.ps("modps")
        S.dma("sp", cc.t[:], I["cc"][:, :, :], [], [cc.b])
        S.dma("sp", bm.t[:], I["bmod_r"][li], [], [bm.b])
        S.dma("sp", ng.t[:], I["ng_r"][li], [], [ng.b])
        S.act(sc.t[:], cc.t[:], AF.Silu, [cc.b], [sc.b])
        for m6 in range(6):
            w = wm[m6 % 2]
            S.dma("sp" if m6 % 2 == 0 else "act", w.t[:], I["wmod_r"][li, m6], [], [w.b])
            for mm_ in range(8):
                m = m6 * 8 + mm_
                for kc in range(8):
                    S.mm(ps.t[:, 2 * m:2 * m + 2], w.t[:, kc, mm_ * 128:(mm_ + 1) * 128], sc.t[:, kc, :],
                         kc == 0, kc == 7, [w.b, sc.b], [ps.b])
        ps3 = ps.t[:, 0:96].rearrange("p (m j) -> p m j", j=2)
        for j in range(2):
            S.tt("dve", mv.t[:, :, j], ps3[:, :, j], bm.t[:, :], ALU.add, [ps.b, bm.b], [mv.b])
        for j in range(2):
            def seg(k):
                return mv.t[:, k * 8:(k + 1) * 8, j]
            S.stt(mvec.t[:, j, 0, :], seg(1), 1.0, ng.t[:, 0, :], ALU.add, ALU.mult, [mv.b, ng.b], [mvec.b])
            S.cp("dve", mvec.t[:, j, 1, :], seg(0), [mv.b], [mvec.b])
            S.cp("dve", mvec.t[:, j, 2, :], seg(2), [mv.b], [mvec.b])
            S.stt(mvec.t[:, j, 3, :], seg(4), 1.0, ng.t[:, 1, :], ALU.add, ALU.mult, [mv.b, ng.b], [mvec.b])
            S.cp("dve", mvec.t[:, j, 4, :], seg(3), [mv.b], [mvec.b])
            S.cp("dve", mvec.t[:, j, 5, :], seg(5), [mv.b], [mvec.b])
        S.emit()


def load_consts(nc, es, I):
    C = Ctx(nc, es)
    cf = C.sb([128, 5, 128], F32, "constf")
    cb = C.sb([128, 2, 128], BF16, "constb")
    with ExitStack() as es2:
        S = Sched(nc)
        S.dma("sp", cf.t[:], I["consts"][:, :, :], [], [cf.b])
        S.dma("pool", cb.t[:, 0, :], I["consts"][:, 0, :], [], [cb.b])
        S.dma("pool", cb.t[:, 1, :], I["consts"][:, 1, :], [], [cb.b])
        S.emit()
    return dict(ident_f=cf.t[:, 0, :], ones_f=cf.t[:, 1, :], blk_f=cf.t[:, 2, :], pm128=cf.t[:, 3, :],
                pm96=cf.t[:, 4, :], ident_b=cb.t[:, 0, :], ones_b=cb.t[:, 1, :])


def attn_groups():
    g = [("ctx", CTX, "ctxT", 0, 0, SEQ)]
    for i in range(8):
        g.append(("own", 512, "xT", i * 512, CTX + i * 512, i * 512))
    return g


def phase_attn_proj(nc, I, K0, mvec, P):
    with ExitStack() as es:
        C = Ctx(nc, es)
        S = Sched(nc)
        win = C.sb([128, 8, ATTN_IN], BF16, "win")
        wkd = C.sb([128, 8, 2, 128], BF16, "wkd")
        gv = C.sb([128, 6], F32, "gv")
        xg = [C.sb([128, 8, 512], F32, "xg") for _ in range(1)]
        hb = [C.sb([128, 8, 512], BF16, "hb") for _ in range(2)]
        rp = [C.sb([128, 4, 512], F32, "rope") for _ in range(1)]
        cqf = C.sb([128, 2, 512], F32, "cqf")
        sq2 = [C.sb([128, 512], F32, "sq2") for _ in range(2)]
        rs2 = [C.sb([128, 512], F32, "rs2") for _ in range(2)]
        qn = [C.sb([128, 512], F32, "qn") for _ in range(2)]
        t1 = [C.sb([128, 512], F32, "t1") for _ in range(2)]
        t2 = [C.sb([128, 512], F32, "t2") for _ in range(2)]
        NS = norm_scratch(C, K0["ones_f"])
        pp = [C.ps("proj") for _ in range(2)]
        aux = C.ps("aux")
        pq = C.ps("pq")
        pv = C.ps("pv")
        for kc in range(8):
            S.dma("pool", win.t[:, kc, :], I["win_r"][:, kc, :], [], [win.b])
        S.dma("sp", gv.t[:], I["attn_g"][:, :], [], [gv.b])
        for kv in range(2):
            for hh in range(2):
                S.cp("pool", wkd.t[:, :, kv, hh * 64:(hh + 1) * 64], win.t[:, :, 928 + kv * 64:928 + (kv + 1) * 64],
                     [win.b], [wkd.b])
        ckvn, krope, cqn, qg, kgd, vg = P["ckvn"], P["krope"], P["cqn"], P["qg"], P["kgd"], P["vg"]
        S.memset("pool", vg.t[:, :, :, 64:128], 1.0, [vg.b])
        ppi = [0]

        def proj(lhs_fn, M, h, T):
            p = pp[ppi[0] % 2]
            ppi[0] += 1
            for kc in range(8):
                S.mm(p.t[:M, :T], lhs_fn(kc), h.t[:, kc, :T], kc == 0, kc == 7, [win.b, wkd.b, h.b], [p.b])
            return p

        cnt = [0]

        def headnorm_rope(p, T, gcol, lat, rpt, out_ap, out_b):
            i = cnt[0] % 2
            cnt[0] += 1
            S.act(sq2[i].t[:, :T], p.t[:, :T], AF.Square, [p.b], [sq2[i].b])
            S.mm(aux.t[:, :T], K0["blk_f"], sq2[i].t[:, :T], True, True, [sq2[i].b], [aux.b])
            emit_rstd(S, aux, rs2[i], 64.0, T)
            S.stt(qn[i].t[:, :T], p.t[:, :T], gv.t[:, gcol:gcol + 1], rs2[i].t[:, :T], ALU.mult, ALU.mult,
                  [p.b, rs2[i].b, gv.b], [qn[i].b])
            if lat:
                S.mm(pq.t[:, :T], K0["pm128"], qn[i].t[:, :T], True, True, [qn[i].b], [pq.b])
                S.tt("pool", t1[i].t[:, :T], qn[i].t[:, :T], rpt.t[:, 0, :T], ALU.mult, [qn[i].b, rpt.b], [t1[i].b])
                S.tt("dve", t2[i].t[:, :T], pq.t[:, :T], rpt.t[:, 1, :T], ALU.mult, [pq.b, rpt.b], [t2[i].b])
                S.tt("dve", out_ap, t1[i].t[:, :T], t2[i].t[:, :T], ALU.add, [t1[i].b, t2[i].b], [out_b])
            else:
                S.cp("dve", out_ap, qn[i].t[:, :T], [qn[i].b], [out_b])

        for gi, (kind, T, src, c0, kpos, qpos) in enumerate(attn_groups()):
            x = xg[0]
            h = hb[gi % 2]
            rpt = rp[0]
            lat = kind != "ctx"
            j = 0 if lat else 1
            S.dma("sp", x.t[:, :, :T], I[src].rearrange("(kc p) t -> p kc t", p=128)[:, :, c0:c0 + T], [], [x.b])
            if lat:
                S.dma("act", rpt.t[:, 0:2, :T], I["ropeg"][:, :, c0:c0 + T], [], [rpt.b])
                S.dma("act", rpt.t[:, 2:4, :T], I["ropem"][:, :, c0:c0 + T], [], [rpt.b])
            emit_norm_mod(S, NS, x, T, mvec.t[:, j, 0, :], mvec.t[:, j, 1, :], h)
            isq = qpos is not None
            if isq:
                for c in range(2):
                    p = proj(lambda kc, c=c: win.t[:, kc, c * 128:(c + 1) * 128], 128, h, T)
                    S.cp("act", cqf.t[:, c, :T], p.t[:, :T], [p.b], [cqf.b])
                    S.act(sq2[c].t[:, :T], p.t[:, :T], AF.Square, [p.b], [sq2[c].b])
                    S.mm(aux.t[:, :T], K0["ones_f"], sq2[c].t[:, :T], c == 0, c == 1, [sq2[c].b], [aux.b])
                emit_rstd(S, aux, rs2[0], 256.0, T)
                for c in range(2):
                    S.stt(cqn.t[:, c, qpos:qpos + T], cqf.t[:, c, :T], gv.t[:, c:c + 1], rs2[0].t[:, :T],
                          ALU.mult, ALU.mult, [cqf.b, rs2[0].b, gv.b], [cqn.b])
            p = proj(lambda kc: win.t[:, kc, 256:384], 128, h, T)
            S.act(sq2[0].t[:, :T], p.t[:, :T], AF.Square, [p.b], [sq2[0].b])
            S.mm(aux.t[:, :T], K0["ones_f"], sq2[0].t[:, :T], True, True, [sq2[0].b], [aux.b])
            emit_rstd(S, aux, rs2[1], 128.0, T)
            S.stt(ckvn.t[:, kpos:kpos + T], p.t[:, :T], gv.t[:, 2:3], rs2[1].t[:, :T], ALU.mult, ALU.mult,
                  [p.b, rs2[1].b, gv.b], [ckvn.b])
            p = proj(lambda kc: win.t[:, kc, 320:416], 96, h, T)
            if lat:
                S.cp("act", qn[0].t[:96, :T], p.t[:96, :T], [p.b], [qn[0].b])
                S.mm(pq.t[:96, :T], K0["pm96"][:96, :96], qn[0].t[:96, :T], True, True, [qn[0].b], [pq.b])
                S.tt("pool", t1[0].t[64:96, :T], qn[0].t[64:96, :T], rpt.t[64:96, 2, :T], ALU.mult,
                     [qn[0].b, rpt.b], [t1[0].b])
                S.tt("dve", t2[0].t[64:96, :T], pq.t[64:96, :T], rpt.t[64:96, 3, :T], ALU.mult,
                     [pq.b, rpt.b], [t2[0].b])
                S.tt("dve", krope.t[64:96, kpos:kpos + T], t1[0].t[64:96, :T], t2[0].t[64:96, :T], ALU.add,
                     [t1[0].b, t2[0].b], [krope.b])
            else:
                S.cp("act", krope.t[64:96, kpos:kpos + T], p.t[64:96, :T], [p.b], [krope.b])
            if isq:
                for c in range(4):
                    p = proj(lambda kc, c=c: win.t[:, kc, 416 + c * 128:416 + (c + 1) * 128], 128, h, T)
                    headnorm_rope(p, T, 3, lat, rpt, qg.t[:, c, qpos:qpos + T], qg.b)
            for kv in range(2):
                p = proj(lambda kc, kv=kv: wkd.t[:, kc, kv, :], 128, h, T)
                headnorm_rope(p, T, 4, lat, rpt, kgd.t[:, kv, kpos:kpos + T], kgd.b)
            for tt_ in range(T // 128):
                for kc in range(8):
                    S.mm(pv.t[:, 0:128], h.t[:, kc, tt_ * 128:(tt_ + 1) * 128], win.t[:, kc, 1056:1184],
                         kc == 0, kc == 7, [h.b, win.b], [pv.b])
                kt = kpos // 128 + tt_
                S.cp("act", vg.t[:, kt, :, 0:64], pv.t[:, 0:128].rearrange("p (a b) -> p a b", a=2), [pv.b], [vg.b])
        S.emit()


def phase_attn_core(nc, I, K0, P, attn_o):
    with ExitStack() as es:
        C = Ctx(nc, es)
        S = Sched(nc)
        wuq = C.sb([128, 2, 768], BF16, "wuq")
        wukv = C.sb([128, 1024], BF16, "wukv")
        KT = [C.sb([96, NK], BF16, "KT") for _ in range(2)]
        VH = [C.sb([128, 34, 128], BF16, "VH") for _ in range(2)]
        QT = [C.sb([96, NQ], BF16, "QT") for _ in range(2)]
        rp = [C.sb([96, 2, 512], F32, "ropq") for _ in range(2)]
        qf = [C.sb([96, 512], F32, "qf") for _ in range(2)]
        t1 = [C.sb([96, 512], F32, "t1") for _ in range(2)]
        t2 = [C.sb([96, 512], F32, "t2") for _ in range(2)]
        pT = [C.sb([128, 1024], BF16, "pT") for _ in range(3)]
        rs = [C.sb([64, 512], F32, "rs") for _ in range(2)]
        ot = [C.sb([64, 512], BF16, "ot") for _ in range(2)]
        sps = [C.ps("sps", (128, 1024)) for _ in range(3)]
        accO = [C.ps("accO") for _ in range(2)]
        gen = accO[0]
        pq = accO[1]
        ckvn, krope, cqn, qg, kgd, vg = P["ckvn"], P["krope"], P["cqn"], P["qg"], P["kgd"], P["vg"]
        for c in range(2):
            S.dma("pool", wuq.t[:, c, :], I["wuq_r"][:, c, :], [], [wuq.b])
        S.dma("pool", wukv.t[:, :], I["wukv"][:, :], [], [wukv.b])
        for v_ in VH:
            S.memset("pool", v_.t[:, :, 64:128], 1.0, [v_.b])
        kgroups = [(0, CTX)] + [(CTX + i * 512, 512) for i in range(8)]
        qgroups = [(i * 512, 512, 0, 34, True) for i in range(8)] + [(SEQ, CTX, 0, 2, False)]
        unit = [0]
        for hd in range(16):
            if hd < 8:
                h = hd
                kt_, vh_, qt_ = KT[h % 2], VH[h % 2], QT[h % 2]
                for (k0, T) in kgroups:
                    S.mm(gen.t[:64, :T], wukv.t[:, h * 128:h * 128 + 64], ckvn.t[:, k0:k0 + T], True, True,
                         [wukv.b, ckvn.b], [gen.b])
                    S.cp("act", kt_.t[0:64, k0:k0 + T], gen.t[:64, :T], [gen.b], [kt_.b])
                S.cp("pool", kt_.t[64:96, :], krope.t[64:96, :], [krope.b], [kt_.b])
                for t0 in range(0, 34, 4):
                    nt = min(4, 34 - t0)
                    for i in range(nt):
                        S.mm(gen.t[:, i * 64:(i + 1) * 64], ckvn.t[:, (t0 + i) * 128:(t0 + i + 1) * 128],
                             wukv.t[:, h * 128 + 64:h * 128 + 128], True, True, [wukv.b, ckvn.b], [gen.b])
                    S.cp("dve", vh_.t[:, t0:t0 + nt, 0:64], gen.t[:, 0:nt * 64].rearrange("p (a b) -> p a b", b=64),
                         [gen.b], [vh_.b])
                for gi, (q0, T, _, _, lat) in enumerate(qgroups):
                    for c in range(2):
                        S.mm(gen.t[:96, :T], wuq.t[:, c, h * 96:(h + 1) * 96], cqn.t[:, c, q0:q0 + T], c == 0, c == 1,
                             [wuq.b, cqn.b], [gen.b])
                    S.cp("act", qt_.t[0:64, q0:q0 + T], gen.t[0:64, :T], [gen.b], [qt_.b])
                    if lat:
                        i = gi % 2
                        S.dma("sp", rp[i].t[:, :, :T], I["ropem"][0:96, :, q0:q0 + T], [], [rp[i].b])
                        S.cp("act", qf[i].t[:96, :T], gen.t[:96, :T], [gen.b], [qf[i].b])
                        S.mm(pq.t[:96, :T], K0["pm96"][:96, :96], qf[i].t[:96, :T], True, True, [qf[i].b], [pq.b])
                        S.tt("pool", t1[i].t[64:96, :T], qf[i].t[64:96, :T], rp[i].t[64:96, 0, :T], ALU.mult,
                             [qf[i].b, rp[i].b], [t1[i].b])
                        S.tt("dve", t2[i].t[64:96, :T], pq.t[64:96, :T], rp[i].t[64:96, 1, :T], ALU.mult,
                             [pq.b, rp[i].b], [t2[i].b])
                        S.tt("dve", qt_.t[64:96, q0:q0 + T], t1[i].t[64:96, :T], t2[i].t[64:96, :T], ALU.add,
                             [t1[i].b, t2[i].b], [qt_.b])
                    else:
                        S.cp("dve", qt_.t[64:96, q0:q0 + T], gen.t[64:96, :T], [gen.b], [qt_.b])
                scale = 96.0 ** -0.5
                k_ap = lambda kt, kt_=kt_: kt_.t[0:96, kt * 128:(kt + 1) * 128]
                q_ap = lambda q0, T, qt_=qt_: qt_.t[0:96, q0:q0 + T]
                v_ap = lambda kt, vh_=vh_: vh_.t[:, kt, :]
                kb, qb, vb = kt_.b, qt_.b, vh_.b
            else:
                h = hd - 8
                hh, c, kv = h % 2, h // 2, h // 4
                scale = 0.125
                k_ap = lambda kt, hh=hh, kv=kv: kgd.t[hh * 64:(hh + 1) * 64, kv, kt * 128:(kt + 1) * 128]
                q_ap = lambda q0, T, hh=hh, c=c: qg.t[hh * 64:(hh + 1) * 64, c, q0:q0 + T]
                v_ap = lambda kt, kv=kv: vg.t[:, kt, kv, :]
                kb, qb, vb = kgd.b, qg.b, vg.b
            for (q0, T, ka, kbnd, lat) in qgroups:
                u = unit[0]
                unit[0] += 1
                ao = accO[u % 2]
                npair = (kbnd - ka) // 2

                def issue_s(pi):
                    sp_ = sps[pi % 3]
                    for a_ in range(2):
                        S.mm(sp_.t[:, a_ * 512:a_ * 512 + T], k_ap(ka + 2 * pi + a_), q_ap(q0, T), True, True,
                             [kb, qb], [sp_.b])

                issue_s(0)
                if npair > 1:
                    issue_s(1)
                for pi in range(npair):
                    sp_ = sps[pi % 3]
                    p_ = pT[pi % 3]
                    if pi + 2 < npair:
                        issue_s(pi + 2)
                    S.act(p_.t[:, :].rearrange("p (a t) -> p a t", a=2)[:, :, :T],
                          sp_.t[:, :].rearrange("p (a t) -> p a t", a=2)[:, :, :T], AF.Exp, [sp_.b], [p_.b], scale=scale)
                    for a_ in range(2):
                        kt = ka + 2 * pi + a_
                        S.mm(ao.t[:, :T], v_ap(kt), p_.t[:, a_ * 512:a_ * 512 + T], kt == ka, kt == kbnd - 1,
                             [vb, p_.b], [ao.b])
                r_ = rs[u % 2]
                o_ = ot[u % 2]
                S.op("dve", lambda e, r_=r_, ao=ao, T=T: e.reciprocal(r_.t[:64, :T], ao.t[64:128, :T]),
                     [ao.b], [r_.b])
                S.tt("dve", o_.t[:64, :T], ao.t[:64, :T], r_.t[:64, :T], ALU.mult, [ao.b, r_.b], [o_.b])
                S.dma("sp", attn_o[hd * 64:(hd + 1) * 64, q0:q0 + T], o_.t[:64, :T], [o_.b], [])
        S.emit()


def phase_attn_out(nc, I, mvec, attn_o, xres):
    with ExitStack() as es:
        C = Ctx(nc, es)
        S = Sched(nc)
        wo = C.sb([128, 8, 1024], BF16, "wo")
        og = [C.sb([128, 8, 512], BF16, "og") for _ in range(2)]
        xg = [C.sb([128, 8, 512], F32, "xg") for _ in range(2)]
        pp = [C.ps("op") for _ in range(2)]
        for kc in range(8):
            S.dma("pool", wo.t[:, kc, :], I["wout_r"][:, kc, :], [], [wo.b])
        groups = [("xT", i * 512, 512, i * 512, 0) for i in range(8)] + [("ctxT", 0, CTX, SEQ, 1)]
        n = 0
        for gi, (src, c0, T, q0, j) in enumerate(groups):
            x, o = xg[gi % 2], og[gi % 2]
            S.dma("sp", x.t[:, :, :T], I[src].rearrange("(kc p) t -> p kc t", p=128)[:, :, c0:c0 + T], [], [x.b])
            S.dma("act", o.t[:, :, :T], attn_o.rearrange("(kc p) t -> p kc t", p=128)[:, :, q0:q0 + T], [], [o.b])
            for c in range(8):
                p = pp[n % 2]
                n += 1
                for kc in range(8):
                    S.mm(p.t[:, :T], wo.t[:, kc, c * 128:(c + 1) * 128], o.t[:, kc, :T], kc == 0, kc == 7,
                         [wo.b, o.b], [p.b])
                S.stt(x.t[:, c, :T], p.t[:, :T], mvec.t[:, j, 2, c:c + 1], x.t[:, c, :T], ALU.mult, ALU.add,
                      [p.b, x.b], [x.b])
            S.dma("sp", xres.rearrange("(kc p) t -> p kc t", p=128)[:, :, q0:q0 + T], x.t[:, :, :T], [x.b], [])
        S.emit()


def attention_layer(nc, I, K0, mvec, attn_o, xres):
    with ExitStack() as es:
        C = Ctx(nc, es)
        P = dict(ckvn=C.sb([128, NK], BF16, "ckvn"), krope=C.sb([96, NK], BF16, "krope"),
                 cqn=C.sb([128, 2, NQ], BF16, "cqn"), qg=C.sb([128, 4, NQ], BF16, "qg"),
                 kgd=C.sb([128, 2, NK], BF16, "kgd"), vg=C.sb([128, 34, 2, 128], BF16, "vg"))
        phase_attn_proj(nc, I, K0, mvec, P)
        phase_attn_core(nc, I, K0, P, attn_o)
    phase_attn_out(nc, I, mvec, attn_o, xres)


def make_consts():
    c = np.zeros((128, 5, 128), np.float32)
    c[:, 0, :] = np.eye(128, dtype=np.float32)
    c[:, 1, :] = 1.0
    c[0:64, 2, 0:64] = 1.0
    c[64:128, 2, 64:128] = 1.0
    for i in range(64):
        c[2 * i + 1, 3, 2 * i] = -1.0
        c[2 * i, 3, 2 * i + 1] = 1.0
    for i in range(32, 48):
        c[2 * i + 1, 4, 2 * i] = -1.0
        c[2 * i, 4, 2 * i + 1] = 1.0
    return c


def rope_tables(hf):
    pos = np.arange(SEQ) if hf == 0 else np.arange(SEQ)[::-1]
    row = (pos // 64).astype(np.float32)
    col = (pos % 64).astype(np.float32)

    def ang(rot_dim):
        nf = rot_dim // 4
        inv = (np.float32(10000.0) ** (-np.arange(nf, dtype=np.float32) / np.float32(nf))).astype(np.float32)
        return np.concatenate([row[:, None] * inv, col[:, None] * inv], axis=-1).astype(np.float32)

    ag = ang(64)
    am = ang(32)
    ropeg = np.zeros((128, 2, SEQ), np.float32)
    ropem = np.zeros((128, 2, SEQ), np.float32)
    for p in range(128):
        a = ag[:, (p % 64) // 2]
        ropeg[p, 0] = np.cos(a)
        ropeg[p, 1] = np.sin(a)
    for p in range(64, 96):
        a = am[:, (p - 64) // 2]
        ropem[p, 0] = np.cos(a)
        ropem[p, 1] = np.sin(a)
    return ropeg, ropem


def fm(v, k):
    return np.ascontiguousarray(np.asarray(v, np.float32).reshape(k, 128).T)


def wr(w):
    w = np.asarray(w, np.float32)
    K, N = w.shape
    return np.ascontiguousarray(w.reshape(K // 128, 128, N).transpose(1, 0, 2))


def shared_inputs(inp):
    sh = {}
    wm = np.asarray(inp["w_mod"], np.float32)
    sh["wmod_r"] = np.ascontiguousarray(wm.reshape(2, 8, 128, 6, 1024).transpose(0, 3, 2, 1, 4))
    bm = np.asarray(inp["b_mod"], np.float32)
    sh["bmod_r"] = np.ascontiguousarray(bm.reshape(2, 48, 128).transpose(0, 2, 1))
    ng = np.asarray(inp["norm_g"], np.float32)
    sh["ng_r"] = np.ascontiguousarray(ng.reshape(2, 2, 8, 128).transpose(0, 3, 1, 2))
    sh["consts"] = make_consts()
    sh["win_r"] = wr(inp["attn_w_in"][0])
    ag = np.zeros((128, 6), np.float32)
    ag[:, 0:2] = fm(inp["mla_g_cq"][0], 2)
    ag[:, 2] = np.asarray(inp["mla_g_ckv"][0], np.float32)
    gq = np.asarray(inp["gqa_g_q"][0], np.float32)
    gk = np.asarray(inp["gqa_g_k"][0], np.float32)
    ag[:, 3] = np.concatenate([gq, gq])
    ag[:, 4] = np.concatenate([gk, gk])
    sh["attn_g"] = ag
    sh["wuq_r"] = wr(inp["mla_w_uq"][0])
    sh["wukv"] = np.ascontiguousarray(np.asarray(inp["mla_w_ukv"][0], np.float32))
    sh["wout_r"] = wr(inp["attn_w_out"][0])
    return sh


def core_inputs_l0(inp, sh, core, ropes):
    b, hf = core // 2, core % 2
    x = np.asarray(inp["x"][b], np.float32)
    if hf == 1:
        x = x[::-1]
    d = dict(sh)
    d["xT"] = np.ascontiguousarray(x.T)
    cx = np.asarray(inp["ctx"][b], np.float32)
    if hf == 1:
        cx = cx[::-1]
    d["ctxT"] = np.ascontiguousarray(cx.T)
    cc = np.zeros((128, 8, 2), np.float32)
    cc[:, :, 0] = fm(inp["c"][b], 8)
    cc[:, :, 1] = fm(inp["c_ctx"], 8)
    d["cc"] = cc
    d["ropeg"], d["ropem"] = ropes[hf]
    return d


L0_INPUT_SHAPES = dict(
    xT=[1024, SEQ], ctxT=[1024, CTX], cc=[128, 8, 2], wmod_r=[2, 6, 128, 8, 1024], bmod_r=[2, 128, 48],
    ng_r=[2, 128, 2, 8], consts=[128, 5, 128], ropeg=[128, 2, SEQ], ropem=[128, 2, SEQ],
    win_r=[128, 8, ATTN_IN], attn_g=[128, 6], wuq_r=[128, 2, 768], wukv=[128, 1024], wout_r=[128, 8, 1024])


def build_l0(with_moe=True, n_exp=N_EXP):
    nc = bass.Bass("TRN2", target_bir_lowering=False)
    shapes = dict(L0_INPUT_SHAPES)
    if with_moe:
        shapes.update(moe_input_shapes(0, n_exp))
    I = {k: dram(nc, k, v, F32, "ExternalInput") for k, v in shapes.items()}
    attn_o = dram(nc, "attn_o", [1024, NQ], BF16, "Internal")
    xres = dram(nc, "xres", [1024, NQ], F32, "ExternalOutput")
    gt_scr = dram(nc, "gt_scr0", [32, 1152], F32, "Internal")
    with ExitStack() as es:
        init_sync(nc, es)
        K0 = load_consts(nc, es, I)
        C = Ctx(nc, es)
        mvec = C.sb([128, 2, 6, 8], F32, "mvec")
        phase_mod(nc, I, 0, mvec)
        attention_layer(nc, I, K0, mvec, attn_o, xres)
        if with_moe:
            moe_layer(nc, I, K0, mvec, 0, xres, gt_scr, moe_passes_l0(), n_exp)
    return nc


AX = mybir.AxisListType


def moe_layer(nc, I, K0, mvec, li, xres, gt_scr, passes, n_exp=N_EXP, dbg=None):
    sfx = str(li)
    for groups in passes:
        TP = sum(g[1] for g in groups)
        pos = []
        a = 0
        for g in groups:
            pos.append(a)
            a += g[1]
        with ExitStack() as es:
            C = Ctx(nc, es)
            xp = C.sb([128, 8, TP], F32, "xp")
            hb = C.sb([128, 8, TP], BF16, "hbp")
            GT = C.sb([32, TP], F32, "GT")
            xres3 = xres.rearrange("(kc p) t -> p kc t", p=128)
            with ExitStack() as es1:
                C1 = Ctx(nc, es1)
                S = Sched(nc)
                NS = norm_scratch(C1, K0["ones_f"])
                hf = [C1.sb([128, 8, 512], F32, "hf") for _ in range(2)]
                wrt = C1.sb([128, 8, 32], F32, "wrt")
                brt = C1.sb([128, 32], F32, "brt")
                lg = [C1.sb([128, 32], F32, "lg") for _ in range(2)]
                mx = [C1.sb([128, 8], F32, "mx") for _ in range(2)]
                ngm = [C1.sb([128, 1], F32, "ngm") for _ in range(2)]
                ex = [C1.sb([128, 32], F32, "ex") for _ in range(2)]
                mk = [C1.sb([128, 32], F32, "mk") for _ in range(2)]
                sm = [C1.sb([128, 1], F32, "sm") for _ in range(2)]
                Gt = [C1.sb([128, 32], F32, "Gt") for _ in range(2)]
                lps = [C1.ps("lps") for _ in range(2)]
                tps = [C1.ps("tps") for _ in range(2)]
                S.dma("sp", wrt.t[:], I["wr_r" + sfx][:, :, :], [], [wrt.b])
                S.dma("sp", brt.t[:], I["br" + sfx][0:1, :].to_broadcast([128, 32]), [], [brt.b])
                xb = [Buf() for _ in groups]
                tile_i = 0
                for gi, (c0, T, j) in enumerate(groups):
                    p0 = pos[gi]
                    xv = Tl(xp.t[:, :, p0:p0 + T])
                    xv.b = xb[gi]
                    S.dma("sp", xv.t, xres3[:, :, c0:c0 + T], [], [xv.b])
                    hfv = hf[gi % 2]
                    hbv = Tl(hb.t[:, :, p0:p0 + T])
                    hbv.b = hb.b
                    emit_norm_mod(S, NS, xv, T, mvec.t[:, j, 3, :], mvec.t[:, j, 4, :], hbv, hfv)
                    for tt_ in range(T // 128):
                        i = tile_i % 2
                        tile_i += 1
                        for kc in range(8):
                            S.mm(lps[i].t[:, 0:32], hfv.t[:, kc, tt_ * 128:(tt_ + 1) * 128], wrt.t[:, kc, :],
                                 kc == 0, kc == 7, [hfv.b, wrt.b], [lps[i].b])
                        S.tt("dve", lg[i].t[:], lps[i].t[:, 0:32], brt.t[:], ALU.add, [lps[i].b, brt.b], [lg[i].b])
                        S.op("dve", lambda e, i=i: e.max(mx[i].t[:], lg[i].t[:]), [lg[i].b], [mx[i].b])
                        S.ts("dve", ngm[i].t[:], mx[i].t[:, 0:1], -1.0, None, ALU.mult, None, [mx[i].b], [ngm[i].b])
                        S.act(ex[i].t[:], lg[i].t[:], AF.Exp, [lg[i].b, ngm[i].b], [ex[i].b], bias=ngm[i].t[:, 0:1])
                        S.ts("dve", mk[i].t[:], lg[i].t[:], mx[i].t[:, 3:4], None, ALU.is_ge, None,
                             [lg[i].b, mx[i].b], [mk[i].b])
                        S.tt("dve", ex[i].t[:], ex[i].t[:], mk[i].t[:], ALU.mult, [ex[i].b, mk[i].b], [ex[i].b])
                        S.op("dve", lambda e, i=i: e.reduce_sum(sm[i].t[:], ex[i].t[:], axis=AX.X), [ex[i].b], [sm[i].b])
                        S.op("dve", lambda e, i=i: e.reciprocal(sm[i].t[:], sm[i].t[:]), [sm[i].b], [sm[i].b])
                        S.ts("dve", Gt[i].t[:], ex[i].t[:], sm[i].t[:, 0:1], None, ALU.mult, None,
                             [ex[i].b, sm[i].b], [Gt[i].b])
                        S.tr(tps[i].t[0:32, 0:128], Gt[i].t[:], K0["ident_f"], [Gt[i].b], [tps[i].b])
                        tcol = p0 + tt_ * 128
                        S.cp("act", GT.t[0:32, tcol:tcol + 128], tps[i].t[0:32, 0:128], [tps[i].b], [GT.b])
                S.dma("sp", gt_scr[:, 0:TP], GT.t[0:32, :], [GT.b], [])
                if dbg is not None:
                    S.dma("sp", dbg["hb"][:, :, 0:TP], hb.t[:, :, :], [hb.b], [])
                S.emit()
            with ExitStack() as es2:
                C2 = Ctx(nc, es2)
                S = Sched(nc)
                actb = C2.sb([128, 8, TP], BF16, "actb")
                wgu = [C2.sb([128, 8, 2, 128], BF16, "wgu") for _ in range(4)]
                wd = [C2.sb([128, 8, 128], BF16, "wd") for _ in range(3)]
                gbc = [C2.sb([128, TP], F32, "gbc") for _ in range(2)]
                bgu = C2.sb([128, n_exp, 16], F32, "bgu")
                bd = C2.sb([32, 1024], F32, "bd")
                g1 = [C2.sb([128, 512], F32, "g1") for _ in range(3)]
                s1 = [C2.sb([128, 512], F32, "s1") for _ in range(3)]
                u1 = [C2.sb([128, 512], F32, "u1") for _ in range(3)]
                v1 = [C2.sb([128, 512], F32, "v1") for _ in range(3)]
                gps = [C2.ps("gps") for _ in range(3)]
                ups = [C2.ps("ups") for _ in range(3)]
                dps = [C2.ps("dps") for _ in range(2)]
                S.dma("sp", bgu.t[:], I["bgu_r" + sfx][:, 0:n_exp, :], [], [bgu.b])
                S.dma("sp", bd.t[0:n_exp, :], I["bd" + sfx][0:n_exp, :], [], [bd.b])
                xb = [Buf() for _ in groups]
                ab = [[Buf() for _ in groups] for _ in range(8)]
                nu = 0
                nw = 0
                nd = 0
                npd = 0
                PFG, PFD = 3, 2

                def issue_gu(u):
                    if u < n_exp * 8:
                        w_ = wgu[u % 4]
                        S.dma("pool", w_.t[:].rearrange("p a b c -> p (a b c)"), I["wgu" + sfx][u // 8, u % 8], [], [w_.b])

                def issue_d(u):
                    if u < n_exp * 8:
                        w_ = wd[u % 3]
                        S.dma("pool", w_.t[:].rearrange("p a b -> p (a b)"), I["wd" + sfx][u // 8, u % 8], [], [w_.b])

                for u in range(PFG):
                    issue_gu(u)
                for u in range(PFD):
                    issue_d(u)
                for e in range(n_exp):
                    gb = gbc[e % 2]
                    S.dma("sp", gb.t[:], gt_scr[e:e + 1, 0:TP].to_broadcast([128, TP]), [], [gb.b])
                    S.act(gb.t[:], gb.t[:], AF.Identity, [gb.b], [gb.b], scale=1.0 / 1.702)
                    for j in range(8):
                        w = wgu[nw % 4]
                        issue_gu(nw + PFG)
                        nw += 1
                        for gi, (c0, T, jj) in enumerate(groups):
                            p0 = pos[gi]
                            i = nu % 3
                            nu += 1
                            for kc in range(8):
                                S.mm(gps[i].t[:, :T], w.t[:, kc, 0, :], hb.t[:, kc, p0:p0 + T], kc == 0, kc == 7,
                                     [w.b, hb.b], [gps[i].b])
                            for kc in range(8):
                                S.mm(ups[i].t[:, :T], w.t[:, kc, 1, :], hb.t[:, kc, p0:p0 + T], kc == 0, kc == 7,
                                     [w.b, hb.b], [ups[i].b])
                            S.ts("dve", g1[i].t[:, :T], gps[i].t[:, :T], bgu.t[:, e, j:j + 1], 7.0, ALU.add, ALU.min,
                                 [gps[i].b, bgu.b], [g1[i].b])
                            S.act(s1[i].t[:, :T], g1[i].t[:, :T], AF.Silu, [g1[i].b], [s1[i].b], scale=1.702)
                            S.act(u1[i].t[:, :T], ups[i].t[:, :T], AF.Identity, [ups[i].b, bgu.b], [u1[i].b],
                                  bias=bgu.t[:, e, 8 + j:9 + j])
                            S.ts("pool", u1[i].t[:, :T], u1[i].t[:, :T], 7.0, -7.0, ALU.min, ALU.max,
                                 [u1[i].b], [u1[i].b])
                            S.stt(v1[i].t[:, :T], u1[i].t[:, :T], 1.0, s1[i].t[:, :T], ALU.add, ALU.mult,
                                  [u1[i].b, s1[i].b], [v1[i].b])
                            S.tt("pool", actb.t[:, j, p0:p0 + T], v1[i].t[:, :T], gb.t[:, p0:p0 + T], ALU.mult,
                                 [v1[i].b, gb.b], [ab[j][gi]])
                    for c in range(8):
                        w = wd[nd % 3]
                        issue_d(nd + PFD)
                        nd += 1
                        for gi, (c0, T, jj) in enumerate(groups):
                            p0 = pos[gi]
                            p = dps[npd % 2]
                            npd += 1
                            for jc in range(8):
                                S.mm(p.t[:, :T], w.t[:, jc, :], actb.t[:, jc, p0:p0 + T], jc == 0, jc == 7,
                                     [w.b, ab[jc][gi]], [p.b])
                            S.stt(xp.t[:, c, p0:p0 + T], p.t[:, :T], mvec.t[:, jj, 5, c:c + 1], xp.t[:, c, p0:p0 + T],
                                  ALU.mult, ALU.add, [p.b, xb[gi]], [xb[gi]])
                for c in range(8):
                    for gi, (c0, T, jj) in enumerate(groups):
                        p0 = pos[gi]
                        p = dps[npd % 2]
                        npd += 1
                        S.mm(p.t[:, :T], bd.t[0:n_exp, c * 128:(c + 1) * 128], GT.t[0:n_exp, p0:p0 + T], True, True,
                             [bd.b, GT.b], [p.b])
                        S.stt(xp.t[:, c, p0:p0 + T], p.t[:, :T], mvec.t[:, jj, 5, c:c + 1], xp.t[:, c, p0:p0 + T],
                              ALU.mult, ALU.add, [p.b, xb[gi]], [xb[gi]])
                for gi, (c0, T, jj) in enumerate(groups):
                    p0 = pos[gi]
                    S.dma("sp", xres3[:, :, c0:c0 + T], xp.t[:, :, p0:p0 + T], [xb[gi]], [])
                if dbg is not None:
                    S.dma("sp", dbg["actb"][:, :, 0:TP], actb.t[:, :, :], [ab[j][gi] for j in range(8) for gi in range(len(groups))], [])
                    S.dma("sp", dbg["gbc"][:, 0:TP], gbc[(n_exp - 1) % 2].t[:, :], [gbc[(n_exp - 1) % 2].b], [])
                S.emit()


def moe_passes_l0():
    ps = []
    for p in range(2):
        ps.append([(p * 1536 + k * 512, 512, 0) for k in range(3)])
    ps.append([(3072, 512, 0), (3584, 512, 0), (SEQ, CTX, 1)])
    return ps


def moe_passes_l1():
    return [[(p * 1024, 512, 0), (p * 1024 + 512, 512, 0)] for p in range(2)]


def moe_shared_inputs(inp, li, n_exp=N_EXP):
    sh = {}
    s = str(li)
    sh["wr_r" + s] = wr(inp["moe_w_router"][li])
    sh["br" + s] = np.ascontiguousarray(np.asarray(inp["moe_b_router"][li], np.float32).reshape(1, 32))
    bg = np.asarray(inp["moe_b_gate_up"][li], np.float32)
    sh["bgu_r" + s] = np.ascontiguousarray(bg.reshape(32, 16, 128).transpose(2, 0, 1))
    sh["bd" + s] = np.ascontiguousarray(np.asarray(inp["moe_b_down"][li], np.float32))
    wg = np.asarray(inp["moe_w_gate_up"][li], np.float32)[:n_exp]
    sh["wgu" + s] = np.ascontiguousarray(
        wg.reshape(n_exp, 8, 128, 2, 8, 128).transpose(0, 4, 2, 1, 3, 5)).reshape(n_exp, 8, 128, 2048)
    wdn = np.asarray(inp["moe_w_down"][li], np.float32)[:n_exp]
    sh["wd" + s] = np.ascontiguousarray(
        wdn.reshape(n_exp, 8, 128, 8, 128).transpose(0, 3, 2, 1, 4)).reshape(n_exp, 8, 128, 1024)
    return sh


def moe_input_shapes(li, n_exp=N_EXP):
    s = str(li)
    return {"wr_r" + s: [128, 8, 32], "br" + s: [1, 32], "bgu_r" + s: [128, 32, 16], "bd" + s: [32, 1024],
            "wgu" + s: [n_exp, 8, 128, 2048], "wd" + s: [n_exp, 8, 128, 1024]}


def build_moe_test(n_exp, li=0):
    nc = bass.Bass("TRN2", target_bir_lowering=False)
    shapes = dict(cc=[128, 8, 2], wmod_r=[2, 6, 128, 8, 1024], bmod_r=[2, 128, 48], ng_r=[2, 128, 2, 8],
                  consts=[128, 5, 128], x1T=[1024, NQ])
    shapes.update(moe_input_shapes(li, n_exp))
    I = {k: dram(nc, k, v, F32, "ExternalInput") for k, v in shapes.items()}
    xres = dram(nc, "xres", [1024, NQ], F32, "ExternalOutput")
    gt_scr = dram(nc, "gt_scr", [32, 1152], F32, "ExternalOutput")
    dbg = dict(hb=dram(nc, "dbg_hb", [128, 8, 1152], BF16, "ExternalOutput"),
               actb=dram(nc, "dbg_actb", [128, 8, 1152], BF16, "ExternalOutput"),
               gbc=dram(nc, "dbg_gbc", [128, 1152], F32, "ExternalOutput"))
    with ExitStack() as es:
        init_sync(nc, es)
        K0 = load_consts(nc, es, I)
        C = Ctx(nc, es)
        mvec = C.sb([128, 2, 6, 8], F32, "mvec")
        with ExitStack() as es1:
            S = Sched(nc)
            S.dma("sp", xres[:, :], I["x1T"][:, :], [], [])
            S.emit()
        phase_mod(nc, I, li, mvec)
        moe_layer(nc, I, K0, mvec, li, xres, gt_scr, moe_passes_l0(), n_exp, dbg)
    return nc


def ssm_scratch(nc):
    d = {}
    def mk(name, shape, dt):
        d[name] = dram(nc, "s1_" + name, shape, dt, "Internal")
    mk("z", [1024, HALF], F32)
    mk("xc_own", [1024, HALF], BF16); mk("B_own", [256, HALF], BF16); mk("C_own", [256, HALF], BF16)
    mk("xc_par", [1024, HALF], BF16); mk("B_par", [256, HALF], BF16)
    mk("xc_ctx", [1024, CTX], BF16); mk("B_ctx", [256, CTX], BF16)
    mk("dt_own", [HALF, 16], F32); mk("dt_par", [HALF, 16], F32); mk("dt_ctx", [CTX, 16], F32)
    mk("vc", [1024, HALF], F32)
    mk("Y", [1024, HALF], F32)
    mk("mix", [2048, HALF], BF16)
    return d


def phase_ssm_proj(nc, I, K0, mvec, SC):
    with ExitStack() as es:
        C = Ctx(nc, es)
        S = Sched(nc)
        win = C.sb([128, 8, 2576], BF16, "swin")
        hb = {"own": C.sb([128, 8, HALF], BF16, "hbo"), "par": C.sb([128, 8, HALF], BF16, "hbp"),
              "ctx": C.sb([128, 8, CTX], BF16, "hbc")}
        xg = [C.sb([128, 8, 512], F32, "xg") for _ in range(1)]
        NS = norm_scratch(C, K0["ones_f"])
        cw = C.sb([128, 12, 2, 5], F32, "cw")
        cb = C.sb([128, 12], F32, "cb")
        dw = C.sb([128, 8, 31], F32, "dw")
        db = C.sb([128, 8], F32, "db")
        uext = [C.sb([128, HALF + 4], F32, "uext") for _ in range(1)]
        vext = [C.sb([128, HALF + 30], F32, "vext") for _ in range(1)]
        acc = [C.sb([128, 512], F32, "acc") for _ in range(2)]
        ob = [C.sb([128, 512], BF16, "ob") for _ in range(2)]
        of = [C.sb([128, 512], F32, "of") for _ in range(2)]
        sg = [C.sb([128, 512], F32, "sg") for _ in range(2)]
        hl = [C.sb([128, 16], F32, "hl") for _ in range(2)]
        dtt = [C.sb([128, 16], F32, "dtt") for _ in range(2)]
        pp = [C.ps("sproj") for _ in range(3)]
        ph = C.ps("shalo")
        pd = C.ps("sdt")
        for kc in range(8):
            S.dma("pool", win.t[:, kc, :], I["swin_r"][:, kc, 0:2576], [], [win.b], max_dma_last_dim=4096)
        S.dma("sp", cw.t[:], I["cw_r"][:, :, :, :], [], [cw.b])
        S.dma("sp", cb.t[:], I["cb_r"][:, :], [], [cb.b])
        S.dma("sp", dw.t[:], I["dw_r"][:, :, :], [], [dw.b])
        S.dma("sp", db.t[:], I["db_r"][:, :], [], [db.b])
        gi = 0
        for (name, src, ng, T, j) in [("own", "x1o", 4, 512, 0), ("par", "x1p", 4, 512, 0), ("ctx", "c1", 1, CTX, 1)]:
            for g in range(ng):
                x = xg[0]
                gi += 1
                S.dma("sp", x.t[:, :, :T], I[src].rearrange("(kc p) t -> p kc t", p=128)[:, :, g * 512:g * 512 + T],
                      [], [x.b])
                hv = Tl(hb[name].t[:, :, g * 512:g * 512 + T])
                hv.b = hb[name].b
                emit_norm_mod(S, NS, x, T, mvec.t[:, j, 0, :], mvec.t[:, j, 1, :], hv)
        npj = [0]

        def proj(col0, M, name, t0, T):
            p = pp[npj[0] % 3]
            npj[0] += 1
            for kc in range(8):
                S.mm(p.t[:M, :T], win.t[:, kc, col0:col0 + M], hb[name].t[:, kc, t0:t0 + T], kc == 0, kc == 7,
                     [win.b, hb[name].b], [p.b])
            return p

        n = [0]
        for m in range(8):
            for g in range(4):
                p = proj(m * 128, 128, "own", g * 512, 512)
                o = of[n[0] % 2]
                n[0] += 1
                S.cp("act", o.t[:, :], p.t[:, :512], [p.b], [o.b])
                S.dma("sp", SC["z"][m * 128:(m + 1) * 128, g * 512:(g + 1) * 512], o.t[:, :], [o.b], [])
        sets = [("own", 12, HALF, 0, "par"), ("par", 10, HALF, 0, "own"), ("ctx", 10, CTX, 0, None)]
        dst = {"own": ("xc_own", "B_own", "C_own"), "par": ("xc_par", "B_par", None), "ctx": ("xc_ctx", "B_ctx", None)}
        nu = 0
        for (name, nch, TT, frame, other) in sets:
            for m in range(nch):
                u = uext[0]
                nu += 1
                col0 = 1024 + m * 128
                if name == "par":
                    for kc in range(8):
                        S.mm(ph.t[:, 0:2], win.t[:, kc, col0:col0 + 128], hb["own"].t[:, kc, HALF - 2:HALF], kc == 0, kc == 7,
                             [win.b, hb["own"].b], [ph.b])
                    S.cp("act", u.t[:, 0:2], ph.t[:, 0:2], [ph.b], [u.b])
                else:
                    S.memset("pool", u.t[:, 0:2], 0.0, [u.b])
                for g in range(max(1, TT // 512)):
                    T = min(512, TT)
                    p = proj(col0, 128, name, g * 512, T)
                    S.cp("act", u.t[:, 2 + g * 512:2 + g * 512 + T], p.t[:, :T], [p.b], [u.b])
                if name != "own":
                    S.memset("pool", u.t[:, 2 + TT:4 + TT], 0.0, [u.b])
                else:
                    for kc in range(8):
                        S.mm(ph.t[:, 0:2], win.t[:, kc, col0:col0 + 128], hb["par"].t[:, kc, 0:2], kc == 0, kc == 7,
                             [win.b, hb["par"].b], [ph.b])
                    S.cp("act", u.t[:, 2 + TT:4 + TT], ph.t[:, 0:2], [ph.b], [u.b])
                for g in range(max(1, TT // 512)):
                    T = min(512, TT)
                    a = acc[n[0] % 2]
                    o = ob[n[0] % 2]
                    n[0] += 1
                    b0 = g * 512
                    S.ts("dve", a.t[:, :T], u.t[:, b0:b0 + T], cw.t[:, m, frame, 0:1], cb.t[:, m:m + 1], ALU.mult, ALU.add,
                         [u.b, cw.b, cb.b], [a.b])
                    for k in range(1, 5):
                        S.stt(a.t[:, :T], u.t[:, b0 + k:b0 + k + T], cw.t[:, m, frame, k:k + 1], a.t[:, :T], ALU.mult, ALU.add,
                              [u.b, a.b], [a.b])
                    S.act(o.t[:, :T], a.t[:, :T], AF.Silu, [a.b], [o.b])
                    if m < 8:
                        d_ = SC[dst[name][0]][m * 128:(m + 1) * 128, b0:b0 + T]
                    elif m < 10:
                        d_ = SC[dst[name][1]][(m - 8) * 128:(m - 7) * 128, b0:b0 + T]
                    else:
                        d_ = SC[dst[name][2]][(m - 10) * 128:(m - 9) * 128, b0:b0 + T]
                    S.dma("sp", d_, o.t[:, :T], [o.b], [])
        nt = 0
        for (name, TT) in [("own", HALF), ("par", HALF), ("ctx", CTX)]:
            for t_ in range(TT // 128):
                d_ = dtt[nt % 2]
                nt += 1
                for kc in range(8):
                    S.mm(pd.t[:, 0:16], hb[name].t[:, kc, t_ * 128:(t_ + 1) * 128], win.t[:, kc, 2560:2576], kc == 0, kc == 7,
                         [hb[name].b, win.b], [pd.b])
                S.cp("act", d_.t[:, :], pd.t[:, 0:16], [pd.b], [d_.b])
                S.dma("sp", SC["dt_" + name][t_ * 128:(t_ + 1) * 128, :], d_.t[:, :], [d_.b], [])
        for kc in range(8):
            S.dma("pool", win.t[:, kc, 0:2048], I["swin_r"][:, kc, 2576:4624], [], [win.b], max_dma_last_dim=4096)
        for m in range(8):
            v = vext[0]
            ca, cbb = m * 128, 1024 + m * 128
            S.memset("pool", v.t[:, 0:15], 0.0, [v.b])
            for g in range(4):
                pa = proj(ca, 128, "own", g * 512, 512)
                pb_ = proj(cbb, 128, "own", g * 512, 512)
                s_ = sg[n[0] % 2]
                n[0] += 1
                S.act(s_.t[:, :], pb_.t[:, :512], AF.Sigmoid, [pb_.b], [s_.b])
                S.tt("dve", v.t[:, 15 + g * 512:15 + (g + 1) * 512], pa.t[:, :512], s_.t[:, :], ALU.mult, [pa.b, s_.b], [v.b])
            h_ = hl[m % 2]
            for kc in range(8):
                S.mm(ph.t[:, 0:16], win.t[:, kc, ca:ca + 128], hb["par"].t[:, kc, 0:16], kc == 0, kc == 7,
                     [win.b, hb["par"].b], [ph.b])
            for kc in range(8):
                S.mm(ph.t[:, 16:32], win.t[:, kc, cbb:cbb + 128], hb["par"].t[:, kc, 0:16], kc == 0, kc == 7,
                     [win.b, hb["par"].b], [ph.b])
            S.act(h_.t[:, :], ph.t[:, 16:32], AF.Sigmoid, [ph.b], [h_.b])
            S.tt("dve", v.t[:, 15 + HALF:30 + HALF], ph.t[:, 0:15], h_.t[:, 0:15], ALU.mult, [ph.b, h_.b], [v.b])
            for g in range(4):
                a = acc[n[0] % 2]
                n[0] += 1
                b0 = g * 512
                S.ts("dve", a.t[:, :], v.t[:, b0:b0 + 512], dw.t[:, m, 0:1], db.t[:, m:m + 1], ALU.mult, ALU.add,
                     [v.b, dw.b, db.b], [a.b])
                for k in range(1, 31):
                    S.stt(a.t[:, :], v.t[:, b0 + k:b0 + k + 512], dw.t[:, m, k:k + 1], a.t[:, :], ALU.mult, ALU.add,
                          [v.b, a.b], [a.b])
                S.dma("sp", SC["vc"][m * 128:(m + 1) * 128, b0:b0 + 512], a.t[:, :], [a.b], [])
        S.emit()


def phase_ssd(nc, I, K0, SC):
    with ExitStack() as es:
        C = Ctx(nc, es)
        S = Sched(nc)
        U = C.sb([128, 2, 128], F32, "Umask")
        par = C.sb([128, 2, 2, 16], F32, "ssmp")
        aneg = C.sb([128, 2, 16], F32, "aneg")
        dsk = C.sb([64, 2, 16], F32, "dsk")
        dsum = C.sb([64, 16], F32, "dsum")
        St = [C.sb([128, 16, 64], F32, "St") for _ in range(2)]
        Sb = [C.sb([128, 16, 64], BF16, "Sb") for _ in range(2)]
        xcf = [C.sb([128, 8, 128], BF16, "xcf") for _ in range(2)]
        bcf = [C.sb([128, 4, 128], BF16, "bcf") for _ in range(2)]
        xtok = [C.sb([128, 16, 64], BF16, "xtok") for _ in range(2)]
        xdt = [C.sb([128, 16, 64], BF16, "xdt") for _ in range(2)]
        xdw = [C.sb([128, 16, 64], BF16, "xdw") for _ in range(2)]
        btok = [C.sb([128, 2, 128], BF16, "btok") for _ in range(2)]
        dtr = [C.sb([128, 16], F32, "dtr") for _ in range(2)]
        dtv = [C.sb([128, 16], F32, "dtv") for _ in range(2)]
        dav = [C.sb([128, 16], F32, "dav") for _ in range(2)]
        csb = [C.sb([128, 16], F32, "csb") for _ in range(2)]
        wend = [C.sb([128, 16], F32, "wend") for _ in range(2)]
        etot = [C.sb([128, 16], F32, "etot") for _ in range(2)]
        cbm = [C.sb([128, 2, 128], F32, "cbm") for _ in range(2)]
        dall = [C.sb([128, 16, 128], F32, "dall") for _ in range(2)]
        dc = [C.sb([128, 512], F32, "dc") for _ in range(2)]
        ee = [C.sb([128, 512], F32, "ee") for _ in range(2)]
        el = [C.sb([128, 512], F32, "el") for _ in range(2)]
        mp = [C.sb([128, 512], BF16, "mp") for _ in range(2)]
        cs_ = [C.sb([128, 512], BF16, "cs") for _ in range(2)]
        ysb = [C.sb([64, 16, 128], F32, "ysb") for _ in range(2)]
        yA = [C.sb([64, 16, 128], F32, "yA") for _ in range(2)]
        xh = [C.sb([64, 16, 128], BF16, "xh") for _ in range(2)]
        ptx = C.ps("ptx", (128, 1024), BF16)
        ptb = C.ps("ptb", (128, 1024), BF16)
        pcb = C.ps("pcb")
        psmb = Buf()
        pab = [C.ps("pab") for _ in range(2)]
        py = [C.ps("py") for _ in range(2)]
        pst = C.ps("pst")
        S.dma("sp", U.t[:], I["consts2"][:, :, :], [], [U.b])
        S.dma("sp", par.t[:], I["ssm_p"][:, :, :, :], [], [par.b])
        S.dma("sp", dsk.t[:], I["dsk_r"][:, :, :], [], [dsk.b])
        S.act(aneg.t[:], par.t[:, :, 1, :], AF.Exp, [par.b], [aneg.b])
        S.ts("dve", aneg.t[:], aneg.t[:], -1.0, None, ALU.mult, None, [aneg.b], [aneg.b])
        S.tt("dve", dsum.t[:], dsk.t[:, 0, :], dsk.t[:, 1, :], ALU.add, [dsk.b], [dsum.b])
        cnt = [0]

        def chunk(d, name, ci, style, full, last_dir):
            i = cnt[0] % 2
            cnt[0] += 1
            t0 = ci * 128
            S.dma("sp", dtr[i].t[:], SC["dt_" + name][t0:t0 + 128, :], [], [dtr[i].b])
            S.dma("act", xcf[i].t[:], SC["xc_" + name].rearrange("(m p) t -> p m t", p=128)[:, :, t0:t0 + 128], [], [xcf[i].b])
            S.dma("sp", bcf[i].t[:, 0:2, :], SC["B_" + name].rearrange("(g p) t -> p g t", p=128)[:, :, t0:t0 + 128], [], [bcf[i].b])
            if full:
                S.dma("sp", bcf[i].t[:, 2:4, :], SC["C_own"].rearrange("(g p) t -> p g t", p=128)[:, :, t0:t0 + 128], [], [bcf[i].b])
            S.tt("dve", dtv[i].t[:], dtr[i].t[:], par.t[:, d, 0, :], ALU.add, [dtr[i].b, par.b], [dtv[i].b])
            S.act(dtv[i].t[:], dtv[i].t[:], AF.Exp, [dtv[i].b], [dtv[i].b])
            S.ts("dve", dtv[i].t[:], dtv[i].t[:], 1.0, None, ALU.add, None, [dtv[i].b], [dtv[i].b])
            S.act(dtv[i].t[:], dtv[i].t[:], AF.Ln, [dtv[i].b], [dtv[i].b])
            S.tt("dve", dav[i].t[:], dtv[i].t[:], aneg.t[:, d, :], ALU.mult, [dtv[i].b, aneg.b], [dav[i].b])
            S.mm(pcb.t[:, 256:272], U.t[:, style, :], dav[i].t[:], True, True, [U.b, dav[i].b], [psmb])
            S.mm(pcb.t[:, 272:288], K0["ones_f"], dav[i].t[:], True, True, [dav[i].b], [psmb])
            S.cp("act", csb[i].t[:], pcb.t[:, 256:272], [psmb], [csb[i].b])
            S.tt("dve", wend[i].t[:], pcb.t[:, 272:288], csb[i].t[:], ALU.subtract, [psmb, csb[i].b], [wend[i].b])
            S.act(wend[i].t[:], wend[i].t[:], AF.Exp, [wend[i].b], [wend[i].b])
            S.act(etot[i].t[:], pcb.t[:, 272:288], AF.Exp, [psmb], [etot[i].b])
            for m in range(8):
                S.tr(ptx.t[:, m * 128:(m + 1) * 128], xcf[i].t[:, m, :], K0["ident_b"], [xcf[i].b], [ptx.b])
            S.cp("act", xtok[i].t[:].rearrange("p h c -> p (h c)"), ptx.t[:, :], [ptx.b], [xtok[i].b])
            S.tt("dve", xdt[i].t[:], xtok[i].t[:], dtv[i].t[:, :].unsqueeze(2).to_broadcast([128, 16, 64]), ALU.mult,
                 [xtok[i].b, dtv[i].b], [xdt[i].b])
            S.tt("dve", xdw[i].t[:], xdt[i].t[:], wend[i].t[:, :].unsqueeze(2).to_broadcast([128, 16, 64]), ALU.mult,
                 [xdt[i].b, wend[i].b], [xdw[i].b])
            for g in range(2):
                S.tr(ptb.t[:, g * 128:(g + 1) * 128], bcf[i].t[:, g, :], K0["ident_b"], [bcf[i].b], [ptb.b])
            S.cp("act", btok[i].t[:].rearrange("p g c -> p (g c)"), ptb.t[:, 0:256], [ptb.b], [btok[i].b])
            if full:
                for g in range(2):
                    S.mm(pcb.t[:, g * 128:(g + 1) * 128], bcf[i].t[:, g, :], bcf[i].t[:, 2 + g, :], True, True, [bcf[i].b], [pcb.b])
                    S.tt("dve", cbm[i].t[:, g, :], pcb.t[:, g * 128:(g + 1) * 128], U.t[:, style, :], ALU.mult, [pcb.b, U.b], [cbm[i].b])
                if last_dir:
                    S.dma("sp", yA[i].t[:], SC["Y"].rearrange("(h p) t -> p h t", p=64)[:, :, t0:t0 + 128], [], [yA[i].b])
                    S.dma("act", xh[i].t[:], SC["xc_own"].rearrange("(h p) t -> p h t", p=64)[:, :, t0:t0 + 128], [], [xh[i].b])
                S.tt("dve", dall[i].t[:], U.t[:, style, :].unsqueeze(1).to_broadcast([128, 16, 128]),
                     dav[i].t[:, :].unsqueeze(2).to_broadcast([128, 16, 128]), ALU.mult, [U.b, dav[i].b], [dall[i].b])
                for blk in range(4):
                    h0 = blk * 4
                    g = h0 // 8
                    k = blk % 2
                    S.mm(pab[k].t[:, :], K0["ones_f"], dall[i].t[:, h0:h0 + 4, :].rearrange("p h l -> p (h l)"), True, True,
                         [dall[i].b], [pab[k].b])
                    pab3 = pab[k].t[:, :].rearrange("p (h l) -> p h l", h=4)
                    S.tt("dve", dc[k].t[:].rearrange("p (h l) -> p h l", h=4), pab3,
                         csb[i].t[:, h0:h0 + 4].unsqueeze(2).to_broadcast([128, 4, 128]), ALU.subtract,
                         [pab[k].b, csb[i].b], [dc[k].b])
                    S.ts("dve", dc[k].t[:], dc[k].t[:], 0.0, None, ALU.min, None, [dc[k].b], [dc[k].b])
                    S.act(ee[k].t[:], dc[k].t[:], AF.Exp, [dc[k].b], [ee[k].b])
                    S.tt("dve", mp[k].t[:].rearrange("p (h l) -> p h l", h=4), ee[k].t[:].rearrange("p (h l) -> p h l", h=4),
                         cbm[i].t[:, g, :].unsqueeze(1).to_broadcast([128, 4, 128]), ALU.mult, [ee[k].b, cbm[i].b], [mp[k].b])
                    S.act(el[k].t[:], pab[k].t[:, :], AF.Exp, [pab[k].b], [el[k].b])
                    S.tt("dve", cs_[k].t[:].rearrange("p (h l) -> p h l", h=4), el[k].t[:].rearrange("p (h l) -> p h l", h=4),
                         bcf[i].t[:, 2 + g, :].unsqueeze(1).to_broadcast([128, 4, 128]), ALU.mult, [bcf[i].b, el[k].b], [cs_[k].b])
                    for hh in range(4):
                        hd = h0 + hh
                        yo = py[k].t[0:64, hh * 128:(hh + 1) * 128]
                        S.mm(yo, xdt[i].t[:, hd, :], mp[k].t[:, hh * 128:(hh + 1) * 128], True, False, [xdt[i].b, mp[k].b], [py[k].b])
                        S.mm(yo, Sb[d].t[:, hd, :], cs_[k].t[:, hh * 128:(hh + 1) * 128], False, True, [Sb[d].b, cs_[k].b], [py[k].b])
                    yv = ysb[i].t[:, h0:h0 + 4, :]
                    pv = py[k].t[0:64, :].rearrange("p (h t) -> p h t", h=4)
                    if last_dir:
                        S.tt("dve", yv, pv, yA[i].t[:, h0:h0 + 4, :], ALU.add, [py[k].b, yA[i].b], [ysb[i].b])
                    else:
                        S.cp("act", yv, pv, [py[k].b], [ysb[i].b])
                if last_dir:
                    S.tt("dve", yA[i].t[:], xh[i].t[:], dsum.t[:, :].unsqueeze(2).to_broadcast([64, 16, 128]), ALU.mult,
                         [xh[i].b, dsum.b, yA[i].b], [yA[i].b])
                    S.tt("dve", ysb[i].t[:], ysb[i].t[:], yA[i].t[:], ALU.add, [ysb[i].b, yA[i].b], [ysb[i].b])
                S.dma("sp", SC["Y"].rearrange("(h p) t -> p h t", p=64)[:, :, t0:t0 + 128], ysb[i].t[:], [ysb[i].b], [])
            for g in range(2):
                S.mm(pst.t[:, :], btok[i].t[:, g, :], xdw[i].t[:, g * 8:(g + 1) * 8, :].rearrange("p h c -> p (h c)"),
                     True, True, [btok[i].b, xdw[i].b], [pst.b])
                sv = St[d].t[:, g * 8:(g + 1) * 8, :]
                S.tt("dve", sv, sv, etot[i].t[:, g * 8:(g + 1) * 8].unsqueeze(2).to_broadcast([128, 8, 64]), ALU.mult,
                     [St[d].b, etot[i].b], [St[d].b])
                S.tt("dve", sv, sv, pst.t[:, :].rearrange("p (h c) -> p h c", h=8), ALU.add, [St[d].b, pst.b], [St[d].b])
                S.cp("act", Sb[d].t[:, g * 8:(g + 1) * 8, :], sv, [St[d].b], [Sb[d].b])

        for d in range(2):
            S.memset("dve", St[d].t[:], 0.0, [St[d].b])
            S.memset("pool", Sb[d].t[:], 0.0, [Sb[d].b])
        for ci in range(2):
            chunk(0, "ctx", ci, 0, False, False)
        for ci in range(16):
            chunk(0, "own", ci, 0, True, False)
        for ci in (1, 0):
            chunk(1, "ctx", ci, 1, False, False)
        for ci in range(15, -1, -1):
            chunk(1, "par", ci, 1, False, False)
        for ci in range(15, -1, -1):
            chunk(1, "own", ci, 1, True, True)
        S.emit()


def phase_ssm_out(nc, I, K0, mvec, SC, xres):
    with ExitStack() as es:
        C = Ctx(nc, es)
        S = Sched(nc)
        wo = C.sb([128, 16, 1024], BF16, "swo")
        pv = C.sb([128, 3, 8], F32, "spv")
        yg = C.sb([128, 8, 512], F32, "yg")
        zg = C.sb([128, 8, 512], F32, "zg")
        vg_ = C.sb([128, 8, 512], F32, "vg")
        xg = C.sb([128, 8, 512], F32, "xg3")
        ys = C.sb([128, 8, 512], BF16, "ys")
        yc = C.sb([128, 8, 512], BF16, "yc")
        sq = [C.sb([128, 512], F32, "sq3") for _ in range(2)]
        rstd = C.sb([128, 512], F32, "rstd3")
        mean = C.sb([128, 512], F32, "mean3")
        var = C.sb([128, 512], F32, "var3")
        tmp = [C.sb([128, 512], F32, "tmp3") for _ in range(2)]
        ss = C.ps("ss3")
        sm = C.ps("sm3")
        pp = [C.ps("po3") for _ in range(2)]
        for kc in range(16):
            S.dma("pool", wo.t[:, kc, :], I["swo_r"][:, kc, :], [], [wo.b])
        S.dma("sp", pv.t[:], I["ssm_v"][:, :, :], [], [pv.b])
        n = 0
        for g in range(4):
            c0 = g * 512
            S.dma("sp", yg.t[:], SC["Y"].rearrange("(kc p) t -> p kc t", p=128)[:, :, c0:c0 + 512], [], [yg.b])
            S.dma("act", zg.t[:], SC["z"].rearrange("(kc p) t -> p kc t", p=128)[:, :, c0:c0 + 512], [], [zg.b])
            S.dma("sp", vg_.t[:], SC["vc"].rearrange("(kc p) t -> p kc t", p=128)[:, :, c0:c0 + 512], [], [vg_.b])
            S.dma("act", xg.t[:], I["x1o"].rearrange("(kc p) t -> p kc t", p=128)[:, :, c0:c0 + 512], [], [xg.b])
            S.act(zg.t[:], zg.t[:], AF.Silu, [zg.b], [zg.b])
            S.tt("dve", yg.t[:], yg.t[:], zg.t[:], ALU.mult, [yg.b, zg.b], [yg.b])
            for kc in range(8):
                s = sq[kc % 2]
                S.act(s.t[:], yg.t[:, kc, :], AF.Square, [yg.b], [s.b])
                S.mm(ss.t[:, :], K0["ones_f"], s.t[:], kc == 0, kc == 7, [s.b], [ss.b])
            emit_rstd(S, ss, rstd, 1024.0, 512)
            for kc in range(8):
                S.stt(ys.t[:, kc, :], yg.t[:, kc, :], pv.t[:, 0, kc:kc + 1], rstd.t[:], ALU.mult, ALU.mult,
                      [yg.b, rstd.b, pv.b], [ys.b])
            for kc in range(8):
                s = sq[kc % 2]
                S.mm(sm.t[:, :], K0["ones_f"], vg_.t[:, kc, :], kc == 0, kc == 7, [vg_.b], [sm.b])
                S.act(s.t[:], vg_.t[:, kc, :], AF.Square, [vg_.b], [s.b])
                S.mm(ss.t[:, :], K0["ones_f"], s.t[:], kc == 0, kc == 7, [s.b], [ss.b])
            S.act(mean.t[:], sm.t[:, :], AF.Identity, [sm.b], [mean.b], scale=1.0 / 1024.0)
            S.tt("dve", var.t[:], mean.t[:], mean.t[:], ALU.mult, [mean.b], [var.b])
            S.stt(var.t[:], ss.t[:, :], 1.0 / 1024.0, var.t[:], ALU.mult, ALU.subtract, [ss.b, var.b], [var.b])
            S.ts("dve", var.t[:], var.t[:], EPS, None, ALU.add, None, [var.b], [var.b])
            S.act(var.t[:], var.t[:], AF.Ln, [var.b], [var.b])
            S.act(var.t[:], var.t[:], AF.Exp, [var.b], [var.b], scale=-0.5)
            for kc in range(8):
                t = tmp[kc % 2]
                S.tt("pool", t.t[:], vg_.t[:, kc, :], mean.t[:], ALU.subtract, [vg_.b, mean.b], [t.b])
                S.stt(t.t[:], t.t[:], pv.t[:, 1, kc:kc + 1], var.t[:], ALU.mult, ALU.mult, [t.b, var.b, pv.b], [t.b])
                S.act(yc.t[:, kc, :], t.t[:], AF.Silu, [t.b, pv.b], [yc.b], bias=pv.t[:, 2, kc:kc + 1])
            for c in range(8):
                p = pp[n % 2]
                n += 1
                for kc in range(16):
                    src = ys if kc < 8 else yc
                    S.mm(p.t[:, :], wo.t[:, kc, c * 128:(c + 1) * 128], src.t[:, kc % 8, :], kc == 0, kc == 15,
                         [wo.b, src.b], [p.b])
                S.stt(xg.t[:, c, :], p.t[:, :], mvec.t[:, 0, 2, c:c + 1], xg.t[:, c, :], ALU.mult, ALU.add,
                      [p.b, xg.b], [xg.b])
            S.dma("sp", xres.rearrange("(kc p) t -> p kc t", p=128)[:, :, c0:c0 + 512], xg.t[:], [xg.b], [])
        S.emit()


def phase_final_norm(nc, I, K0, xres, out):
    with ExitStack() as es:
        C = Ctx(nc, es)
        S = Sched(nc)
        fg = C.sb([128, 8], F32, "fg")
        xg = [C.sb([128, 8, 512], F32, "xgf") for _ in range(2)]
        sq = [C.sb([128, 512], F32, "sqf") for _ in range(2)]
        rstd = C.sb([128, 512], F32, "rstdf")
        ss = C.ps("ssf")
        S.dma("sp", fg.t[:], I["fg_r"][:, :], [], [fg.b])
        for g in range(4):
            x = xg[g % 2]
            c0 = g * 512
            S.dma("sp", x.t[:], xres.rearrange("(kc p) t -> p kc t", p=128)[:, :, c0:c0 + 512], [], [x.b])
            for kc in range(8):
                s = sq[kc % 2]
                S.act(s.t[:], x.t[:, kc, :], AF.Square, [x.b], [s.b])
                S.mm(ss.t[:, :], K0["ones_f"], s.t[:], kc == 0, kc == 7, [s.b], [ss.b])
            emit_rstd(S, ss, rstd, 1024.0, 512)
            for kc in range(8):
                S.stt(x.t[:, kc, :], x.t[:, kc, :], fg.t[:, kc:kc + 1], rstd.t[:], ALU.mult, ALU.mult,
                      [x.b, rstd.b, fg.b], [x.b])
            S.dma("sp", out.rearrange("(kc p) t -> p kc t", p=128)[:, :, c0:c0 + 512], x.t[:], [x.b], [])
        S.emit()


def make_consts2():
    c = np.zeros((128, 2, 128), np.float32)
    t = np.arange(128)
    c[:, 0, :] = (t[:, None] <= t[None, :]).astype(np.float32)
    c[:, 1, :] = (t[:, None] >= t[None, :]).astype(np.float32)
    return c


def shared_inputs_l1(inp):
    sh = {}
    wm = np.asarray(inp["w_mod"], np.float32)
    sh["wmod_r"] = np.ascontiguousarray(wm.reshape(2, 8, 128, 6, 1024).transpose(0, 3, 2, 1, 4))
    bm = np.asarray(inp["b_mod"], np.float32)
    sh["bmod_r"] = np.ascontiguousarray(bm.reshape(2, 48, 128).transpose(0, 2, 1))
    ng = np.asarray(inp["norm_g"], np.float32)
    sh["ng_r"] = np.ascontiguousarray(ng.reshape(2, 2, 8, 128).transpose(0, 3, 1, 2))
    sh["consts"] = make_consts()
    sh["consts2"] = make_consts2()
    sh["swin_r"] = wr(inp["ssm_w_in"][0])
    sh["swo_r"] = wr(inp["ssm_w_out"][0])
    v = np.zeros((128, 3, 8), np.float32)
    v[:, 0, :] = fm(inp["ssm_norm_g"][0], 8)
    v[:, 1, :] = fm(inp["conf_ln_g"][0], 8)
    v[:, 2, :] = fm(inp["conf_ln_b"][0], 8)
    sh["ssm_v"] = v
    sh["cb_r"] = fm(inp["ssm_conv_b"][0], 12)
    sh["db_r"] = fm(inp["conf_dw_b"][0], 8)
    sh["fg_r"] = fm(inp["final_g"], 8)
    ds = np.asarray(inp["ssm_d"][0], np.float32)
    sh["dsk_r"] = np.ascontiguousarray(np.broadcast_to(ds[None], (64, 2, 16)))
    return sh


def core_inputs_l1(inp, sh, core):
    b, hf = core // 2, core % 2
    d = dict(sh)
    cc = np.zeros((128, 8, 2), np.float32)
    cc[:, :, 0] = fm(inp["c"][b], 8)
    cc[:, :, 1] = fm(inp["c_ctx"], 8)
    d["cc"] = cc
    cwt = np.asarray(inp["ssm_conv_w"][0], np.float32)
    own = cwt if hf == 0 else cwt[::-1]
    parf = own[::-1]
    cw = np.zeros((128, 12, 2, 5), np.float32)
    cw[:, :, 0, :] = own.T.reshape(12, 128, 5).transpose(1, 0, 2)
    cw[:, :, 1, :] = parf.T.reshape(12, 128, 5).transpose(1, 0, 2)
    d["cw_r"] = cw
    dwt = np.asarray(inp["conf_dw_w"][0], np.float32)
    dwl = dwt if hf == 0 else dwt[::-1]
    d["dw_r"] = np.ascontiguousarray(dwl.T.reshape(8, 128, 31).transpose(1, 0, 2))
    p = np.zeros((128, 2, 2, 16), np.float32)
    for ld in range(2):
        gd = hf if ld == 0 else 1 - hf
        p[:, ld, 0, :] = np.asarray(inp["ssm_dt_bias"][0][gd], np.float32)[None]
        p[:, ld, 1, :] = np.asarray(inp["ssm_a_log"][0][gd], np.float32)[None]
    d["ssm_p"] = p
    return d


L1_INPUT_SHAPES = dict(
    x1o=[1024, HALF], x1p=[1024, HALF], c1=[1024, CTX], cc=[128, 8, 2], wmod_r=[2, 6, 128, 8, 1024],
    bmod_r=[2, 128, 48], ng_r=[2, 128, 2, 8], consts=[128, 5, 128], consts2=[128, 2, 128],
    swin_r=[128, 8, SSM_IN], swo_r=[128, 16, 1024], ssm_v=[128, 3, 8], cw_r=[128, 12, 2, 5], cb_r=[128, 12],
    dw_r=[128, 8, 31], db_r=[128, 8], ssm_p=[128, 2, 2, 16], dsk_r=[64, 2, 16], fg_r=[128, 8])


def build_l1(with_moe=True, n_exp=N_EXP):
    nc = bass.Bass("TRN2", target_bir_lowering=False)
    shapes = dict(L1_INPUT_SHAPES)
    if with_moe:
        shapes.update(moe_input_shapes(1, n_exp))
    I = {k: dram(nc, k, v, F32, "ExternalInput") for k, v in shapes.items()}
    xres = dram(nc, "xres1", [1024, HALF], F32, "Internal" if with_moe else "ExternalOutput")
    out = dram(nc, "outT", [1024, HALF], F32, "ExternalOutput") if with_moe else None
    gt_scr = dram(nc, "gt_scr1", [32, 1152], F32, "Internal")
    SC = ssm_scratch(nc)
    with ExitStack() as es:
        init_sync(nc, es)
        K0 = load_consts(nc, es, I)
        C = Ctx(nc, es)
        mvec = C.sb([128, 2, 6, 8], F32, "mvec1")
        phase_mod(nc, I, 1, mvec)
        phase_ssm_proj(nc, I, K0, mvec, SC)
        phase_ssd(nc, I, K0, SC)
        phase_ssm_out(nc, I, K0, mvec, SC, xres)
        if with_moe:
            moe_layer(nc, I, K0, mvec, 1, xres, gt_scr, moe_passes_l1(), n_exp)
            phase_final_norm(nc, I, K0, xres, out)
    return nc


def fused_input_shapes(n_exp=N_EXP):
    shapes = dict(L0_INPUT_SHAPES)
    shapes.update(moe_input_shapes(0, n_exp))
    for k, v in L1_INPUT_SHAPES.items():
        if k not in ("x1o", "x1p", "c1"):
            shapes[k] = v
    shapes.update(moe_input_shapes(1, n_exp))
    return shapes


def build_fused(n_exp=N_EXP, stop_after=None):
    nc = bass.Bass("TRN2", target_bir_lowering=False)
    I = {k: dram(nc, k, v, F32, "ExternalInput") for k, v in fused_input_shapes(n_exp).items()}
    attn_o = dram(nc, "attn_o", [1024, NQ], BF16, "Internal")
    xres0 = dram(nc, "xres0", [1024, NQ], F32, "Internal")
    xres1 = dram(nc, "xres1", [1024, HALF], F32, "Internal")
    gt_scr = dram(nc, "gt_scr", [32, 1536], F32, "Internal")
    out = dram(nc, "outT", [1024, HALF], F32, "ExternalOutput")
    SC = ssm_scratch(nc)
    I["x1o"] = xres0[:, 0:HALF]
    I["x1p"] = xres0[:, HALF:SEQ]
    I["c1"] = xres0[:, SEQ:NQ]
    with ExitStack() as es:
        init_sync(nc, es)
        K0 = load_consts(nc, es, I)
        C = Ctx(nc, es)
        mvec = C.sb([128, 2, 6, 8], F32, "mvec")
        phase_mod(nc, I, 0, mvec)
        attention_layer(nc, I, K0, mvec, attn_o, xres0)
        moe_layer(nc, I, K0, mvec, 0, xres0, gt_scr, moe_passes_l0(), n_exp)
        phase_mod(nc, I, 1, mvec)
        phase_ssm_proj(nc, I, K0, mvec, SC)
        phase_ssd(nc, I, K0, SC)
        phase_ssm_out(nc, I, K0, mvec, SC, xres1)
        moe_layer(nc, I, K0, mvec, 1, xres1, gt_scr, moe_passes_l1(), n_exp)
        phase_final_norm(nc, I, K0, xres1, out)
    return nc


def fused_core_inputs(inp, sh, core, ropes):
    d = core_inputs_l0(inp, sh, core, ropes)
    d.update(core_inputs_l1(inp, sh, core))
    return d


def fused_shared_inputs(inp, n_exp=N_EXP):
    sh = shared_inputs(inp)
    sh.update(shared_inputs_l1(inp))
    sh.update(moe_shared_inputs(inp, 0, n_exp))
    sh.update(moe_shared_inputs(inp, 1, n_exp))
    return sh


def assemble_output(results):
    out = np.zeros((4, SEQ, D), np.float32)
    for c in range(8):
        b, hf = c // 2, c % 2
        y = np.asarray(results[c]["outT"]).T
        if hf == 0:
            out[b, :HALF] = y
        else:
            out[b, HALF:] = y[::-1]
    return out


def kernel(**inp):
    n = 8
    inp = {k: np.asarray(v) for k, v in inp.items()}
    sh = fused_shared_inputs(inp)
    ropes = [rope_tables(0), rope_tables(1)]
    nc = build_fused()
    maps = [fused_core_inputs(inp, sh, c, ropes) for c in range(n)]
    res = run_bass_kernel_spmd(nc, maps, core_ids=list(range(n)))
    return assemble_output(res.results)
```

```python
import numpy as np
from contextlib import ExitStack
import concourse.bass as bass
import concourse.mybir as mybir
from concourse.bass_utils import run_bass_kernel_spmd

F32 = mybir.dt.float32
BF16 = mybir.dt.bfloat16
U32 = mybir.dt.uint32
AF = mybir.ActivationFunctionType
ALU = mybir.AluOpType

D = 1024
SEQ = 4096
HALF = 2048
CTX = 256
NQ = SEQ + CTX
NK = CTX + SEQ
EPS = 1e-6
ATTN_IN = 1184
N_EXP = 32
SSM_IN = 4624


class Buf:
    __slots__ = ("w", "r", "owner")

    def __init__(self):
        self.w = None
        self.r = []
        self.owner = None


class Sched:
    ENGS = ("pe", "dve", "act", "pool", "sp")
    NDSEM = 28
    NHW = 16

    def __init__(self, nc, same_engine_sync=True):
        self.nc = nc
        self.ops = []
        self.same_engine_sync = same_engine_sync

    def op(self, eng, fn, reads=(), writes=(), dma=False):
        j = len(self.ops)
        deps = set()
        for b in list(reads) + list(writes):
            if b.owner is not self:
                b.owner = self
                b.w = None
                b.r = []
        for b in reads:
            if b.w is not None:
                deps.add(b.w)
        for b in writes:
            if b.w is not None:
                deps.add(b.w)
            deps.update(b.r)
        for b in reads:
            b.r.append(j)
        for b in writes:
            b.w = j
            b.r = []
        deps.discard(j)
        self.ops.append([eng, fn, deps, dma])
        return j

    def mm(self, out, lhsT, rhs, start, stop, r, w):
        self.op("pe", lambda e: e.matmul(out, lhsT, rhs, start=start, stop=stop), r, w)

    def tr(self, out, in_, ident, r, w):
        self.op("pe", lambda e: e.transpose(out, in_, ident), r, w)

    def act(self, out, in_, func, r, w, bias=None, scale=None):
        kw = {}
        if bias is not None:
            kw["bias"] = bias
        if scale is not None:
            kw["scale"] = scale
        self.op("act", lambda e: e.activation(out=out, in_=in_, func=func, **kw), r, w)

    def ts(self, eng, out, in0, s1, s2, op0, op1, r, w):
        if op1 is None:
            self.op(eng, lambda e: e.tensor_scalar(out, in0, s1, None, op0=op0), r, w)
        else:
            self.op(eng, lambda e: e.tensor_scalar(out, in0, s1, s2, op0=op0, op1=op1), r, w)

    def tt(self, eng, out, in0, in1, op, r, w):
        self.op(eng, lambda e: e.tensor_tensor(out, in0, in1, op), r, w)

    def stt(self, out, in0, scalar, in1, op0, op1, r, w):
        self.op("dve", lambda e: e.scalar_tensor_tensor(out, in0, scalar, in1, op0=op0, op1=op1), r, w)

    def cp(self, eng, out, in_, r, w):
        if eng == "act":
            self.op("act", lambda e: e.copy(out, in_), r, w)
        else:
            self.op(eng, lambda e: e.tensor_copy(out, in_), r, w)

    def memset(self, eng, ap, val, w):
        self.op(eng, lambda e: e.memset(ap, val), (), w)

    def dma(self, eng, out, in_, r, w, **kw):
        self.op(eng, lambda e: e.dma_start(out=out, in_=in_, **kw), r, w, dma=True)

    def emit(self):
        nc = self.nc
        G = GSYNC[id(nc)]
        ops = self.ops
        n = len(ops)
        if n == 0:
            return
        needs = [False] * n
        for j, (eng, fn, deps, dma) in enumerate(ops):
            for d in deps:
                de, _, _, ddma = ops[d]
                if ddma:
                    continue
                if de != eng or dma or (self.same_engine_sync and eng != "pe"):
                    needs[d] = True
        last_of = {}
        for j, o in enumerate(ops):
            if not o[3]:
                last_of[o[0]] = j
        for e_, j in last_of.items():
            needs[j] = True
        cnt0 = dict(G["cnt"])
        dlast0 = list(G["dlast"])
        cnt = G["cnt"]
        dlast = G["dlast"]
        val = [0] * n
        prev_same = {}
        for j, (eng, fn, deps, dma) in enumerate(ops):
            if dma:
                if eng == "pool":
                    s = self.NHW + G["dks"] % (self.NDSEM - self.NHW)
                    G["dks"] += 1
                else:
                    s = G["dk"] % self.NHW
                    G["dk"] += 1
                prev_same[j] = dlast[s]
                dlast[s] += 16
                val[j] = (s, dlast[s])
            elif needs[j]:
                cnt[eng] += 1
                val[j] = cnt[eng]
        streams = {e: [] for e in self.ENGS}
        for j, o in enumerate(ops):
            streams[o[0]].append(j)
        sss = self.same_engine_sync
        csem, dsem = G["csem"], G["dsem"]
        with ExitStack() as es:
            block = es.enter_context(nc.Block())

            def run_stream(ename, e):
                known_c = dict(cnt0)
                known_d = list(dlast0)
                for j in streams[ename]:
                    eng, fn, deps, dma = ops[j]
                    needc = {}
                    needd = {}
                    for d in deps:
                        de, _, _, ddma = ops[d]
                        if ddma:
                            s, v = val[d]
                            if v > needd.get(s, 0):
                                needd[s] = v
                        else:
                            if de == eng and not dma and (eng == "pe" or not sss):
                                continue
                            v = val[d]
                            if v > needc.get(de, 0):
                                needc[de] = v
                    if dma:
                        s = val[j][0]
                        v = prev_same[j]
                        if v > needd.get(s, 0):
                            needd[s] = v
                    for de, v in needc.items():
                        if known_c[de] < v:
                            e.wait_ge(csem[de], v)
                            known_c[de] = v
                    for s, v in needd.items():
                        if known_d[s] < v:
                            e.wait_ge(dsem[s], v)
                            known_d[s] = v
                    ins = fn(e)
                    if dma:
                        ins.then_inc(dsem[val[j][0]], 16)
                    elif needs[j]:
                        ins.then_inc(csem[eng], 1)
                for x in self.ENGS:
                    if cnt[x] > known_c[x]:
                        e.wait_ge(csem[x], cnt[x])
                for s in range(self.NDSEM):
                    if dlast[s] > known_d[s]:
                        e.wait_ge(dsem[s], dlast[s])

            @block.tensor
            def _(e):
                run_stream("pe", e)

            @block.vector
            def _(e):
                run_stream("dve", e)

            @block.scalar
            def _(e):
                run_stream("act", e)

            @block.gpsimd
            def _(e):
                run_stream("pool", e)

            @block.sync
            def _(e):
                run_stream("sp", e)


GSYNC = {}


def init_sync(nc, es):
    GSYNC.clear()
    GSYNC[id(nc)] = dict(
        csem={e: es.enter_context(nc.semaphore("c_" + e)) for e in Sched.ENGS},
        dsem=[es.enter_context(nc.semaphore("d_%d" % i)) for i in range(Sched.NDSEM)],
        cnt={e: 0 for e in Sched.ENGS}, dlast=[0] * Sched.NDSEM, dk=0, dks=0)


_uid = [0]


class Tl:
    __slots__ = ("t", "b")

    def __init__(self, t):
        self.t = t
        self.b = Buf()


class Ctx:
    def __init__(self, nc, es):
        self.nc = nc
        self.es = es
        self.n = 0

    def sb(self, shape, dt, name=None):
        _uid[0] += 1
        return Tl(self.es.enter_context(self.nc.sbuf_tensor("%s_%d" % (name or "s", _uid[0]), list(shape), dt)))

    def ps(self, name=None, shape=(128, 512), dt=F32):
        _uid[0] += 1
        return Tl(self.es.enter_context(self.nc.psum_tensor("%s_%d" % (name or "p", _uid[0]), list(shape), dt)))


def dram(nc, name, shape, dt, kind):
    return nc.dram_tensor(name, list(shape), dt, kind=kind).ap()


def emit_rstd(S, ssps, rstd, n_feat, T):
    S.ts("dve", rstd.t[:, :T], ssps.t[:, :T], 1.0 / n_feat, EPS, ALU.mult, ALU.add, [ssps.b], [rstd.b])
    S.act(rstd.t[:, :T], rstd.t[:, :T], AF.Ln, [rstd.b], [rstd.b])
    S.act(rstd.t[:, :T], rstd.t[:, :T], AF.Exp, [rstd.b], [rstd.b], scale=-0.5)


def emit_norm_mod(S, K, xg, T, A, SH, hb, hf=None):
    sq, ssps, rstd, tmp, ones = K["sq"], K["ssps"], K["rstd"], K["tmp"], K["ones"]
    for kc in range(8):
        s = sq[kc % 2]
        S.act(s.t[:, :T], xg.t[:, kc, :T], AF.Square, [xg.b], [s.b])
        S.mm(ssps.t[:, :T], ones[:, :], s.t[:, :T], kc == 0, kc == 7, [s.b], [ssps.b])
    emit_rstd(S, ssps, rstd, 1024.0, T)
    for kc in range(8):
        t = tmp[kc % 2]
        S.stt(t.t[:, :T], xg.t[:, kc, :T], A[:, kc:kc + 1], rstd.t[:, :T], ALU.mult, ALU.mult,
              [xg.b, rstd.b], [t.b])
        if hf is not None:
            S.act(hf.t[:, kc, :T], t.t[:, :T], AF.Identity, [t.b], [hf.b], bias=SH[:, kc:kc + 1])
            S.cp("pool", hb.t[:, kc, :T], hf.t[:, kc, :T], [hf.b], [hb.b])
        else:
            S.act(hb.t[:, kc, :T], t.t[:, :T], AF.Identity, [t.b], [hb.b], bias=SH[:, kc:kc + 1])


def norm_scratch(C, ones_f32):
    return dict(sq=[C.sb([128, 512], F32, "sq") for _ in range(2)], ssps=C.ps("ssps"),
                rstd=C.sb([128, 512], F32, "rstd"), tmp=[C.sb([128, 512], F32, "tmp") for _ in range(2)],
                ones=ones_f32)


def phase_mod(nc, I, li, mvec):
    with ExitStack() as es:
        C = Ctx(nc, es)
        S = Sched(nc)
        cc = C.sb([128, 8, 2], F32, "cc")
        sc = C.sb([128, 8, 2], F32, "sc")
        bm = C.sb([128, 48], F32, "bm")
        ng = C.sb([128, 2, 8], F32, "ng")
        mv = C.sb([128, 48, 2], F32, "mv")
        wm = [C.sb([128, 8, 1024], F32, "wm") for _ in range(2)]
        ps = C.ps("modps")
        S.dma("sp", cc.t[:], I["cc"][:, :, :], [], [cc.b])
        S.dma("sp", bm.t[:], I["bmod_r"][li], [], [bm.b])
        S.dma("sp", ng.t[:], I["ng_r"][li], [], [ng.b])
        S.act(sc.t[:], cc.t[:], AF.Silu, [cc.b], [sc.b])
        for m6 in range(6):
            w = wm[m6 % 2]
            S.dma("sp" if m6 % 2 == 0 else "act", w.t[:], I["wmod_r"][li, m6], [], [w.b])
            for mm_ in range(8):
                m = m6 * 8 + mm_
                for kc in range(8):
                    S.mm(ps.t[:, 2 * m:2 * m + 2], w.t[:, kc, mm_ * 128:(mm_ + 1) * 128], sc.t[:, kc, :],
                         kc == 0, kc == 7, [w.b, sc.b], [ps.b])
        ps3 = ps.t[:, 0:96].rearrange("p (m j) -> p m j", j=2)
        for j in range(2):
            S.tt("dve", mv.t[:, :, j], ps3[:, :, j], bm.t[:, :], ALU.add, [ps.b, bm.b], [mv.b])
        for j in range(2):
            def seg(k):
                return mv.t[:, k * 8:(k + 1) * 8, j]
            S.stt(mvec.t[:, j, 0, :], seg(1), 1.0, ng.t[:, 0, :], ALU.add, ALU.mult, [mv.b, ng.b], [mvec.b])
            S.cp("dve", mvec.t[:, j, 1, :], seg(0), [mv.b], [mvec.b])
            S.cp("dve", mvec.t[:, j, 2, :], seg(2), [mv.b], [mvec.b])
            S.stt(mvec.t[:, j, 3, :], seg(4), 1.0, ng.t[:, 1, :], ALU.add, ALU.mult, [mv.b, ng.b], [mvec.b])
            S.cp("dve", mvec.t[:, j, 4, :], seg(3), [mv.b], [mvec.b])
            S.cp("dve", mvec.t[:, j, 5, :], seg(5), [mv.b], [mvec.b])
        S.emit()


def load_consts(nc, es, I):
    C = Ctx(nc, es)
    cf = C.sb([128, 5, 128], F32, "constf")
    cb = C.sb([128, 2, 128], BF16, "constb")
    with ExitStack() as es2:
        S = Sched(nc)
        S.dma("sp", cf.t[:], I["consts"][:, :, :], [], [cf.b])
        S.dma("pool", cb.t[:, 0, :], I["consts"][:, 0, :], [], [cb.b])
        S.dma("pool", cb.t[:, 1, :], I["consts"][:, 1, :], [], [cb.b])
        S.emit()
    return dict(ident_f=cf.t[:, 0, :], ones_f=cf.t[:, 1, :], blk_f=cf.t[:, 2, :], pm128=cf.t[:, 3, :],
                pm96=cf.t[:, 4, :], ident_b=cb.t[:, 0, :], ones_b=cb.t[:, 1, :])


def attn_groups():
    g = [("ctx", CTX, "ctxT", 0, 0, SEQ)]
    for i in range(8):
        g.append(("own", 512, "xT", i * 512, CTX + i * 512, i * 512))
    return g


def phase_attn_proj(nc, I, K0, mvec, P):
    with ExitStack() as es:
        C = Ctx(nc, es)
        S = Sched(nc)
        win = C.sb([128, 8, ATTN_IN], BF16, "win")
        wkd = C.sb([128, 8, 2, 128], BF16, "wkd")
        gv = C.sb([128, 6], F32, "gv")
        xg = [C.sb([128, 8, 512], F32, "xg") for _ in range(1)]
        hb = [C.sb([128, 8, 512], BF16, "hb") for _ in range(2)]
        rp = [C.sb([128, 4, 512], F32, "rope") for _ in range(1)]
        cqf = C.sb([128, 2, 512], F32, "cqf")
        sq2 = [C.sb([128, 512], F32, "sq2") for _ in range(2)]
        rs2 = [C.sb([128, 512], F32, "rs2") for _ in range(2)]
        qn = [C.sb([128, 512], F32, "qn") for _ in range(2)]
        t1 = [C.sb([128, 512], F32, "t1") for _ in range(2)]
        t2 = [C.sb([128, 512], F32, "t2") for _ in range(2)]
        NS = norm_scratch(C, K0["ones_f"])
        pp = [C.ps("proj") for _ in range(2)]
        aux = C.ps("aux")
        pq = C.ps("pq")
        pv = C.ps("pv")
        for kc in range(8):
            S.dma("pool", win.t[:, kc, :], I["win_r"][:, kc, :], [], [win.b])
        S.dma("sp", gv.t[:], I["attn_g"][:, :], [], [gv.b])
        for kv in range(2):
            for hh in range(2):
                S.cp("pool", wkd.t[:, :, kv, hh * 64:(hh + 1) * 64], win.t[:, :, 928 + kv * 64:928 + (kv + 1) * 64],
                     [win.b], [wkd.b])
        ckvn, krope, cqn, qg, kgd, vg = P["ckvn"], P["krope"], P["cqn"], P["qg"], P["kgd"], P["vg"]
        S.memset("pool", vg.t[:, :, :, 64:128], 1.0, [vg.b])
        ppi = [0]

        def proj(lhs_fn, M, h, T):
            p = pp[ppi[0] % 2]
            ppi[0] += 1
            for kc in range(8):
                S.mm(p.t[:M, :T], lhs_fn(kc), h.t[:, kc, :T], kc == 0, kc == 7, [win.b, wkd.b, h.b], [p.b])
            return p

        cnt = [0]

        def headnorm_rope(p, T, gcol, lat, rpt, out_ap, out_b):
            i = cnt[0] % 2
            cnt[0] += 1
            S.act(sq2[i].t[:, :T], p.t[:, :T], AF.Square, [p.b], [sq2[i].b])
            S.mm(aux.t[:, :T], K0["blk_f"], sq2[i].t[:, :T], True, True, [sq2[i].b], [aux.b])
            emit_rstd(S, aux, rs2[i], 64.0, T)
            S.stt(qn[i].t[:, :T], p.t[:, :T], gv.t[:, gcol:gcol + 1], rs2[i].t[:, :T], ALU.mult, ALU.mult,
                  [p.b, rs2[i].b, gv.b], [qn[i].b])
            if lat:
                S.mm(pq.t[:, :T], K0["pm128"], qn[i].t[:, :T], True, True, [qn[i].b], [pq.b])
                S.tt("pool", t1[i].t[:, :T], qn[i].t[:, :T], rpt.t[:, 0, :T], ALU.mult, [qn[i].b, rpt.b], [t1[i].b])
                S.tt("dve", t2[i].t[:, :T], pq.t[:, :T], rpt.t[:, 1, :T], ALU.mult, [pq.b, rpt.b], [t2[i].b])
                S.tt("dve", out_ap, t1[i].t[:, :T], t2[i].t[:, :T], ALU.add, [t1[i].b, t2[i].b], [out_b])
            else:
                S.cp("dve", out_ap, qn[i].t[:, :T], [qn[i].b], [out_b])

        for gi, (kind, T, src, c0, kpos, qpos) in enumerate(attn_groups()):
            x = xg[0]
            h = hb[gi % 2]
            rpt = rp[0]
            lat = kind != "ctx"
            j = 0 if lat else 1
            S.dma("sp", x.t[:, :, :T], I[src].rearrange("(kc p) t -> p kc t", p=128)[:, :, c0:c0 + T], [], [x.b])
            if lat:
                S.dma("act", rpt.t[:, 0:2, :T], I["ropeg"][:, :, c0:c0 + T], [], [rpt.b])
                S.dma("act", rpt.t[:, 2:4, :T], I["ropem"][:, :, c0:c0 + T], [], [rpt.b])
            emit_norm_mod(S, NS, x, T, mvec.t[:, j, 0, :], mvec.t[:, j, 1, :], h)
            isq = qpos is not None
            if isq:
                for c in range(2):
                    p = proj(lambda kc, c=c: win.t[:, kc, c * 128:(c + 1) * 128], 128, h, T)
                    S.cp("act", cqf.t[:, c, :T], p.t[:, :T], [p.b], [cqf.b])
                    S.act(sq2[c].t[:, :T], p.t[:, :T], AF.Square, [p.b], [sq2[c].b])
                    S.mm(aux.t[:, :T], K0["ones_f"], sq2[c].t[:, :T], c == 0, c == 1, [sq2[c].b], [aux.b])
                emit_rstd(S, aux, rs2[0], 256.0, T)
                for c in range(2):
                    S.stt(cqn.t[:, c, qpos:qpos + T], cqf.t[:, c, :T], gv.t[:, c:c + 1], rs2[0].t[:, :T],
                          ALU.mult, ALU.mult, [cqf.b, rs2[0].b, gv.b], [cqn.b])
            p = proj(lambda kc: win.t[:, kc, 256:384], 128, h, T)
            S.act(sq2[0].t[:, :T], p.t[:, :T], AF.Square, [p.b], [sq2[0].b])
            S.mm(aux.t[:, :T], K0["ones_f"], sq2[0].t[:, :T], True, True, [sq2[0].b], [aux.b])
            emit_rstd(S, aux, rs2[1], 128.0, T)
            S.stt(ckvn.t[:, kpos:kpos + T], p.t[:, :T], gv.t[:, 2:3], rs2[1].t[:, :T], ALU.mult, ALU.mult,
                  [p.b, rs2[1].b, gv.b], [ckvn.b])
            p = proj(lambda kc: win.t[:, kc, 320:416], 96, h, T)
            if lat:
                S.cp("act", qn[0].t[:96, :T], p.t[:96, :T], [p.b], [qn[0].b])
                S.mm(pq.t[:96, :T], K0["pm96"][:96, :96], qn[0].t[:96, :T], True, True, [qn[0].b], [pq.b])
                S.tt("pool", t1[0].t[64:96, :T], qn[0].t[64:96, :T], rpt.t[64:96, 2, :T], ALU.mult,
                     [qn[0].b, rpt.b], [t1[0].b])
                S.tt("dve", t2[0].t[64:96, :T], pq.t[64:96, :T], rpt.t[64:96, 3, :T], ALU.mult,
                     [pq.b, rpt.b], [t2[0].b])
                S.tt("dve", krope.t[64:96, kpos:kpos + T], t1[0].t[64:96, :T], t2[0].t[64:96, :T], ALU.add,
                     [t1[0].b, t2[0].b], [krope.b])
            else:
                S.cp("act", krope.t[64:96, kpos:kpos + T], p.t[64:96, :T], [p.b], [krope.b])
            if isq:
                for c in range(4):
                    p = proj(lambda kc, c=c: win.t[:, kc, 416 + c * 128:416 + (c + 1) * 128], 128, h, T)
                    headnorm_rope(p, T, 3, lat, rpt, qg.t[:, c, qpos:qpos + T], qg.b)
            for kv in range(2):
                p = proj(lambda kc, kv=kv: wkd.t[:, kc, kv, :], 128, h, T)
                headnorm_rope(p, T, 4, lat, rpt, kgd.t[:, kv, kpos:kpos + T], kgd.b)
            for tt_ in range(T // 128):
                for kc in range(8):
                    S.mm(pv.t[:, 0:128], h.t[:, kc, tt_ * 128:(tt_ + 1) * 128], win.t[:, kc, 1056:1184],
                         kc == 0, kc == 7, [h.b, win.b], [pv.b])
                kt = kpos // 128 + tt_
                S.cp("act", vg.t[:, kt, :, 0:64], pv.t[:, 0:128].rearrange("p (a b) -> p a b", a=2), [pv.b], [vg.b])
        S.emit()


def phase_attn_core(nc, I, K0, P, attn_o):
    with ExitStack() as es:
        C = Ctx(nc, es)
        S = Sched(nc)
        wuq = C.sb([128, 2, 768], BF16, "wuq")
        wukv = C.sb([128, 1024], BF16, "wukv")
        KT = [C.sb([96, NK], BF16, "KT") for _ in range(2)]
        VH = [C.sb([128, 34, 128], BF16, "VH") for _ in range(2)]
        QT = [C.sb([96, NQ], BF16, "QT") for _ in range(2)]
        rp = [C.sb([96, 2, 512], F32, "ropq") for _ in range(2)]
        qf = [C.sb([96, 512], F32, "qf") for _ in range(2)]
        t1 = [C.sb([96, 512], F32, "t1") for _ in range(2)]
        t2 = [C.sb([96, 512], F32, "t2") for _ in range(2)]
        pT = [C.sb([128, 1024], BF16, "pT") for _ in range(3)]
        rs = [C.sb([64, 512], F32, "rs") for _ in range(2)]
        ot = [C.sb([64, 512], BF16, "ot") for _ in range(2)]
        sps = [C.ps("sps", (128, 1024)) for _ in range(3)]
        accO = [C.ps("accO") for _ in range(2)]
        gen = accO[0]
        pq = accO[1]
        ckvn, krope, cqn, qg, kgd, vg = P["ckvn"], P["krope"], P["cqn"], P["qg"], P["kgd"], P["vg"]
        for c in range(2):
            S.dma("pool", wuq.t[:, c, :], I["wuq_r"][:, c, :], [], [wuq.b])
        S.dma("pool", wukv.t[:, :], I["wukv"][:, :], [], [wukv.b])
        for v_ in VH:
            S.memset("pool", v_.t[:, :, 64:128], 1.0, [v_.b])
        kgroups = [(0, CTX)] + [(CTX + i * 512, 512) for i in range(8)]
        qgroups = [(i * 512, 512, 0, 34, True) for i in range(8)] + [(SEQ, CTX, 0, 2, False)]
        unit = [0]
        for hd in range(16):
            if hd < 8:
                h = hd
                kt_, vh_, qt_ = KT[h % 2], VH[h % 2], QT[h % 2]
                for (k0, T) in kgroups:
                    S.mm(gen.t[:64, :T], wukv.t[:, h * 128:h * 128 + 64], ckvn.t[:, k0:k0 + T], True, True,
                         [wukv.b, ckvn.b], [gen.b])
                    S.cp("act", kt_.t[0:64, k0:k0 + T], gen.t[:64, :T], [gen.b], [kt_.b])
                S.cp("pool", kt_.t[64:96, :], krope.t[64:96, :], [krope.b], [kt_.b])
                for t0 in range(0, 34, 4):
                    nt = min(4, 34 - t0)
                    for i in range(nt):
                        S.mm(gen.t[:, i * 64:(i + 1) * 64], ckvn.t[:, (t0 + i) * 128:(t0 + i + 1) * 128],
                             wukv.t[:, h * 128 + 64:h * 128 + 128], True, True, [wukv.b, ckvn.b], [gen.b])
                    S.cp("dve", vh_.t[:, t0:t0 + nt, 0:64], gen.t[:, 0:nt * 64].rearrange("p (a b) -> p a b", b=64),
                         [gen.b], [vh_.b])
                for gi, (q0, T, _, _, lat) in enumerate(qgroups):
                    for c in range(2):
                        S.mm(gen.t[:96, :T], wuq.t[:, c, h * 96:(h + 1) * 96], cqn.t[:, c, q0:q0 + T], c == 0, c == 1,
                             [wuq.b, cqn.b], [gen.b])
                    S.cp("act", qt_.t[0:64, q0:q0 + T], gen.t[0:64, :T], [gen.b], [qt_.b])
                    if lat:
                        i = gi % 2
                        S.dma("sp", rp[i].t[:, :, :T], I["ropem"][0:96, :, q0:q0 + T], [], [rp[i].b])
                        S.cp("act", qf[i].t[:96, :T], gen.t[:96, :T], [gen.b], [qf[i].b])
                        S.mm(pq.t[:96, :T], K0["pm96"][:96, :96], qf[i].t[:96, :T], True, True, [qf[i].b], [pq.b])
                        S.tt("pool", t1[i].t[64:96, :T], qf[i].t[64:96, :T], rp[i].t[64:96, 0, :T], ALU.mult,
                             [qf[i].b, rp[i].b], [t1[i].b])
                        S.tt("dve", t2[i].t[64:96, :T], pq.t[64:96, :T], rp[i].t[64:96, 1, :T], ALU.mult,
                             [pq.b, rp[i].b], [t2[i].b])
                        S.tt("dve", qt_.t[64:96, q0:q0 + T], t1[i].t[64:96, :T], t2[i].t[64:96, :T], ALU.add,
                             [t1[i].b, t2[i].b], [qt_.b])
                    else:
                        S.cp("dve", qt_.t[64:96, q0:q0 + T], gen.t[64:96, :T], [gen.b], [qt_.b])
                scale = 96.0 ** -0.5
                k_ap = lambda kt, kt_=kt_: kt_.t[0:96, kt * 128:(kt + 1) * 128]
                q_ap = lambda q0, T, qt_=qt_: qt_.t[0:96, q0:q0 + T]
                v_ap = lambda kt, vh_=vh_: vh_.t[:, kt, :]
                kb, qb, vb = kt_.b, qt_.b, vh_.b
            else:
                h = hd - 8
                hh, c, kv = h % 2, h // 2, h // 4
                scale = 0.125
                k_ap = lambda kt, hh=hh, kv=kv: kgd.t[hh * 64:(hh + 1) * 64, kv, kt * 128:(kt + 1) * 128]
                q_ap = lambda q0, T, hh=hh, c=c: qg.t[hh * 64:(hh + 1) * 64, c, q0:q0 + T]
                v_ap = lambda kt, kv=kv: vg.t[:, kt, kv, :]
                kb, qb, vb = kgd.b, qg.b, vg.b
            for (q0, T, ka, kbnd, lat) in qgroups:
                u = unit[0]
                unit[0] += 1
                ao = accO[u % 2]
                npair = (kbnd - ka) // 2

                def issue_s(pi):
                    sp_ = sps[pi % 3]
                    for a_ in range(2):
                        S.mm(sp_.t[:, a_ * 512:a_ * 512 + T], k_ap(ka + 2 * pi + a_), q_ap(q0, T), True, True,
                             [kb, qb], [sp_.b])

                issue_s(0)
                if npair > 1:
                    issue_s(1)
                for pi in range(npair):
                    sp_ = sps[pi % 3]
                    p_ = pT[pi % 3]
                    if pi + 2 < npair:
                        issue_s(pi + 2)
                    S.act(p_.t[:, :].rearrange("p (a t) -> p a t", a=2)[:, :, :T],
                          sp_.t[:, :].rearrange("p (a t) -> p a t", a=2)[:, :, :T], AF.Exp, [sp_.b], [p_.b], scale=scale)
                    for a_ in range(2):
                        kt = ka + 2 * pi + a_
                        S.mm(ao.t[:, :T], v_ap(kt), p_.t[:, a_ * 512:a_ * 512 + T], kt == ka, kt == kbnd - 1,
                             [vb, p_.b], [ao.b])
                r_ = rs[u % 2]
                o_ = ot[u % 2]
                S.op("dve", lambda e, r_=r_, ao=ao, T=T: e.reciprocal(r_.t[:64, :T], ao.t[64:128, :T]),
                     [ao.b], [r_.b])
                S.tt("dve", o_.t[:64, :T], ao.t[:64, :T], r_.t[:64, :T], ALU.mult, [ao.b, r_.b], [o_.b])
                S.dma("sp", attn_o[hd * 64:(hd + 1) * 64, q0:q0 + T], o_.t[:64, :T], [o_.b], [])
        S.emit()


def phase_attn_out(nc, I, mvec, attn_o, xres):
    with ExitStack() as es:
        C = Ctx(nc, es)
        S = Sched(nc)
        wo = C.sb([128, 8, 1024], BF16, "wo")
        og = [C.sb([128, 8, 512], BF16, "og") for _ in range(2)]
        xg = [C.sb([128, 8, 512], F32, "xg") for _ in range(2)]
        pp = [C.ps("op") for _ in range(2)]
        for kc in range(8):
            S.dma("pool", wo.t[:, kc, :], I["wout_r"][:, kc, :], [], [wo.b])
        groups = [("xT", i * 512, 512, i * 512, 0) for i in range(8)] + [("ctxT", 0, CTX, SEQ, 1)]
        n = 0
        for gi, (src, c0, T, q0, j) in enumerate(groups):
            x, o = xg[gi % 2], og[gi % 2]
            S.dma("sp", x.t[:, :, :T], I[src].rearrange("(kc p) t -> p kc t", p=128)[:, :, c0:c0 + T], [], [x.b])
            S.dma("act", o.t[:, :, :T], attn_o.rearrange("(kc p) t -> p kc t", p=128)[:, :, q0:q0 + T], [], [o.b])
            for c in range(8):
                p = pp[n % 2]
                n += 1
                for kc in range(8):
                    S.mm(p.t[:, :T], wo.t[:, kc, c * 128:(c + 1) * 128], o.t[:, kc, :T], kc == 0, kc == 7,
                         [wo.b, o.b], [p.b])
                S.stt(x.t[:, c, :T], p.t[:, :T], mvec.t[:, j, 2, c:c + 1], x.t[:, c, :T], ALU.mult, ALU.add,
                      [p.b, x.b], [x.b])
            S.dma("sp", xres.rearrange("(kc p) t -> p kc t", p=128)[:, :, q0:q0 + T], x.t[:, :, :T], [x.b], [])
        S.emit()


def attention_layer(nc, I, K0, mvec, attn_o, xres):
    with ExitStack() as es:
        C = Ctx(nc, es)
        P = dict(ckvn=C.sb([128, NK], BF16, "ckvn"), krope=C.sb([96, NK], BF16, "krope"),
                 cqn=C.sb([128, 2, NQ], BF16, "cqn"), qg=C.sb([128, 4, NQ], BF16, "qg"),
                 kgd=C.sb([128, 2, NK], BF16, "kgd"), vg=C.sb([128, 34, 2, 128], BF16, "vg"))
        phase_attn_proj(nc, I, K0, mvec, P)
        phase_attn_core(nc, I, K0, P, attn_o)
    phase_attn_out(nc, I, mvec, attn_o, xres)


def make_consts():
    c = np.zeros((128, 5, 128), np.float32)
    c[:, 0, :] = np.eye(128, dtype=np.float32)
    c[:, 1, :] = 1.0
    c[0:64, 2, 0:64] = 1.0
    c[64:128, 2, 64:128] = 1.0
    for i in range(64):
        c[2 * i + 1, 3, 2 * i] = -1.0
        c[2 * i, 3, 2 * i + 1] = 1.0
    for i in range(32, 48):
        c[2 * i + 1, 4, 2 * i] = -1.0
        c[2 * i, 4, 2 * i + 1] = 1.0
    return c


def rope_tables(hf):
    pos = np.arange(SEQ) if hf == 0 else np.arange(SEQ)[::-1]
    row = (pos // 64).astype(np.float32)
    col = (pos % 64).astype(np.float32)

    def ang(rot_dim):
        nf = rot_dim // 4
        inv = (np.float32(10000.0) ** (-np.arange(nf, dtype=np.float32) / np.float32(nf))).astype(np.float32)
        return np.concatenate([row[:, None] * inv, col[:, None] * inv], axis=-1).astype(np.float32)

    ag = ang(64)
    am = ang(32)
    ropeg = np.zeros((128, 2, SEQ), np.float32)
    ropem = np.zeros((128, 2, SEQ), np.float32)
    for p in range(128):
        a = ag[:, (p % 64) // 2]
        ropeg[p, 0] = np.cos(a)
        ropeg[p, 1] = np.sin(a)
    for p in range(64, 96):
        a = am[:, (p - 64) // 2]
        ropem[p, 0] = np.cos(a)
        ropem[p, 1] = np.sin(a)
    return ropeg, ropem


def fm(v, k):
    return np.ascontiguousarray(np.asarray(v, np.float32).reshape(k, 128).T)


def wr(w):
    w = np.asarray(w, np.float32)
    K, N = w.shape
    return np.ascontiguousarray(w.reshape(K // 128, 128, N).transpose(1, 0, 2))


def shared_inputs(inp):
    sh = {}
    wm = np.asarray(inp["w_mod"], np.float32)
    sh["wmod_r"] = np.ascontiguousarray(wm.reshape(2, 8, 128, 6, 1024).transpose(0, 3, 2, 1, 4))
    bm = np.asarray(inp["b_mod"], np.float32)
    sh["bmod_r"] = np.ascontiguousarray(bm.reshape(2, 48, 128).transpose(0, 2, 1))
    ng = np.asarray(inp["norm_g"], np.float32)
    sh["ng_r"] = np.ascontiguousarray(ng.reshape(2, 2, 8, 128).transpose(0, 3, 1, 2))
    sh["consts"] = make_consts()
    sh["win_r"] = wr(inp["attn_w_in"][0])
    ag = np.zeros((128, 6), np.float32)
    ag[:, 0:2] = fm(inp["mla_g_cq"][0], 2)
    ag[:, 2] = np.asarray(inp["mla_g_ckv"][0], np.float32)
    gq = np.asarray(inp["gqa_g_q"][0], np.float32)
    gk = np.asarray(inp["gqa_g_k"][0], np.float32)
    ag[:, 3] = np.concatenate([gq, gq])
    ag[:, 4] = np.concatenate([gk, gk])
    sh["attn_g"] = ag
    sh["wuq_r"] = wr(inp["mla_w_uq"][0])
    sh["wukv"] = np.ascontiguousarray(np.asarray(inp["mla_w_ukv"][0], np.float32))
    sh["wout_r"] = wr(inp["attn_w_out"][0])
    return sh


def core_inputs_l0(inp, sh, core, ropes):
    b, hf = core // 2, core % 2
    x = np.asarray(inp["x"][b], np.float32)
    if hf == 1:
        x = x[::-1]
    d = dict(sh)
    d["xT"] = np.ascontiguousarray(x.T)
    cx = np.asarray(inp["ctx"][b], np.float32)
    if hf == 1:
        cx = cx[::-1]
    d["ctxT"] = np.ascontiguousarray(cx.T)
    cc = np.zeros((128, 8, 2), np.float32)
    cc[:, :, 0] = fm(inp["c"][b], 8)
    cc[:, :, 1] = fm(inp["c_ctx"], 8)
    d["cc"] = cc
    d["ropeg"], d["ropem"] = ropes[hf]
    return d


L0_INPUT_SHAPES = dict(
    xT=[1024, SEQ], ctxT=[1024, CTX], cc=[128, 8, 2], wmod_r=[2, 6, 128, 8, 1024], bmod_r=[2, 128, 48],
    ng_r=[2, 128, 2, 8], consts=[128, 5, 128], ropeg=[128, 2, SEQ], ropem=[128, 2, SEQ],
    win_r=[128, 8, ATTN_IN], attn_g=[128, 6], wuq_r=[128, 2, 768], wukv=[128, 1024], wout_r=[128, 8, 1024])


def build_l0(with_moe=True, n_exp=N_EXP):
    nc = bass.Bass("TRN2", target_bir_lowering=False)
    shapes = dict(L0_INPUT_SHAPES)
    if with_moe:
        shapes.update(moe_input_shapes(0, n_exp))
    I = {k: dram(nc, k, v, F32, "ExternalInput") for k, v in shapes.items()}
    attn_o = dram(nc, "attn_o", [1024, NQ], BF16, "Internal")
    xres = dram(nc, "xres", [1024, NQ], F32, "ExternalOutput")
    gt_scr = dram(nc, "gt_scr0", [32, 1152], F32, "Internal")
    with ExitStack() as es:
        init_sync(nc, es)
        K0 = load_consts(nc, es, I)
        C = Ctx(nc, es)
        mvec = C.sb([128, 2, 6, 8], F32, "mvec")
        phase_mod(nc, I, 0, mvec)
        attention_layer(nc, I, K0, mvec, attn_o, xres)
        if with_moe:
            moe_layer(nc, I, K0, mvec, 0, xres, gt_scr, moe_passes_l0(), n_exp)
    return nc


AX = mybir.AxisListType


def moe_layer(nc, I, K0, mvec, li, xres, gt_scr, passes, n_exp=N_EXP, dbg=None):
    sfx = str(li)
    for groups in passes:
        TP = sum(g[1] for g in groups)
        pos = []
        a = 0
        for g in groups:
            pos.append(a)
            a += g[1]
        with ExitStack() as es:
            C = Ctx(nc, es)
            xp = C.sb([128, 8, TP], F32, "xp")
            hb = C.sb([128, 8, TP], BF16, "hbp")
            GT = C.sb([32, TP], F32, "GT")
            xres3 = xres.rearrange("(kc p) t -> p kc t", p=128)
            with ExitStack() as es1:
                C1 = Ctx(nc, es1)
                S = Sched(nc)
                NS = norm_scratch(C1, K0["ones_f"])
                hf = [C1.sb([128, 8, 512], F32, "hf") for _ in range(2)]
                wrt = C1.sb([128, 8, 32], F32, "wrt")
                brt = C1.sb([128, 32], F32, "brt")
                lg = [C1.sb([128, 32], F32, "lg") for _ in range(2)]
                mx = [C1.sb([128, 8], F32, "mx") for _ in range(2)]
                ngm = [C1.sb([128, 1], F32, "ngm") for _ in range(2)]
                ex = [C1.sb([128, 32], F32, "ex") for _ in range(2)]
                mk = [C1.sb([128, 32], F32, "mk") for _ in range(2)]
                sm = [C1.sb([128, 1], F32, "sm") for _ in range(2)]
                Gt = [C1.sb([128, 32], F32, "Gt") for _ in range(2)]
                lps = [C1.ps("lps") for _ in range(2)]
                tps = [C1.ps("tps") for _ in range(2)]
                S.dma("sp", wrt.t[:], I["wr_r" + sfx][:, :, :], [], [wrt.b])
                S.dma("sp", brt.t[:], I["br" + sfx][0:1, :].to_broadcast([128, 32]), [], [brt.b])
                xb = [Buf() for _ in groups]
                tile_i = 0
                for gi, (c0, T, j) in enumerate(groups):
                    p0 = pos[gi]
                    xv = Tl(xp.t[:, :, p0:p0 + T])
                    xv.b = xb[gi]
                    S.dma("sp", xv.t, xres3[:, :, c0:c0 + T], [], [xv.b])
                    hfv = hf[gi % 2]
                    hbv = Tl(hb.t[:, :, p0:p0 + T])
                    hbv.b = hb.b
                    emit_norm_mod(S, NS, xv, T, mvec.t[:, j, 3, :], mvec.t[:, j, 4, :], hbv, hfv)
                    for tt_ in range(T // 128):
                        i = tile_i % 2
                        tile_i += 1
                        for kc in range(8):
                            S.mm(lps[i].t[:, 0:32], hfv.t[:, kc, tt_ * 128:(tt_ + 1) * 128], wrt.t[:, kc, :],
                                 kc == 0, kc == 7, [hfv.b, wrt.b], [lps[i].b])
                        S.tt("dve", lg[i].t[:], lps[i].t[:, 0:32], brt.t[:], ALU.add, [lps[i].b, brt.b], [lg[i].b])
                        S.op("dve", lambda e, i=i: e.max(mx[i].t[:], lg[i].t[:]), [lg[i].b], [mx[i].b])
                        S.ts("dve", ngm[i].t[:], mx[i].t[:, 0:1], -1.0, None, ALU.mult, None, [mx[i].b], [ngm[i].b])
                        S.act(ex[i].t[:], lg[i].t[:], AF.Exp, [lg[i].b, ngm[i].b], [ex[i].b], bias=ngm[i].t[:, 0:1])
                        S.ts("dve", mk[i].t[:], lg[i].t[:], mx[i].t[:, 3:4], None, ALU.is_ge, None,
                             [lg[i].b, mx[i].b], [mk[i].b])
                        S.tt("dve", ex[i].t[:], ex[i].t[:], mk[i].t[:], ALU.mult, [ex[i].b, mk[i].b], [ex[i].b])
                        S.op("dve", lambda e, i=i: e.reduce_sum(sm[i].t[:], ex[i].t[:], axis=AX.X), [ex[i].b], [sm[i].b])
                        S.op("dve", lambda e, i=i: e.reciprocal(sm[i].t[:], sm[i].t[:]), [sm[i].b], [sm[i].b])
                        S.ts("dve", Gt[i].t[:], ex[i].t[:], sm[i].t[:, 0:1], None, ALU.mult, None,
                             [ex[i].b, sm[i].b], [Gt[i].b])
                        S.tr(tps[i].t[0:32, 0:128], Gt[i].t[:], K0["ident_f"], [Gt[i].b], [tps[i].b])
                        tcol = p0 + tt_ * 128
                        S.cp("act", GT.t[0:32, tcol:tcol + 128], tps[i].t[0:32, 0:128], [tps[i].b], [GT.b])
                S.dma("sp", gt_scr[:, 0:TP], GT.t[0:32, :], [GT.b], [])
                if dbg is not None:
                    S.dma("sp", dbg["hb"][:, :, 0:TP], hb.t[:, :, :], [hb.b], [])
                S.emit()
            with ExitStack() as es2:
                C2 = Ctx(nc, es2)
                S = Sched(nc)
                NB3 = 3 if TP <= 1536 else 2
                actb = C2.sb([128, 8, TP], BF16, "actb")
                wgu = [C2.sb([128, 8, 2, 128], BF16, "wgu") for _ in range(4)]
                wd = [C2.sb([128, 8, 128], BF16, "wd") for _ in range(3)]
                gbc = [C2.sb([128, TP], F32, "gbc") for _ in range(2)]
                bgu = C2.sb([128, n_exp, 16], F32, "bgu")
                bd = C2.sb([32, 1024], F32, "bd")
                g1 = [C2.sb([128, 512], F32, "g1") for _ in range(NB3)]
                s1 = [C2.sb([128, 512], F32, "s1") for _ in range(NB3)]
                u1 = [C2.sb([128, 512], F32, "u1") for _ in range(NB3)]
                v1 = [C2.sb([128, 512], F32, "v1") for _ in range(NB3)]
                gps = [C2.ps("gps") for _ in range(NB3)]
                ups = [C2.ps("ups") for _ in range(NB3)]
                dps = [C2.ps("dps") for _ in range(2)]
                S.dma("sp", bgu.t[:], I["bgu_r" + sfx][:, 0:n_exp, :], [], [bgu.b])
                S.dma("sp", bd.t[0:n_exp, :], I["bd" + sfx][0:n_exp, :], [], [bd.b])
                bgu1 = C2.sb([128, n_exp, 8], F32, "bgu1")
                S.ts("dve", bgu1.t[:], bgu.t[:, :, 8:16], 1.0, None, ALU.add, None, [bgu.b], [bgu1.b])
                xb = [Buf() for _ in groups]
                ab = [[Buf() for _ in groups] for _ in range(8)]
                nu = 0
                nw = 0
                nd = 0
                npd = 0
                PFG, PFD = 3, 2

                def issue_gu(u):
                    if u < n_exp * 8:
                        w_ = wgu[u % 4]
                        S.dma("pool", w_.t[:].rearrange("p a b c -> p (a b c)"), I["wgu" + sfx][u // 8, u % 8], [], [w_.b])

                def issue_d(u):
                    if u < n_exp * 8:
                        w_ = wd[u % 3]
                        S.dma("pool", w_.t[:].rearrange("p a b -> p (a b)"), I["wd" + sfx][u // 8, u % 8], [], [w_.b])

                for u in range(PFG):
                    issue_gu(u)
                for u in range(PFD):
                    issue_d(u)
                for e in range(n_exp):
                    gb = gbc[e % 2]
                    S.dma("sp", gb.t[:], gt_scr[e:e + 1, 0:TP].to_broadcast([128, TP]), [], [gb.b])
                    S.act(gb.t[:], gb.t[:], AF.Identity, [gb.b], [gb.b], scale=1.0 / 1.702)
                    for j in range(8):
                        w = wgu[nw % 4]
                        issue_gu(nw + PFG)
                        nw += 1
                        for gi, (c0, T, jj) in enumerate(groups):
                            p0 = pos[gi]
                            i = nu % NB3
                            nu += 1
                            for kc in range(8):
                                S.mm(gps[i].t[:, :T], w.t[:, kc, 0, :], hb.t[:, kc, p0:p0 + T], kc == 0, kc == 7,
                                     [w.b, hb.b], [gps[i].b])
                            for kc in range(8):
                                S.mm(ups[i].t[:, :T], w.t[:, kc, 1, :], hb.t[:, kc, p0:p0 + T], kc == 0, kc == 7,
                                     [w.b, hb.b], [ups[i].b])
                            S.ts("dve", g1[i].t[:, :T], gps[i].t[:, :T], bgu.t[:, e, j:j + 1], 7.0, ALU.add, ALU.min,
                                 [gps[i].b, bgu.b], [g1[i].b])
                            S.act(s1[i].t[:, :T], g1[i].t[:, :T], AF.Silu, [g1[i].b], [s1[i].b], scale=1.702)
                            S.ts("dve", u1[i].t[:, :T], ups[i].t[:, :T], bgu1.t[:, e, j:j + 1], 8.0, ALU.add, ALU.min,
                                 [ups[i].b, bgu1.b], [u1[i].b])
                            S.stt(actb.t[:, j, p0:p0 + T], u1[i].t[:, :T], -6.0, s1[i].t[:, :T], ALU.max, ALU.mult,
                                  [u1[i].b, s1[i].b], [ab[j][gi]])
                    for c in range(8):
                        w = wd[nd % 3]
                        issue_d(nd + PFD)
                        nd += 1
                        for gi, (c0, T, jj) in enumerate(groups):
                            p0 = pos[gi]
                            p = dps[npd % 2]
                            npd += 1
                            for jc in range(8):
                                S.mm(p.t[:, :T], w.t[:, jc, :], actb.t[:, jc, p0:p0 + T], jc == 0, jc == 7,
                                     [w.b, ab[jc][gi]], [p.b])
                            tm = v1[npd % NB3]
                            S.tt("dve", tm.t[:, :T], p.t[:, :T], gb.t[:, p0:p0 + T], ALU.mult, [p.b, gb.b], [tm.b])
                            S.stt(xp.t[:, c, p0:p0 + T], tm.t[:, :T], mvec.t[:, jj, 5, c:c + 1], xp.t[:, c, p0:p0 + T],
                                  ALU.mult, ALU.add, [tm.b, xb[gi]], [xb[gi]])
                for c in range(8):
                    for gi, (c0, T, jj) in enumerate(groups):
                        p0 = pos[gi]
                        p = dps[npd % 2]
                        npd += 1
                        S.mm(p.t[:, :T], bd.t[0:n_exp, c * 128:(c + 1) * 128], GT.t[0:n_exp, p0:p0 + T], True, True,
                             [bd.b, GT.b], [p.b])
                        S.stt(xp.t[:, c, p0:p0 + T], p.t[:, :T], mvec.t[:, jj, 5, c:c + 1], xp.t[:, c, p0:p0 + T],
                              ALU.mult, ALU.add, [p.b, xb[gi]], [xb[gi]])
                for gi, (c0, T, jj) in enumerate(groups):
                    p0 = pos[gi]
                    S.dma("sp", xres3[:, :, c0:c0 + T], xp.t[:, :, p0:p0 + T], [xb[gi]], [])
                if dbg is not None:
                    S.dma("sp", dbg["actb"][:, :, 0:TP], actb.t[:, :, :], [ab[j][gi] for j in range(8) for gi in range(len(groups))], [])
                    S.dma("sp", dbg["gbc"][:, 0:TP], gbc[(n_exp - 1) % 2].t[:, :], [gbc[(n_exp - 1) % 2].b], [])
                S.emit()


def moe_passes_l0():
    ps = []
    for p in range(2):
        ps.append([(p * 1536 + k * 512, 512, 0) for k in range(3)])
    ps.append([(3072, 512, 0), (3584, 512, 0), (SEQ, CTX, 1)])
    return ps


def moe_passes_l1():
    return [[(k * 512, 512, 0) for k in range(4)]]


def moe_shared_inputs(inp, li, n_exp=N_EXP):
    sh = {}
    s = str(li)
    sh["wr_r" + s] = wr(inp["moe_w_router"][li])
    sh["br" + s] = np.ascontiguousarray(np.asarray(inp["moe_b_router"][li], np.float32).reshape(1, 32))
    bg = np.asarray(inp["moe_b_gate_up"][li], np.float32)
    sh["bgu_r" + s] = np.ascontiguousarray(bg.reshape(32, 16, 128).transpose(2, 0, 1))
    sh["bd" + s] = np.ascontiguousarray(np.asarray(inp["moe_b_down"][li], np.float32))
    wg = np.asarray(inp["moe_w_gate_up"][li], np.float32)[:n_exp]
    sh["wgu" + s] = np.ascontiguousarray(
        wg.reshape(n_exp, 8, 128, 2, 8, 128).transpose(0, 4, 2, 1, 3, 5)).reshape(n_exp, 8, 128, 2048)
    wdn = np.asarray(inp["moe_w_down"][li], np.float32)[:n_exp]
    sh["wd" + s] = np.ascontiguousarray(
        wdn.reshape(n_exp, 8, 128, 8, 128).transpose(0, 3, 2, 1, 4)).reshape(n_exp, 8, 128, 1024)
    return sh


def moe_input_shapes(li, n_exp=N_EXP):
    s = str(li)
    return {"wr_r" + s: [128, 8, 32], "br" + s: [1, 32], "bgu_r" + s: [128, 32, 16], "bd" + s: [32, 1024],
            "wgu" + s: [n_exp, 8, 128, 2048], "wd" + s: [n_exp, 8, 128, 1024]}


def build_moe_test(n_exp, li=0):
    nc = bass.Bass("TRN2", target_bir_lowering=False)
    shapes = dict(cc=[128, 8, 2], wmod_r=[2, 6, 128, 8, 1024], bmod_r=[2, 128, 48], ng_r=[2, 128, 2, 8],
                  consts=[128, 5, 128], x1T=[1024, NQ])
    shapes.update(moe_input_shapes(li, n_exp))
    I = {k: dram(nc, k, v, F32, "ExternalInput") for k, v in shapes.items()}
    xres = dram(nc, "xres", [1024, NQ], F32, "ExternalOutput")
    gt_scr = dram(nc, "gt_scr", [32, 1152], F32, "ExternalOutput")
    dbg = dict(hb=dram(nc, "dbg_hb", [128, 8, 1152], BF16, "ExternalOutput"),
               actb=dram(nc, "dbg_actb", [128, 8, 1152], BF16, "ExternalOutput"),
               gbc=dram(nc, "dbg_gbc", [128, 1152], F32, "ExternalOutput"))
    with ExitStack() as es:
        init_sync(nc, es)
        K0 = load_consts(nc, es, I)
        C = Ctx(nc, es)
        mvec = C.sb([128, 2, 6, 8], F32, "mvec")
        with ExitStack() as es1:
            S = Sched(nc)
            S.dma("sp", xres[:, :], I["x1T"][:, :], [], [])
            S.emit()
        phase_mod(nc, I, li, mvec)
        moe_layer(nc, I, K0, mvec, li, xres, gt_scr, moe_passes_l0(), n_exp, dbg)
    return nc


def ssm_scratch(nc):
    d = {}
    def mk(name, shape, dt):
        d[name] = dram(nc, "s1_" + name, shape, dt, "Internal")
    mk("z", [1024, HALF], F32)
    mk("xc_own", [1024, HALF], BF16); mk("B_own", [256, HALF], BF16); mk("C_own", [256, HALF], BF16)
    mk("xc_par", [1024, HALF], BF16); mk("B_par", [256, HALF], BF16)
    mk("xc_ctx", [1024, CTX], BF16); mk("B_ctx", [256, CTX], BF16)
    mk("dt_own", [HALF, 16], F32); mk("dt_par", [HALF, 16], F32); mk("dt_ctx", [CTX, 16], F32)
    mk("vc", [1024, HALF], F32)
    mk("Y", [1024, HALF], F32)
    mk("mix", [2048, HALF], BF16)
    return d


def phase_ssm_proj(nc, I, K0, mvec, SC):
    with ExitStack() as es:
        C = Ctx(nc, es)
        S = Sched(nc)
        win = C.sb([128, 8, 2576], BF16, "swin")
        hb = {"own": C.sb([128, 8, HALF], BF16, "hbo"), "par": C.sb([128, 8, HALF], BF16, "hbp"),
              "ctx": C.sb([128, 8, CTX], BF16, "hbc")}
        xg = [C.sb([128, 8, 512], F32, "xg") for _ in range(1)]
        NS = norm_scratch(C, K0["ones_f"])
        cw = C.sb([128, 12, 2, 5], F32, "cw")
        cb = C.sb([128, 12], F32, "cb")
        dw = C.sb([128, 8, 31], F32, "dw")
        db = C.sb([128, 8], F32, "db")
        uext = [C.sb([128, HALF + 4], F32, "uext") for _ in range(1)]
        vext = [C.sb([128, HALF + 30], F32, "vext") for _ in range(1)]
        acc = [C.sb([128, 512], F32, "acc") for _ in range(2)]
        ob = [C.sb([128, 512], BF16, "ob") for _ in range(2)]
        of = [C.sb([128, 512], F32, "of") for _ in range(2)]
        sg = [C.sb([128, 512], F32, "sg") for _ in range(2)]
        hl = [C.sb([128, 16], F32, "hl") for _ in range(2)]
        dtt = [C.sb([128, 16], F32, "dtt") for _ in range(2)]
        pp = [C.ps("sproj") for _ in range(3)]
        ph = C.ps("shalo")
        pd = C.ps("sdt")
        for kc in range(8):
            S.dma("pool", win.t[:, kc, :], I["swin_r"][:, kc, 0:2576], [], [win.b], max_dma_last_dim=4096)
        S.dma("sp", cw.t[:], I["cw_r"][:, :, :, :], [], [cw.b])
        S.dma("sp", cb.t[:], I["cb_r"][:, :], [], [cb.b])
        S.dma("sp", dw.t[:], I["dw_r"][:, :, :], [], [dw.b])
        S.dma("sp", db.t[:], I["db_r"][:, :], [], [db.b])
        gi = 0
        for (name, src, ng, T, j) in [("own", "x1o", 4, 512, 0), ("par", "x1p", 4, 512, 0), ("ctx", "c1", 1, CTX, 1)]:
            for g in range(ng):
                x = xg[0]
                gi += 1
                S.dma("sp", x.t[:, :, :T], I[src].rearrange("(kc p) t -> p kc t", p=128)[:, :, g * 512:g * 512 + T],
                      [], [x.b])
                hv = Tl(hb[name].t[:, :, g * 512:g * 512 + T])
                hv.b = hb[name].b
                emit_norm_mod(S, NS, x, T, mvec.t[:, j, 0, :], mvec.t[:, j, 1, :], hv)
        npj = [0]

        def proj(col0, M, name, t0, T):
            p = pp[npj[0] % 3]
            npj[0] += 1
            for kc in range(8):
                S.mm(p.t[:M, :T], win.t[:, kc, col0:col0 + M], hb[name].t[:, kc, t0:t0 + T], kc == 0, kc == 7,
                     [win.b, hb[name].b], [p.b])
            return p

        n = [0]
        for m in range(8):
            for g in range(4):
                p = proj(m * 128, 128, "own", g * 512, 512)
                o = of[n[0] % 2]
                n[0] += 1
                S.cp("act", o.t[:, :], p.t[:, :512], [p.b], [o.b])
                S.dma("sp", SC["z"][m * 128:(m + 1) * 128, g * 512:(g + 1) * 512], o.t[:, :], [o.b], [])
        sets = [("own", 12, HALF, 0, "par"), ("par", 10, HALF, 0, "own"), ("ctx", 10, CTX, 0, None)]
        dst = {"own": ("xc_own", "B_own", "C_own"), "par": ("xc_par", "B_par", None), "ctx": ("xc_ctx", "B_ctx", None)}
        nu = 0
        for (name, nch, TT, frame, other) in sets:
            for m in range(nch):
                u = uext[0]
                nu += 1
                col0 = 1024 + m * 128
                if name == "par":
                    for kc in range(8):
                        S.mm(ph.t[:, 0:2], win.t[:, kc, col0:col0 + 128], hb["own"].t[:, kc, HALF - 2:HALF], kc == 0, kc == 7,
                             [win.b, hb["own"].b], [ph.b])
                    S.cp("act", u.t[:, 0:2], ph.t[:, 0:2], [ph.b], [u.b])
                else:
                    S.memset("pool", u.t[:, 0:2], 0.0, [u.b])
                for g in range(max(1, TT // 512)):
                    T = min(512, TT)
                    p = proj(col0, 128, name, g * 512, T)
                    S.cp("act", u.t[:, 2 + g * 512:2 + g * 512 + T], p.t[:, :T], [p.b], [u.b])
                if name != "own":
                    S.memset("pool", u.t[:, 2 + TT:4 + TT], 0.0, [u.b])
                else:
                    for kc in range(8):
                        S.mm(ph.t[:, 0:2], win.t[:, kc, col0:col0 + 128], hb["par"].t[:, kc, 0:2], kc == 0, kc == 7,
                             [win.b, hb["par"].b], [ph.b])
                    S.cp("act", u.t[:, 2 + TT:4 + TT], ph.t[:, 0:2], [ph.b], [u.b])
                for g in range(max(1, TT // 512)):
                    T = min(512, TT)
                    a = acc[n[0] % 2]
                    o = ob[n[0] % 2]
                    n[0] += 1
                    b0 = g * 512
                    S.ts("dve", a.t[:, :T], u.t[:, b0:b0 + T], cw.t[:, m, frame, 0:1], cb.t[:, m:m + 1], ALU.mult, ALU.add,
                         [u.b, cw.b, cb.b], [a.b])
                    for k in range(1, 5):
                        S.stt(a.t[:, :T], u.t[:, b0 + k:b0 + k + T], cw.t[:, m, frame, k:k + 1], a.t[:, :T], ALU.mult, ALU.add,
                              [u.b, a.b], [a.b])
                    S.act(o.t[:, :T], a.t[:, :T], AF.Silu, [a.b], [o.b])
                    if m < 8:
                        d_ = SC[dst[name][0]][m * 128:(m + 1) * 128, b0:b0 + T]
                    elif m < 10:
                        d_ = SC[dst[name][1]][(m - 8) * 128:(m - 7) * 128, b0:b0 + T]
                    else:
                        d_ = SC[dst[name][2]][(m - 10) * 128:(m - 9) * 128, b0:b0 + T]
                    S.dma("sp", d_, o.t[:, :T], [o.b], [])
        nt = 0
        for (name, TT) in [("own", HALF), ("par", HALF), ("ctx", CTX)]:
            for t_ in range(TT // 128):
                d_ = dtt[nt % 2]
                nt += 1
                for kc in range(8):
                    S.mm(pd.t[:, 0:16], hb[name].t[:, kc, t_ * 128:(t_ + 1) * 128], win.t[:, kc, 2560:2576], kc == 0, kc == 7,
                         [hb[name].b, win.b], [pd.b])
                S.cp("act", d_.t[:, :], pd.t[:, 0:16], [pd.b], [d_.b])
                S.dma("sp", SC["dt_" + name][t_ * 128:(t_ + 1) * 128, :], d_.t[:, :], [d_.b], [])
        for kc in range(8):
            S.dma("pool", win.t[:, kc, 0:2048], I["swin_r"][:, kc, 2576:4624], [], [win.b], max_dma_last_dim=4096)
        for m in range(8):
            v = vext[0]
            ca, cbb = m * 128, 1024 + m * 128
            S.memset("pool", v.t[:, 0:15], 0.0, [v.b])
            for g in range(4):
                pa = proj(ca, 128, "own", g * 512, 512)
                pb_ = proj(cbb, 128, "own", g * 512, 512)
                s_ = sg[n[0] % 2]
                n[0] += 1
                S.act(s_.t[:, :], pb_.t[:, :512], AF.Sigmoid, [pb_.b], [s_.b])
                S.tt("dve", v.t[:, 15 + g * 512:15 + (g + 1) * 512], pa.t[:, :512], s_.t[:, :], ALU.mult, [pa.b, s_.b], [v.b])
            h_ = hl[m % 2]
            for kc in range(8):
                S.mm(ph.t[:, 0:16], win.t[:, kc, ca:ca + 128], hb["par"].t[:, kc, 0:16], kc == 0, kc == 7,
                     [win.b, hb["par"].b], [ph.b])
            for kc in range(8):
                S.mm(ph.t[:, 16:32], win.t[:, kc, cbb:cbb + 128], hb["par"].t[:, kc, 0:16], kc == 0, kc == 7,
                     [win.b, hb["par"].b], [ph.b])
            S.act(h_.t[:, :], ph.t[:, 16:32], AF.Sigmoid, [ph.b], [h_.b])
            S.tt("dve", v.t[:, 15 + HALF:30 + HALF], ph.t[:, 0:15], h_.t[:, 0:15], ALU.mult, [ph.b, h_.b], [v.b])
            for g in range(4):
                a = acc[n[0] % 2]
                n[0] += 1
                b0 = g * 512
                S.ts("dve", a.t[:, :], v.t[:, b0:b0 + 512], dw.t[:, m, 0:1], db.t[:, m:m + 1], ALU.mult, ALU.add,
                     [v.b, dw.b, db.b], [a.b])
                for k in range(1, 31):
                    S.stt(a.t[:, :], v.t[:, b0 + k:b0 + k + 512], dw.t[:, m, k:k + 1], a.t[:, :], ALU.mult, ALU.add,
                          [v.b, a.b], [a.b])
                S.dma("sp", SC["vc"][m * 128:(m + 1) * 128, b0:b0 + 512], a.t[:, :], [a.b], [])
        S.emit()


def phase_ssd(nc, I, K0, SC):
    with ExitStack() as es:
        C = Ctx(nc, es)
        S = Sched(nc)
        U = C.sb([128, 2, 128], F32, "Umask")
        par = C.sb([128, 2, 2, 16], F32, "ssmp")
        aneg = C.sb([128, 2, 16], F32, "aneg")
        dsk = C.sb([64, 2, 16], F32, "dsk")
        dsum = C.sb([64, 16], F32, "dsum")
        St = [C.sb([128, 16, 64], F32, "St") for _ in range(2)]
        Sb = [C.sb([128, 16, 64], BF16, "Sb") for _ in range(2)]
        xcf = [C.sb([128, 8, 128], BF16, "xcf") for _ in range(2)]
        bcf = [C.sb([128, 4, 128], BF16, "bcf") for _ in range(2)]
        xtok = [C.sb([128, 16, 64], BF16, "xtok") for _ in range(2)]
        xdt = [C.sb([128, 16, 64], BF16, "xdt") for _ in range(2)]
        xdw = [C.sb([128, 16, 64], BF16, "xdw") for _ in range(2)]
        btok = [C.sb([128, 2, 128], BF16, "btok") for _ in range(2)]
        dtr = [C.sb([128, 16], F32, "dtr") for _ in range(2)]
        dtv = [C.sb([128, 16], F32, "dtv") for _ in range(2)]
        dav = [C.sb([128, 16], F32, "dav") for _ in range(2)]
        csb = [C.sb([128, 16], F32, "csb") for _ in range(2)]
        wend = [C.sb([128, 16], F32, "wend") for _ in range(2)]
        etot = [C.sb([128, 16], F32, "etot") for _ in range(2)]
        cbm = [C.sb([128, 2, 128], F32, "cbm") for _ in range(2)]
        dall = [C.sb([128, 16, 128], F32, "dall") for _ in range(2)]
        dc = [C.sb([128, 512], F32, "dc") for _ in range(2)]
        ee = [C.sb([128, 512], F32, "ee") for _ in range(2)]
        el = [C.sb([128, 512], F32, "el") for _ in range(2)]
        mp = [C.sb([128, 512], BF16, "mp") for _ in range(2)]
        cs_ = [C.sb([128, 512], BF16, "cs") for _ in range(2)]
        ysb = [C.sb([64, 16, 128], F32, "ysb") for _ in range(2)]
        yA = [C.sb([64, 16, 128], F32, "yA") for _ in range(2)]
        xh = [C.sb([64, 16, 128], BF16, "xh") for _ in range(2)]
        ptx = C.ps("ptx", (128, 1024), BF16)
        ptb = C.ps("ptb", (128, 1024), BF16)
        pcb = C.ps("pcb")
        psmb = Buf()
        pab = [C.ps("pab") for _ in range(2)]
        py = [C.ps("py") for _ in range(2)]
        pst = C.ps("pst")
        S.dma("sp", U.t[:], I["consts2"][:, :, :], [], [U.b])
        S.dma("sp", par.t[:], I["ssm_p"][:, :, :, :], [], [par.b])
        S.dma("sp", dsk.t[:], I["dsk_r"][:, :, :], [], [dsk.b])
        S.act(aneg.t[:], par.t[:, :, 1, :], AF.Exp, [par.b], [aneg.b])
        S.ts("dve", aneg.t[:], aneg.t[:], -1.0, None, ALU.mult, None, [aneg.b], [aneg.b])
        S.tt("dve", dsum.t[:], dsk.t[:, 0, :], dsk.t[:, 1, :], ALU.add, [dsk.b], [dsum.b])
        cnt = [0]

        def chunk(d, name, ci, style, full, last_dir):
            i = cnt[0] % 2
            cnt[0] += 1
            t0 = ci * 128
            S.dma("sp", dtr[i].t[:], SC["dt_" + name][t0:t0 + 128, :], [], [dtr[i].b])
            S.dma("act", xcf[i].t[:], SC["xc_" + name].rearrange("(m p) t -> p m t", p=128)[:, :, t0:t0 + 128], [], [xcf[i].b])
            S.dma("sp", bcf[i].t[:, 0:2, :], SC["B_" + name].rearrange("(g p) t -> p g t", p=128)[:, :, t0:t0 + 128], [], [bcf[i].b])
            if full:
                S.dma("sp", bcf[i].t[:, 2:4, :], SC["C_own"].rearrange("(g p) t -> p g t", p=128)[:, :, t0:t0 + 128], [], [bcf[i].b])
            S.tt("dve", dtv[i].t[:], dtr[i].t[:], par.t[:, d, 0, :], ALU.add, [dtr[i].b, par.b], [dtv[i].b])
            S.act(dtv[i].t[:], dtv[i].t[:], AF.Exp, [dtv[i].b], [dtv[i].b])
            S.ts("dve", dtv[i].t[:], dtv[i].t[:], 1.0, None, ALU.add, None, [dtv[i].b], [dtv[i].b])
            S.act(dtv[i].t[:], dtv[i].t[:], AF.Ln, [dtv[i].b], [dtv[i].b])
            S.tt("dve", dav[i].t[:], dtv[i].t[:], aneg.t[:, d, :], ALU.mult, [dtv[i].b, aneg.b], [dav[i].b])
            S.mm(pcb.t[:, 256:272], U.t[:, style, :], dav[i].t[:], True, True, [U.b, dav[i].b], [psmb])
            S.mm(pcb.t[:, 272:288], K0["ones_f"], dav[i].t[:], True, True, [dav[i].b], [psmb])
            S.cp("act", csb[i].t[:], pcb.t[:, 256:272], [psmb], [csb[i].b])
            S.tt("dve", wend[i].t[:], pcb.t[:, 272:288], csb[i].t[:], ALU.subtract, [psmb, csb[i].b], [wend[i].b])
            S.act(wend[i].t[:], wend[i].t[:], AF.Exp, [wend[i].b], [wend[i].b])
            S.act(etot[i].t[:], pcb.t[:, 272:288], AF.Exp, [psmb], [etot[i].b])
            for m in range(8):
                S.tr(ptx.t[:, m * 128:(m + 1) * 128], xcf[i].t[:, m, :], K0["ident_b"], [xcf[i].b], [ptx.b])
            S.cp("act", xtok[i].t[:].rearrange("p h c -> p (h c)"), ptx.t[:, :], [ptx.b], [xtok[i].b])
            S.tt("dve", xdt[i].t[:], xtok[i].t[:], dtv[i].t[:, :].unsqueeze(2).to_broadcast([128, 16, 64]), ALU.mult,
                 [xtok[i].b, dtv[i].b], [xdt[i].b])
            S.tt("dve", xdw[i].t[:], xdt[i].t[:], wend[i].t[:, :].unsqueeze(2).to_broadcast([128, 16, 64]), ALU.mult,
                 [xdt[i].b, wend[i].b], [xdw[i].b])
            for g in range(2):
                S.tr(ptb.t[:, g * 128:(g + 1) * 128], bcf[i].t[:, g, :], K0["ident_b"], [bcf[i].b], [ptb.b])
            S.cp("act", btok[i].t[:].rearrange("p g c -> p (g c)"), ptb.t[:, 0:256], [ptb.b], [btok[i].b])
            if full:
                for g in range(2):
                    S.mm(pcb.t[:, g * 128:(g + 1) * 128], bcf[i].t[:, g, :], bcf[i].t[:, 2 + g, :], True, True, [bcf[i].b], [pcb.b])
                    S.tt("dve", cbm[i].t[:, g, :], pcb.t[:, g * 128:(g + 1) * 128], U.t[:, style, :], ALU.mult, [pcb.b, U.b], [cbm[i].b])
                if last_dir:
                    S.dma("sp", yA[i].t[:], SC["Y"].rearrange("(h p) t -> p h t", p=64)[:, :, t0:t0 + 128], [], [yA[i].b])
                    S.dma("act", xh[i].t[:], SC["xc_own"].rearrange("(h p) t -> p h t", p=64)[:, :, t0:t0 + 128], [], [xh[i].b])
                S.tt("dve", dall[i].t[:], U.t[:, style, :].unsqueeze(1).to_broadcast([128, 16, 128]),
                     dav[i].t[:, :].unsqueeze(2).to_broadcast([128, 16, 128]), ALU.mult, [U.b, dav[i].b], [dall[i].b])
                for blk in range(4):
                    h0 = blk * 4
                    g = h0 // 8
                    k = blk % 2
                    S.mm(pab[k].t[:, :], K0["ones_f"], dall[i].t[:, h0:h0 + 4, :].rearrange("p h l -> p (h l)"), True, True,
                         [dall[i].b], [pab[k].b])
                    pab3 = pab[k].t[:, :].rearrange("p (h l) -> p h l", h=4)
                    S.tt("dve", dc[k].t[:].rearrange("p (h l) -> p h l", h=4), pab3,
                         csb[i].t[:, h0:h0 + 4].unsqueeze(2).to_broadcast([128, 4, 128]), ALU.subtract,
                         [pab[k].b, csb[i].b], [dc[k].b])
                    S.ts("dve", dc[k].t[:], dc[k].t[:], 0.0, None, ALU.min, None, [dc[k].b], [dc[k].b])
                    S.act(ee[k].t[:], dc[k].t[:], AF.Exp, [dc[k].b], [ee[k].b])
                    S.tt("dve", mp[k].t[:].rearrange("p (h l) -> p h l", h=4), ee[k].t[:].rearrange("p (h l) -> p h l", h=4),
                         cbm[i].t[:, g, :].unsqueeze(1).to_broadcast([128, 4, 128]), ALU.mult, [ee[k].b, cbm[i].b], [mp[k].b])
                    S.act(el[k].t[:], pab[k].t[:, :], AF.Exp, [pab[k].b], [el[k].b])
                    S.tt("dve", cs_[k].t[:].rearrange("p (h l) -> p h l", h=4), el[k].t[:].rearrange("p (h l) -> p h l", h=4),
                         bcf[i].t[:, 2 + g, :].unsqueeze(1).to_broadcast([128, 4, 128]), ALU.mult, [bcf[i].b, el[k].b], [cs_[k].b])
                    for hh in range(4):
                        hd = h0 + hh
                        yo = py[k].t[0:64, hh * 128:(hh + 1) * 128]
                        S.mm(yo, xdt[i].t[:, hd, :], mp[k].t[:, hh * 128:(hh + 1) * 128], True, False, [xdt[i].b, mp[k].b], [py[k].b])
                        S.mm(yo, Sb[d].t[:, hd, :], cs_[k].t[:, hh * 128:(hh + 1) * 128], False, True, [Sb[d].b, cs_[k].b], [py[k].b])
                    yv = ysb[i].t[:, h0:h0 + 4, :]
                    pv = py[k].t[0:64, :].rearrange("p (h t) -> p h t", h=4)
                    if last_dir:
                        S.tt("dve", yv, pv, yA[i].t[:, h0:h0 + 4, :], ALU.add, [py[k].b, yA[i].b], [ysb[i].b])
                    else:
                        S.cp("act", yv, pv, [py[k].b], [ysb[i].b])
                if last_dir:
                    S.tt("dve", yA[i].t[:], xh[i].t[:], dsum.t[:, :].unsqueeze(2).to_broadcast([64, 16, 128]), ALU.mult,
                         [xh[i].b, dsum.b, yA[i].b], [yA[i].b])
                    S.tt("dve", ysb[i].t[:], ysb[i].t[:], yA[i].t[:], ALU.add, [ysb[i].b, yA[i].b], [ysb[i].b])
                S.dma("sp", SC["Y"].rearrange("(h p) t -> p h t", p=64)[:, :, t0:t0 + 128], ysb[i].t[:], [ysb[i].b], [])
            for g in range(2):
                S.mm(pst.t[:, :], btok[i].t[:, g, :], xdw[i].t[:, g * 8:(g + 1) * 8, :].rearrange("p h c -> p (h c)"),
                     True, True, [btok[i].b, xdw[i].b], [pst.b])
                sv = St[d].t[:, g * 8:(g + 1) * 8, :]
                S.tt("dve", sv, sv, etot[i].t[:, g * 8:(g + 1) * 8].unsqueeze(2).to_broadcast([128, 8, 64]), ALU.mult,
                     [St[d].b, etot[i].b], [St[d].b])
                S.tt("dve", sv, sv, pst.t[:, :].rearrange("p (h c) -> p h c", h=8), ALU.add, [St[d].b, pst.b], [St[d].b])
                S.cp("act", Sb[d].t[:, g * 8:(g + 1) * 8, :], sv, [St[d].b], [Sb[d].b])

        for d in range(2):
            S.memset("dve", St[d].t[:], 0.0, [St[d].b])
            S.memset("pool", Sb[d].t[:], 0.0, [Sb[d].b])
        for ci in range(2):
            chunk(0, "ctx", ci, 0, False, False)
        for ci in range(16):
            chunk(0, "own", ci, 0, True, False)
        for ci in (1, 0):
            chunk(1, "ctx", ci, 1, False, False)
        for ci in range(15, -1, -1):
            chunk(1, "par", ci, 1, False, False)
        for ci in range(15, -1, -1):
            chunk(1, "own", ci, 1, True, True)
        S.emit()


def phase_ssm_out(nc, I, K0, mvec, SC, xres):
    with ExitStack() as es:
        C = Ctx(nc, es)
        S = Sched(nc)
        wo = C.sb([128, 16, 1024], BF16, "swo")
        pv = C.sb([128, 3, 8], F32, "spv")
        yg = C.sb([128, 8, 512], F32, "yg")
        zg = C.sb([128, 8, 512], F32, "zg")
        vg_ = C.sb([128, 8, 512], F32, "vg")
        xg = C.sb([128, 8, 512], F32, "xg3")
        ys = C.sb([128, 8, 512], BF16, "ys")
        yc = C.sb([128, 8, 512], BF16, "yc")
        sq = [C.sb([128, 512], F32, "sq3") for _ in range(2)]
        rstd = C.sb([128, 512], F32, "rstd3")
        mean = C.sb([128, 512], F32, "mean3")
        var = C.sb([128, 512], F32, "var3")
        tmp = [C.sb([128, 512], F32, "tmp3") for _ in range(2)]
        ss = C.ps("ss3")
        sm = C.ps("sm3")
        pp = [C.ps("po3") for _ in range(2)]
        for kc in range(16):
            S.dma("pool", wo.t[:, kc, :], I["swo_r"][:, kc, :], [], [wo.b])
        S.dma("sp", pv.t[:], I["ssm_v"][:, :, :], [], [pv.b])
        n = 0
        for g in range(4):
            c0 = g * 512
            S.dma("sp", yg.t[:], SC["Y"].rearrange("(kc p) t -> p kc t", p=128)[:, :, c0:c0 + 512], [], [yg.b])
            S.dma("act", zg.t[:], SC["z"].rearrange("(kc p) t -> p kc t", p=128)[:, :, c0:c0 + 512], [], [zg.b])
            S.dma("sp", vg_.t[:], SC["vc"].rearrange("(kc p) t -> p kc t", p=128)[:, :, c0:c0 + 512], [], [vg_.b])
            S.dma("act", xg.t[:], I["x1o"].rearrange("(kc p) t -> p kc t", p=128)[:, :, c0:c0 + 512], [], [xg.b])
            S.act(zg.t[:], zg.t[:], AF.Silu, [zg.b], [zg.b])
            S.tt("dve", yg.t[:], yg.t[:], zg.t[:], ALU.mult, [yg.b, zg.b], [yg.b])
            for kc in range(8):
                s = sq[kc % 2]
                S.act(s.t[:], yg.t[:, kc, :], AF.Square, [yg.b], [s.b])
                S.mm(ss.t[:, :], K0["ones_f"], s.t[:], kc == 0, kc == 7, [s.b], [ss.b])
            emit_rstd(S, ss, rstd, 1024.0, 512)
            for kc in range(8):
                S.stt(ys.t[:, kc, :], yg.t[:, kc, :], pv.t[:, 0, kc:kc + 1], rstd.t[:], ALU.mult, ALU.mult,
                      [yg.b, rstd.b, pv.b], [ys.b])
            for kc in range(8):
                s = sq[kc % 2]
                S.mm(sm.t[:, :], K0["ones_f"], vg_.t[:, kc, :], kc == 0, kc == 7, [vg_.b], [sm.b])
                S.act(s.t[:], vg_.t[:, kc, :], AF.Square, [vg_.b], [s.b])
                S.mm(ss.t[:, :], K0["ones_f"], s.t[:], kc == 0, kc == 7, [s.b], [ss.b])
            S.act(mean.t[:], sm.t[:, :], AF.Identity, [sm.b], [mean.b], scale=1.0 / 1024.0)
            S.tt("dve", var.t[:], mean.t[:], mean.t[:], ALU.mult, [mean.b], [var.b])
            S.stt(var.t[:], ss.t[:, :], 1.0 / 1024.0, var.t[:], ALU.mult, ALU.subtract, [ss.b, var.b], [var.b])
            S.ts("dve", var.t[:], var.t[:], EPS, None, ALU.add, None, [var.b], [var.b])
            S.act(var.t[:], var.t[:], AF.Ln, [var.b], [var.b])
            S.act(var.t[:], var.t[:], AF.Exp, [var.b], [var.b], scale=-0.5)
            for kc in range(8):
                t = tmp[kc % 2]
                S.tt("pool", t.t[:], vg_.t[:, kc, :], mean.t[:], ALU.subtract, [vg_.b, mean.b], [t.b])
                S.stt(t.t[:], t.t[:], pv.t[:, 1, kc:kc + 1], var.t[:], ALU.mult, ALU.mult, [t.b, var.b, pv.b], [t.b])
                S.act(yc.t[:, kc, :], t.t[:], AF.Silu, [t.b, pv.b], [yc.b], bias=pv.t[:, 2, kc:kc + 1])
            for c in range(8):
                p = pp[n % 2]
                n += 1
                for kc in range(16):
                    src = ys if kc < 8 else yc
                    S.mm(p.t[:, :], wo.t[:, kc, c * 128:(c + 1) * 128], src.t[:, kc % 8, :], kc == 0, kc == 15,
                         [wo.b, src.b], [p.b])
                S.stt(xg.t[:, c, :], p.t[:, :], mvec.t[:, 0, 2, c:c + 1], xg.t[:, c, :], ALU.mult, ALU.add,
                      [p.b, xg.b], [xg.b])
            S.dma("sp", xres.rearrange("(kc p) t -> p kc t", p=128)[:, :, c0:c0 + 512], xg.t[:], [xg.b], [])
        S.emit()


def phase_final_norm(nc, I, K0, xres, out):
    with ExitStack() as es:
        C = Ctx(nc, es)
        S = Sched(nc)
        fg = C.sb([128, 8], F32, "fg")
        xg = [C.sb([128, 8, 512], F32, "xgf") for _ in range(2)]
        sq = [C.sb([128, 512], F32, "sqf") for _ in range(2)]
        rstd = C.sb([128, 512], F32, "rstdf")
        ss = C.ps("ssf")
        S.dma("sp", fg.t[:], I["fg_r"][:, :], [], [fg.b])
        for g in range(4):
            x = xg[g % 2]
            c0 = g * 512
            S.dma("sp", x.t[:], xres.rearrange("(kc p) t -> p kc t", p=128)[:, :, c0:c0 + 512], [], [x.b])
            for kc in range(8):
                s = sq[kc % 2]
                S.act(s.t[:], x.t[:, kc, :], AF.Square, [x.b], [s.b])
                S.mm(ss.t[:, :], K0["ones_f"], s.t[:], kc == 0, kc == 7, [s.b], [ss.b])
            emit_rstd(S, ss, rstd, 1024.0, 512)
            for kc in range(8):
                S.stt(x.t[:, kc, :], x.t[:, kc, :], fg.t[:, kc:kc + 1], rstd.t[:], ALU.mult, ALU.mult,
                      [x.b, rstd.b, fg.b], [x.b])
            S.dma("sp", out.rearrange("(kc p) t -> p kc t", p=128)[:, :, c0:c0 + 512], x.t[:], [x.b], [])
        S.emit()


def make_consts2():
    c = np.zeros((128, 2, 128), np.float32)
    t = np.arange(128)
    c[:, 0, :] = (t[:, None] <= t[None, :]).astype(np.float32)
    c[:, 1, :] = (t[:, None] >= t[None, :]).astype(np.float32)
    return c


def shared_inputs_l1(inp):
    sh = {}
    wm = np.asarray(inp["w_mod"], np.float32)
    sh["wmod_r"] = np.ascontiguousarray(wm.reshape(2, 8, 128, 6, 1024).transpose(0, 3, 2, 1, 4))
    bm = np.asarray(inp["b_mod"], np.float32)
    sh["bmod_r"] = np.ascontiguousarray(bm.reshape(2, 48, 128).transpose(0, 2, 1))
    ng = np.asarray(inp["norm_g"], np.float32)
    sh["ng_r"] = np.ascontiguousarray(ng.reshape(2, 2, 8, 128).transpose(0, 3, 1, 2))
    sh["consts"] = make_consts()
    sh["consts2"] = make_consts2()
    sh["swin_r"] = wr(inp["ssm_w_in"][0])
    sh["swo_r"] = wr(inp["ssm_w_out"][0])
    v = np.zeros((128, 3, 8), np.float32)
    v[:, 0, :] = fm(inp["ssm_norm_g"][0], 8)
    v[:, 1, :] = fm(inp["conf_ln_g"][0], 8)
    v[:, 2, :] = fm(inp["conf_ln_b"][0], 8)
    sh["ssm_v"] = v
    sh["cb_r"] = fm(inp["ssm_conv_b"][0], 12)
    sh["db_r"] = fm(inp["conf_dw_b"][0], 8)
    sh["fg_r"] = fm(inp["final_g"], 8)
    ds = np.asarray(inp["ssm_d"][0], np.float32)
    sh["dsk_r"] = np.ascontiguousarray(np.broadcast_to(ds[None], (64, 2, 16)))
    return sh


def core_inputs_l1(inp, sh, core):
    b, hf = core // 2, core % 2
    d = dict(sh)
    cc = np.zeros((128, 8, 2), np.float32)
    cc[:, :, 0] = fm(inp["c"][b], 8)
    cc[:, :, 1] = fm(inp["c_ctx"], 8)
    d["cc"] = cc
    cwt = np.asarray(inp["ssm_conv_w"][0], np.float32)
    own = cwt if hf == 0 else cwt[::-1]
    parf = own[::-1]
    cw = np.zeros((128, 12, 2, 5), np.float32)
    cw[:, :, 0, :] = own.T.reshape(12, 128, 5).transpose(1, 0, 2)
    cw[:, :, 1, :] = parf.T.reshape(12, 128, 5).transpose(1, 0, 2)
    d["cw_r"] = cw
    dwt = np.asarray(inp["conf_dw_w"][0], np.float32)
    dwl = dwt if hf == 0 else dwt[::-1]
    d["dw_r"] = np.ascontiguousarray(dwl.T.reshape(8, 128, 31).transpose(1, 0, 2))
    p = np.zeros((128, 2, 2, 16), np.float32)
    for ld in range(2):
        gd = hf if ld == 0 else 1 - hf
        p[:, ld, 0, :] = np.asarray(inp["ssm_dt_bias"][0][gd], np.float32)[None]
        p[:, ld, 1, :] = np.asarray(inp["ssm_a_log"][0][gd], np.float32)[None]
    d["ssm_p"] = p
    return d


L1_INPUT_SHAPES = dict(
    x1o=[1024, HALF], x1p=[1024, HALF], c1=[1024, CTX], cc=[128, 8, 2], wmod_r=[2, 6, 128, 8, 1024],
    bmod_r=[2, 128, 48], ng_r=[2, 128, 2, 8], consts=[128, 5, 128], consts2=[128, 2, 128],
    swin_r=[128, 8, SSM_IN], swo_r=[128, 16, 1024], ssm_v=[128, 3, 8], cw_r=[128, 12, 2, 5], cb_r=[128, 12],
    dw_r=[128, 8, 31], db_r=[128, 8], ssm_p=[128, 2, 2, 16], dsk_r=[64, 2, 16], fg_r=[128, 8])


def build_l1(with_moe=True, n_exp=N_EXP):
    nc = bass.Bass("TRN2", target_bir_lowering=False)
    shapes = dict(L1_INPUT_SHAPES)
    if with_moe:
        shapes.update(moe_input_shapes(1, n_exp))
    I = {k: dram(nc, k, v, F32, "ExternalInput") for k, v in shapes.items()}
    xres = dram(nc, "xres1", [1024, HALF], F32, "Internal" if with_moe else "ExternalOutput")
    out = dram(nc, "outT", [1024, HALF], F32, "ExternalOutput") if with_moe else None
    gt_scr = dram(nc, "gt_scr1", [32, 1152], F32, "Internal")
    SC = ssm_scratch(nc)
    with ExitStack() as es:
        init_sync(nc, es)
        K0 = load_consts(nc, es, I)
        C = Ctx(nc, es)
        mvec = C.sb([128, 2, 6, 8], F32, "mvec1")
        phase_mod(nc, I, 1, mvec)
        phase_ssm_proj(nc, I, K0, mvec, SC)
        phase_ssd(nc, I, K0, SC)
        phase_ssm_out(nc, I, K0, mvec, SC, xres)
        if with_moe:
            moe_layer(nc, I, K0, mvec, 1, xres, gt_scr, moe_passes_l1(), n_exp)
            phase_final_norm(nc, I, K0, xres, out)
    return nc


def fused_input_shapes(n_exp=N_EXP):
    shapes = dict(L0_INPUT_SHAPES)
    shapes.update(moe_input_shapes(0, n_exp))
    for k, v in L1_INPUT_SHAPES.items():
        if k not in ("x1o", "x1p", "c1"):
            shapes[k] = v
    shapes.update(moe_input_shapes(1, n_exp))
    return shapes


def build_fused(n_exp=N_EXP, stop_after=None):
    nc = bass.Bass("TRN2", target_bir_lowering=False)
    I = {k: dram(nc, k, v, F32, "ExternalInput") for k, v in fused_input_shapes(n_exp).items()}
    attn_o = dram(nc, "attn_o", [1024, NQ], BF16, "Internal")
    xres0 = dram(nc, "xres0", [1024, NQ], F32, "Internal")
    xres1 = dram(nc, "xres1", [1024, HALF], F32, "Internal")
    gt_scr = dram(nc, "gt_scr", [32, 2048], F32, "Internal")
    out = dram(nc, "outT", [1024, HALF], F32, "ExternalOutput")
    SC = ssm_scratch(nc)
    I["x1o"] = xres0[:, 0:HALF]
    I["x1p"] = xres0[:, HALF:SEQ]
    I["c1"] = xres0[:, SEQ:NQ]
    with ExitStack() as es:
        init_sync(nc, es)
        K0 = load_consts(nc, es, I)
        C = Ctx(nc, es)
        mvec = C.sb([128, 2, 6, 8], F32, "mvec")
        phase_mod(nc, I, 0, mvec)
        attention_layer(nc, I, K0, mvec, attn_o, xres0)
        moe_layer(nc, I, K0, mvec, 0, xres0, gt_scr, moe_passes_l0(), n_exp)
        phase_mod(nc, I, 1, mvec)
        phase_ssm_proj(nc, I, K0, mvec, SC)
        phase_ssd(nc, I, K0, SC)
        phase_ssm_out(nc, I, K0, mvec, SC, xres1)
        moe_layer(nc, I, K0, mvec, 1, xres1, gt_scr, moe_passes_l1(), n_exp)
        phase_final_norm(nc, I, K0, xres1, out)
    return nc


def fused_core_inputs(inp, sh, core, ropes):
    d = core_inputs_l0(inp, sh, core, ropes)
    d.update(core_inputs_l1(inp, sh, core))
    return d


def fused_shared_inputs(inp, n_exp=N_EXP):
    sh = shared_inputs(inp)
    sh.update(shared_inputs_l1(inp))
    sh.update(moe_shared_inputs(inp, 0, n_exp))
    sh.update(moe_shared_inputs(inp, 1, n_exp))
    return sh


def assemble_output(results):
    out = np.zeros((4, SEQ, D), np.float32)
    for c in range(8):
        b, hf = c // 2, c % 2
        y = np.asarray(results[c]["outT"]).T
        if hf == 0:
            out[b, :HALF] = y
        else:
            out[b, HALF:] = y[::-1]
    return out


def kernel(**inp):
    n = 8
    inp = {k: np.asarray(v) for k, v in inp.items()}
    sh = fused_shared_inputs(inp)
    ropes = [rope_tables(0), rope_tables(1)]
    nc = build_fused()
    maps = [fused_core_inputs(inp, sh, c, ropes) for c in range(n)]
    res = run_bass_kernel_spmd(nc, maps, core_ids=list(range(n)))
    return assemble_output(res.results)
```
